# Optimizing a Trainium2 kernel written in Bass

```python
import jax, jax.numpy as jnp
from jax import lax
import numpy as np

D_MODEL = 1024
BATCH = 32
SEQ = 2048
DEPTH = 1

D_MIX = D_MODEL
SB_HEADS = 8
SB_HEAD_DIM = 64
SB_WIDTH = SB_HEADS * SB_HEAD_DIM
MLA_HEADS = 8
MLA_NOPE = 64
MLA_ROPE = 32
MLA_V = 64
MLA_WIDTH = MLA_HEADS * MLA_V
Q_LORA = 256
KV_LORA = 128
ROPE_BASE = 10000.0
Q_BLOCK = 128
SPLITS = (SB_WIDTH, 2 * SB_WIDTH, 3 * SB_WIDTH, 3 * SB_WIDTH + Q_LORA, 3 * SB_WIDTH + Q_LORA + KV_LORA)
IN_COLS = 3 * SB_WIDTH + Q_LORA + KV_LORA + MLA_ROPE
N_GROUPS = 4
EXPERTS_PER_GROUP = 8
N_EXPERTS = N_GROUPS * EXPERTS_PER_GROUP
TOP_K = 2
D_EXPERT = 256
EPS = 1e-6

kernel_name = "hybrid_sb_mla_hmoe_layer"


def _rmsnorm(x, g):
    x32 = x.astype(jnp.float32)
    y = x32 * lax.rsqrt(jnp.mean(x32 * x32, axis=-1, keepdims=True) + EPS)
    return y.astype(x.dtype) * g


def _rope(x, pos):
    half = x.shape[-1] // 2
    inv_freq = ROPE_BASE ** (-jnp.arange(half, dtype=jnp.float32) / half)
    ang = pos.astype(jnp.float32)[..., None] * inv_freq
    cos = jnp.cos(ang)[:, :, None, :].astype(x.dtype)
    sin = jnp.sin(ang)[:, :, None, :].astype(x.dtype)
    x1, x2 = x[..., :half], x[..., half:]
    return jnp.concatenate([x1 * cos - x2 * sin, x2 * cos + x1 * sin], axis=-1)


def _stick_breaking_attention(q, k, v):
    S = q.shape[2]
    scale = SB_HEAD_DIM ** -0.5
    outs = []
    for i in range(S // Q_BLOCK):
        q0 = i * Q_BLOCK
        kv_len = q0 + Q_BLOCK
        z = jnp.einsum('bhqd,bhkd->bhqk', q[:, :, q0:kv_len], k[:, :, :kv_len]).astype(jnp.float32) * scale
        t_idx = q0 + jnp.arange(Q_BLOCK)[:, None]
        s_idx = jnp.arange(kv_len)[None, :]
        strict = s_idx < t_idx
        log1m = jnp.where(strict, -jax.nn.softplus(z), 0.0)
        after = lax.cumsum(log1m, axis=log1m.ndim - 1, reverse=True) - log1m
        a = jnp.where(strict, jnp.exp(jax.nn.log_sigmoid(z) + after), 0.0)
        outs.append(jnp.einsum('bhqk,bhkd->bhqd', a.astype(v.dtype), v[:, :, :kv_len]))
    return jnp.concatenate(outs, axis=2)


def _causal_softmax_attention(q, k, v):
    S = q.shape[2]
    scale = q.shape[-1] ** -0.5
    outs = []
    for i in range(S // Q_BLOCK):
        q0 = i * Q_BLOCK
        kv_len = q0 + Q_BLOCK
        s = jnp.einsum('bhqd,bhkd->bhqk', q[:, :, q0:kv_len], k[:, :, :kv_len]).astype(jnp.float32) * scale
        causal = jnp.arange(kv_len)[None, :] <= (q0 + jnp.arange(Q_BLOCK)[:, None])
        p = jax.nn.softmax(jnp.where(causal, s, -jnp.inf), axis=-1)
        outs.append(jnp.einsum('bhqk,bhkd->bhqd', p.astype(v.dtype), v[:, :, :kv_len]))
    return jnp.concatenate(outs, axis=2)


def _mixer(h, positions, w_in, q_norm, w_uq, kv_norm, w_ukv, sb_out_norm, mla_out_norm, w_out):
    B, S, _ = h.shape
    proj = h @ w_in
    q_sb, k_sb, v_sb, c_q, c_kv, k_r = jnp.split(proj, SPLITS, axis=-1)
    heads = lambda t: t.reshape(B, S, SB_HEADS, SB_HEAD_DIM).transpose(0, 2, 1, 3)
    o_sb = _stick_breaking_attention(heads(q_sb), heads(k_sb), heads(v_sb))
    o_sb = o_sb.transpose(0, 2, 1, 3).reshape(B, S, SB_WIDTH)
    q = (_rmsnorm(c_q, q_norm) @ w_uq).reshape(B, S, MLA_HEADS, MLA_NOPE + MLA_ROPE)
    q = jnp.concatenate([q[..., :MLA_NOPE], _rope(q[..., MLA_NOPE:], positions)], axis=-1)
    kv = (_rmsnorm(c_kv, kv_norm) @ w_ukv).reshape(B, S, MLA_HEADS, MLA_NOPE + MLA_V)
    k_rope = jnp.broadcast_to(_rope(k_r[:, :, None, :], positions), (B, S, MLA_HEADS, MLA_ROPE))
    k = jnp.concatenate([kv[..., :MLA_NOPE], k_rope], axis=-1)
    v = kv[..., MLA_NOPE:]
    o_mla = _causal_softmax_attention(q.transpose(0, 2, 1, 3), k.transpose(0, 2, 1, 3), v.transpose(0, 2, 1, 3))
    o_mla = o_mla.transpose(0, 2, 1, 3).reshape(B, S, MLA_WIDTH)
    o = jnp.concatenate([_rmsnorm(o_sb, sb_out_norm), _rmsnorm(o_mla, mla_out_norm)], axis=-1)
    return o @ w_out


def _hierarchical_moe(h, w_group_router, b_group_router, w_expert_router, b_expert_router, w_gate, w_up, w_down):
    B, S, D = h.shape
    T = B * S
    tok = h.reshape(T, D)
    p_group = jax.nn.softmax((tok @ w_group_router + b_group_router).astype(jnp.float32), axis=-1)
    g_val, g_idx = lax.top_k(p_group, 1)
    g_val, g_idx = g_val[:, 0], g_idx[:, 0]
    e_logits = (tok @ w_expert_router + b_expert_router).astype(jnp.float32).reshape(T, N_GROUPS, EXPERTS_PER_GROUP)
    sel = jnp.broadcast_to(g_idx[:, None, None], (T, 1, EXPERTS_PER_GROUP))
    local = jnp.take_along_axis(e_logits, sel, axis=1)[:, 0]
    e_val, e_idx = lax.top_k(jax.nn.softmax(local, axis=-1), TOP_K)
    weights = g_val[:, None] * e_val / jnp.sum(e_val, axis=-1, keepdims=True)
    flat_e = (g_idx[:, None] * EXPERTS_PER_GROUP + e_idx).reshape(-1)
    flat_w = weights.reshape(-1)
    flat_tok = jnp.arange(T * TOP_K, dtype=jnp.int32) // TOP_K
    order = jnp.argsort(flat_e)
    tok_sorted = flat_tok[order]
    group_sizes = jnp.bincount(flat_e, length=N_EXPERTS).astype(jnp.int32)
    xs = tok[tok_sorted]
    hid = jax.nn.silu(lax.ragged_dot(xs, w_gate, group_sizes)) * lax.ragged_dot(xs, w_up, group_sizes)
    out = lax.ragged_dot(hid, w_down, group_sizes)
    y = jax.ops.segment_sum(out * flat_w[order][:, None].astype(out.dtype), tok_sorted, num_segments=T)
    return y.reshape(B, S, D).astype(h.dtype)


def setup_inputs(seed: int = 0) -> dict:
    key = jax.random.key(seed)
    ks = jax.random.split(key, 20)
    f32 = jnp.float32
    nrm = lambda k, shape, scale: jax.random.normal(k, shape, f32) * scale
    gain = lambda k, n: 1.0 + 0.02 * jax.random.normal(k, (DEPTH, n), f32)
    return {
        "x": jax.random.normal(ks[0], (BATCH, SEQ, D_MODEL), f32),
        "positions": jnp.broadcast_to(jnp.arange(SEQ, dtype=jnp.int32), (BATCH, SEQ)),
        "attn_norm": gain(ks[1], D_MODEL),
        "w_in": nrm(ks[2], (DEPTH, D_MODEL, IN_COLS), D_MODEL ** -0.5),
        "q_norm": gain(ks[3], Q_LORA),
        "w_uq": nrm(ks[4], (DEPTH, Q_LORA, MLA_HEADS * (MLA_NOPE + MLA_ROPE)), Q_LORA ** -0.5),
        "kv_norm": gain(ks[5], KV_LORA),
        "w_ukv": nrm(ks[6], (DEPTH, KV_LORA, MLA_HEADS * (MLA_NOPE + MLA_V)), KV_LORA ** -0.5),
        "sb_out_norm": gain(ks[7], SB_WIDTH),
        "mla_out_norm": gain(ks[8], MLA_WIDTH),
        "w_out": nrm(ks[9], (DEPTH, D_MIX, D_MODEL), D_MIX ** -0.5),
        "ffn_norm": gain(ks[10], D_MODEL),
        "w_group_router": nrm(ks[11], (DEPTH, D_MODEL, N_GROUPS), D_MODEL ** -0.5),
        "b_group_router": nrm(ks[12], (DEPTH, N_GROUPS), 0.01),
        "w_expert_router": nrm(ks[13], (DEPTH, D_MODEL, N_EXPERTS), D_MODEL ** -0.5),
        "b_expert_router": nrm(ks[14], (DEPTH, N_EXPERTS), 0.01),
        "w_gate": nrm(ks[15], (DEPTH, N_EXPERTS, D_MODEL, D_EXPERT), D_MODEL ** -0.5),
        "w_up": nrm(ks[16], (DEPTH, N_EXPERTS, D_MODEL, D_EXPERT), D_MODEL ** -0.5),
        "w_down": nrm(ks[17], (DEPTH, N_EXPERTS, D_EXPERT, D_MODEL), D_EXPERT ** -0.5),
        "final_norm": 1.0 + 0.02 * jax.random.normal(ks[18], (D_MODEL,), f32),
    }


def reference(x, positions, attn_norm, w_in, q_norm, w_uq, kv_norm, w_ukv, sb_out_norm, mla_out_norm,
              w_out, ffn_norm, w_group_router, b_group_router, w_expert_router, b_expert_router,
              w_gate, w_up, w_down, final_norm):
    for l in range(DEPTH):
        h = _rmsnorm(x, attn_norm[l])
        x = x + _mixer(h, positions, w_in[l], q_norm[l], w_uq[l], kv_norm[l], w_ukv[l],
                       sb_out_norm[l], mla_out_norm[l], w_out[l])
        h = _rmsnorm(x, ffn_norm[l])
        x = x + _hierarchical_moe(h, w_group_router[l], b_group_router[l], w_expert_router[l],
                                  b_expert_router[l], w_gate[l], w_up[l], w_down[l])
    return _rmsnorm(x, final_norm)
```

```python
import numpy as np
import ml_dtypes
from contextlib import ExitStack
import concourse.bass as bass
import concourse.mybir as mybir
from concourse.bass_utils import run_bass_kernel_spmd

F32 = mybir.dt.float32
BF16 = mybir.dt.bfloat16
I32 = mybir.dt.int32
AF = mybir.ActivationFunctionType
ALU = mybir.AluOpType
AX = mybir.AxisListType

ENGS = ["pe", "act", "dve", "pool", "sp"]
DMA_ENGS = ["sp", "pool"]
ND = 8
EPS = 1e-6
TWO_PI = float(2 * np.pi)


class _Proxy:
    def __init__(self):
        self.call = None

    def __getattr__(self, name):
        def f(*a, **k):
            self.call = (name, a, k)
            return self
        return f


class Sched:
    def __init__(self, nc, es):
        self.nc = nc
        self.eng = {"pe": nc.tensor, "act": nc.scalar, "dve": nc.vector, "pool": nc.gpsimd, "sp": nc.sync}
        self.sem = {e: es.enter_context(nc.semaphore("c_" + e)) for e in ENGS}
        self.dsem = {e: [es.enter_context(nc.semaphore(f"d_{e}_{i}")) for i in range(ND)] for e in DMA_ENGS}
        self.cnt = {e: 0 for e in ENGS}
        self.ndma = {e: 0 for e in DMA_ENGS}
        self.dlast = {e: {} for e in DMA_ENGS}
        self.lastw = {}
        self.readers = {}
        self.seen = {e: {} for e in ENGS}
        self.nins = 0
        self.rec = None

    def record(self, chunk_fns):
        self.rec = []
        for f in chunk_fns:
            f()
        out, self.rec = self.rec, None
        return out

    def _semobj(self, key):
        return self.sem[key[1]] if key[0] == "c" else self.dsem[key[1]][key[2]]

    def _collect(self, reads, writes):
        toks = {}

        def add(k, v):
            if toks.get(k, 0) < v:
                toks[k] = v
        for r in reads:
            t = self.lastw.get(r)
            if t is not None:
                add(*t)
        for w in writes:
            t = self.lastw.get(w)
            if t is not None:
                add(*t)
            for k, v in self.readers.get(w, {}).items():
                add(k, v)
        return toks

    def _wait(self, eng, toks):
        seen = self.seen[eng]
        for k, v in toks.items():
            if k[0] == "c" and k[1] == eng and eng == "pe":
                continue
            if seen.get(k, 0) >= v:
                continue
            self.eng[eng].wait_ge(self._semobj(k), v)
            seen[k] = v
            self.nins += 1

    def _record(self, tok, reads, writes):
        for w in writes:
            self.lastw[w] = tok
            self.readers[w] = {}
        for r in reads:
            d = self.readers.setdefault(r, {})
            if d.get(tok[0], 0) < tok[1]:
                d[tok[0]] = tok[1]

    def op(self, eng, fn, reads=(), writes=()):
        if self.rec is not None:
            p = _Proxy()
            fn(p)
            name, a, k = p.call
            self.rec.append(lambda: self.op(eng, lambda e: getattr(e, name)(*a, **k), reads, writes))
            return
        toks = self._collect(reads, writes)
        self._wait(eng, toks)
        ins = fn(self.eng[eng])
        self.cnt[eng] += 1
        ins.then_inc(self.sem[eng], 1)
        self.nins += 1
        tok = (("c", eng), self.cnt[eng])
        self._record(tok, reads, writes)

    def dma(self, eng, fn, reads=(), writes=()):
        if self.rec is not None:
            p = _Proxy()
            fn(p)
            name, a, k = p.call
            self.rec.append(lambda: self.dma(eng, lambda e: getattr(e, name)(*a, **k), reads, writes))
            return
        n = self.ndma[eng]
        self.ndma[eng] += 1
        slot = n % ND
        val = 16 * (n // ND + 1)
        toks = self._collect(reads, writes)
        key = ("d", eng, slot)
        if val > 16:
            toks[key] = max(toks.get(key, 0), val - 16)
        self._wait(eng, toks)
        ins = fn(self.eng[eng])
        ins.then_inc(self.dsem[eng][slot], 16)
        self.nins += 1
        self.dlast[eng][slot] = val
        self._record((key, val), reads, writes)

    def barrier(self):
        toks = {}
        for e in ENGS:
            if self.cnt[e] > 0:
                toks[("c", e)] = self.cnt[e]
        for q in DMA_ENGS:
            for slot, val in self.dlast[q].items():
                toks[("d", q, slot)] = val
        for e in ENGS:
            t = {k: v for k, v in toks.items() if not (k[0] == "c" and k[1] == e)}
            self._wait(e, t)

    def final_wait(self, eng="sp"):
        toks = {}
        for q in DMA_ENGS:
            for slot, val in self.dlast[q].items():
                toks[("d", q, slot)] = val
        for e in ENGS:
            if self.cnt[e] > 0 and e != eng:
                toks[("c", e)] = self.cnt[e]
        self._wait(eng, toks)


def build(NSEQ=4, debug=False, n_experts=32, phases=("attn", "moe"), nd_sb=0, nd_mla=0):
    nc = bass.Bass("TRN2", target_bir_lowering=False)

    def din(name, shape, dtype=F32):
        return nc.dram_tensor(name, shape, dtype, kind="ExternalInput").ap()

    x = din("x", [NSEQ, 2048, 1024])
    positions = din("positions", [NSEQ, 2048], I32)
    attn_norm = din("attn_norm", [1, 1024])
    w_in = din("w_in", [1024, 1952])
    q_norm = din("q_norm", [256, 1])
    w_uq = din("w_uq", [256, 768])
    kv_norm = din("kv_norm", [128, 1])
    w_ukv = din("w_ukv", [128, 1024])
    out_norm = din("out_norm", [1024, 1])
    w_out = din("w_out", [1024, 1024])
    ffn_norm = din("ffn_norm", [1, 1024])
    w_gr = din("w_group_router", [1024, 4])
    b_gr = din("b_group_router", [1, 4])
    w_er = din("w_expert_router", [1024, 32])
    b_er = din("b_expert_router", [1, 32])
    w_gate = din("w_gate", [32, 1024, 256])
    w_up = din("w_up", [32, 1024, 256])
    w_down = din("w_down", [32, 256, 1024])
    final_norm = din("final_norm", [1, 1024])
    consts = din("consts", [128, 640], BF16)
    invf = din("invf", [128, 1])
    y = nc.dram_tensor("y", [NSEQ, 2048, 1024], F32, kind="ExternalOutput").ap()
    x1s = nc.dram_tensor("x1s", [NSEQ, 2048, 1024], F32,
                         kind="ExternalOutput" if debug else "Internal").ap()

    dbg_oT = nc.dram_tensor("dbg_oT", [128, 8, 2048], BF16, kind="ExternalOutput").ap() if debug else None
    dbg_hT = nc.dram_tensor("dbg_hT", [128, 8, 2048], BF16, kind="ExternalOutput").ap() if debug else None
    dbg_lat = nc.dram_tensor("dbg_lat", [128, 4, 2048], BF16, kind="ExternalOutput").ap() if debug else None

    with ExitStack() as es:
        S = Sched(nc, es)

        uid = [0]

        def sb(stack, name, shape, dtype=F32):
            uid[0] += 1
            return stack.enter_context(nc.sbuf_tensor(f"{name}_{uid[0]}", shape, dtype))

        PS = [es.enter_context(nc.psum_tensor(f"ps{i}", [128, 1024], F32)) for i in range(4)]
        PSB = [p[:].bitcast(BF16) for p in PS]

        def bank(k):
            return PS[k // 2][:, (k % 2) * 512:(k % 2) * 512 + 512]

        def bank_bf(k):
            return PSB[k // 2][:, (k % 2) * 1024:(k % 2) * 1024 + 1024]

        def bk(k):
            return f"B{k}"

        cst = sb(es, "cst", [128, 640], BF16)
        ident = cst[:, 0:128]
        tri = cst[:, 128:256]
        mstrict = cst[:, 256:384]
        mcausal = cst[:, 384:512]
        slow = cst[:, 512:640]
        ones_bf = sb(es, "ones_bf", [128, 128], BF16)
        ones_f = sb(es, "ones_f", [128, 64], F32)
        epsc = sb(es, "epsc", [128, 1])
        onec = sb(es, "onec", [128, 1])
        invf_sb = sb(es, "invf_sb", [128, 1])
        gbc = sb(es, "gbc", [128, 1024])
        w_uq_sb = sb(es, "w_uq_sb", [128, 2, 8, 96], BF16)
        w2_sb = sb(es, "w2_sb", [128, 2, 8, 96], BF16)
        w_ukv_sb = sb(es, "w_ukv_sb", [128, 1024], BF16)
        wv_sb = sb(es, "wv_sb", [128, 8, 64], BF16)
        wkr = sb(es, "wkr", [128, 8, 96], BF16)
        wkr2 = sb(es, "wkr2", [128, 8, 96], BF16)
        w_out_sb = sb(es, "w_out_sb", [128, 8, 1024], BF16)
        wr_sb = sb(es, "wr_sb", [128, 8, 36], BF16)
        br_bc = sb(es, "br_bc", [128, 36])
        hT = sb(es, "hT", [128, 8, 2048], BF16)
        Wfull = sb(es, "Wfull", [128, 16, 32])
        rs2all = sb(es, "rs2all", [128, 16])

        S.dma("sp", lambda e: e.dma_start(out=cst[:], in_=consts[:, :]), writes=["cst"])
        S.dma("sp", lambda e: e.dma_start(out=invf_sb[:], in_=invf[:, :]), writes=["invf"])
        S.op("pool", lambda e: e.memset(ones_bf[:], 1.0), writes=["ones_bf"])
        S.op("pool", lambda e: e.memset(ones_f[:], 1.0), writes=["ones_f"])
        S.op("pool", lambda e: e.memset(epsc[:], EPS), writes=["epsc"])
        S.op("pool", lambda e: e.memset(onec[:], 1.0), writes=["onec"])
        S.op("pool", lambda e: e.memset(w2_sb[:], 0.0), writes=["w2"])
        S.op("pool", lambda e: e.memset(wkr[:], 0.0), writes=["wkr"])
        S.op("pool", lambda e: e.memset(wkr2[:], 0.0), writes=["wkr2"])
        with ExitStack() as ss:
            stg = sb(ss, "stg", [128, 2048])
            qn = sb(ss, "qn", [128, 2])
            kvn = sb(ss, "kvn", [128, 1])
            og = sb(ss, "og", [128, 8])
            S.dma("sp", lambda e: e.dma_start(out=stg[:, 0:1536].rearrange("p (c n) -> p c n", c=2),
                                              in_=w_uq.rearrange("(c p) n -> p c n", p=128)), writes=["stg"])
            for c in range(2):
                S.dma("sp", lambda e: e.dma_start(out=qn[:, c:c + 1], in_=q_norm[c * 128:(c + 1) * 128, :]), writes=["qn"])
            stv = stg[:, 0:1536].rearrange("p (c h d) -> p c h d", c=2, h=8)
            for c in range(2):
                S.op("dve", lambda e: e.tensor_scalar(w_uq_sb[:, c], stv[:, c], qn[:, c:c + 1], None, ALU.mult),
                     reads=["stg", "qn"], writes=["w_uq"])
                S.op("dve", lambda e: e.tensor_scalar(w2_sb[:, c, :, 64:80], stv[:, c, :, 80:96], qn[:, c:c + 1], -1.0,
                                                      ALU.mult, ALU.mult), reads=["stg", "qn"], writes=["w2"])
                S.op("dve", lambda e: e.tensor_scalar(w2_sb[:, c, :, 80:96], stv[:, c, :, 64:80], qn[:, c:c + 1], None,
                                                      ALU.mult), reads=["stg", "qn"], writes=["w2"])
            S.dma("sp", lambda e: e.dma_start(out=stg[:, 0:1024], in_=w_ukv[:, :]), writes=["stg"])
            S.dma("sp", lambda e: e.dma_start(out=kvn[:], in_=kv_norm[:, :]), writes=["kvn"])
            S.op("dve", lambda e: e.tensor_scalar(w_ukv_sb[:], stg[:, 0:1024], kvn[:, 0:1], None, ALU.mult),
                 reads=["stg", "kvn"], writes=["w_ukv"])
            S.op("dve", lambda e: e.tensor_scalar(wv_sb[:], stg[:, 0:1024].rearrange("p (h d) -> p h d", h=8)[:, :, 64:128],
                                                  kvn[:, 0:1], None, ALU.mult), reads=["stg", "kvn"], writes=["wv"])
            wi_c = w_in.rearrange("(c p) n -> p c n", p=128)
            S.dma("pool", lambda e: e.dma_start(out=wkr[:, :, 64:96], in_=wi_c[:, :, 1920:1952]), writes=["wkr"])
            S.dma("pool", lambda e: e.dma_start(out=wkr2[:, :, 80:96], in_=wi_c[:, :, 1920:1936]), writes=["wkr2"])
            S.dma("pool", lambda e: e.dma_start(out=wkr2[:, :, 64:80], in_=wi_c[:, :, 1936:1952]), writes=["wkr2"])
            S.op("pool", lambda e: e.tensor_scalar(wkr2[:, :, 64:80], wkr2[:, :, 64:80], -1.0, None, ALU.mult),
                 reads=["wkr2"], writes=["wkr2"])
            for j in range(8):
                S.dma("sp", lambda e: e.dma_start(out=og[:, j:j + 1], in_=out_norm[j * 128:(j + 1) * 128, :]), writes=["og"])
            wo_c = w_out.rearrange("(c p) n -> p c n", p=128)
            for jj in range(4):
                S.dma("sp", lambda e: e.dma_start(out=stg[:].rearrange("p (c n) -> p c n", c=2),
                                                  in_=wo_c[:, 2 * jj:2 * jj + 2, :]), writes=["stg"])
                for jl in range(2):
                    j = 2 * jj + jl
                    S.op("dve", lambda e: e.tensor_scalar(w_out_sb[:, j, :], stg[:, jl * 1024:(jl + 1) * 1024],
                                                          og[:, j:j + 1], None, ALU.mult),
                         reads=["stg", "og"], writes=["w_out"])
            S.dma("pool", lambda e: e.dma_start(out=wr_sb[:, :, 0:4], in_=w_gr.rearrange("(c p) n -> p c n", p=128)), writes=["wr"])
            S.dma("pool", lambda e: e.dma_start(out=wr_sb[:, :, 4:36], in_=w_er.rearrange("(c p) n -> p c n", p=128)), writes=["wr"])
            S.dma("sp", lambda e: e.dma_start(out=br_bc[:, 0:4], in_=b_gr[0:1, :].partition_broadcast(128)), writes=["br"])
            S.dma("sp", lambda e: e.dma_start(out=br_bc[:, 4:36], in_=b_er[0:1, :].partition_broadcast(128)), writes=["br"])
            S.barrier()

        def rstd_from_ss(rs, ss_ap, n, rkey, sskey):
            S.op("act", lambda e: e.activation(rs, ss_ap, AF.Sqrt, bias=epsc[:], scale=1.0 / n),
                 reads=[sskey, "epsc"], writes=[rkey])
            S.op("dve", lambda e: e.reciprocal(rs, rs), reads=[rkey], writes=[rkey])

        def transpose_to_hT(src_bf, srckey, t, bnk):
            pb = bank_bf(bnk)
            for c in range(8):
                S.op("pe", lambda e: e.transpose(pb[:, c * 128:(c + 1) * 128], src_bf[:, c * 128:(c + 1) * 128], ident),
                     reads=[srckey, "cst", ("hTt", t)] if c == 0 else [srckey, "cst"], writes=[bk(bnk)])
            S.op("act", lambda e: e.copy(hT[:, :, t * 128:(t + 1) * 128], pb.rearrange("p (c n) -> p c n", c=8)),
                 reads=[bk(bnk)], writes=[("hT", t // 4), ("hTt", t)])

        for b in range(NSEQ):
            if "attn" in phases:
              with ExitStack() as s1:
                cqTn = sb(s1, "cqTn", [128, 2, 2048], BF16)
                ckvTn = sb(s1, "ckvTn", [128, 2048], BF16)
                krope = sb(s1, "krope", [128, 2048], BF16)
                oT = sb(s1, "oT", [128, 8, 2048], BF16)
                sinT = sb(s1, "sinT", [128, 2048])
                cosT = sb(s1, "cosT", [128, 2048])

                with ExitStack() as sa:
                    xts = [sb(sa, f"xt{i}", [128, 1024]) for i in range(2)]
                    hbs = [sb(sa, f"hb{i}", [128, 1024], BF16) for i in range(2)]
                    junk = sb(sa, "junk", [128, 1024], BF16)
                    ssA = sb(sa, "ssA", [128, 2])
                    rsA = sb(sa, "rsA", [128, 2])
                    wlat = sb(sa, "wlat", [128, 8, 384], BF16)
                    latf = sb(sa, "latf", [128, 3, 512])
                    latsq = sb(sa, "latsq", [128, 3, 512], BF16)
                    rbc = sb(sa, "rbc", [128, 2, 512])
                    posi = sb(sa, "posi", [128, 512], I32)
                    ang = sb(sa, "ang", [128, 512])
                    rtmp = sb(sa, "rtmp", [128, 512])
                    ru = sb(sa, "ru", [128, 512])
                    ta = sb(sa, "ta", [128, 512])
                    tb = sb(sa, "tb", [128, 512])

                    S.dma("sp", lambda e: e.dma_start(out=gbc[:], in_=attn_norm[0:1, :].partition_broadcast(128)), writes=["gbc"])
                    S.dma("pool", lambda e: e.dma_start(out=wlat[:], in_=w_in.rearrange("(c p) n -> p c n", p=128)[:, :, 1536:1920]),
                          writes=["wlat"])
                    for t in range(16):
                        xt = xts[t % 2]
                        hb = hbs[t % 2]
                        xk, hk = f"xt{t % 2}", f"hb{t % 2}"
                        sl = t % 2
                        S.dma("sp", lambda e: e.dma_start(out=xt[:], in_=x[b, t * 128:(t + 1) * 128, :]), writes=[xk])
                        S.op("dve", lambda e: e.memset(ssA[:, sl:sl + 1], 0.0), writes=[f"ssA{sl}"])
                        S.op("act", lambda e: e.activation(junk[:], xt[:], AF.Square, accum_out=ssA[:, sl:sl + 1]),
                             reads=[xk], writes=["junk", f"ssA{sl}"])
                        rstd_from_ss(rsA[:, sl:sl + 1], ssA[:, sl:sl + 1], 1024.0, f"rsA{sl}", f"ssA{sl}")
                        S.op("dve", lambda e: e.scalar_tensor_tensor(hb[:], xt[:], rsA[:, sl:sl + 1], gbc[:], ALU.mult, ALU.mult),
                             reads=[xk, f"rsA{sl}", "gbc"], writes=[hk])
                        transpose_to_hT(hb, hk, t, t % 2)

                    for G in range(4):
                        gs = slice(G * 512, (G + 1) * 512)
                        hTk = ("hT", G)
                        for lc in range(3):
                            bn = 2 + lc
                            for c in range(8):
                                S.op("pe", lambda e: e.matmul(bank(bn), wlat[:, c, lc * 128:(lc + 1) * 128], hT[:, c, gs],
                                                              start=(c == 0), stop=(c == 7)),
                                     reads=["wlat", hTk], writes=[bk(bn)])
                            S.op("act", lambda e: e.copy(latf[:, lc, :], bank(bn)), reads=[bk(bn)], writes=[("latf", lc)])
                            S.op("dve", lambda e: e.tensor_tensor(latsq[:, lc, :], latf[:, lc, :], latf[:, lc, :], ALU.mult),
                                 reads=[("latf", lc)], writes=[("latsq", lc)])
                        S.op("pe", lambda e: e.matmul(bank(5), ones_bf[:], latsq[:, 0, :], start=True, stop=False),
                             reads=["ones_bf", ("latsq", 0)], writes=[bk(5)])
                        S.op("pe", lambda e: e.matmul(bank(5), ones_bf[:], latsq[:, 1, :], start=False, stop=True),
                             reads=["ones_bf", ("latsq", 1)], writes=[bk(5)])
                        S.op("pe", lambda e: e.matmul(bank(6), ones_bf[:], latsq[:, 2, :], start=True, stop=True),
                             reads=["ones_bf", ("latsq", 2)], writes=[bk(6)])
                        for (ri, bnk_, nn) in ((0, 5, 256.0), (1, 6, 128.0)):
                            S.op("act", lambda e: e.activation(rbc[:, ri, :], bank(bnk_), AF.Ln, bias=epsc[:], scale=1.0 / nn),
                                 reads=[bk(bnk_), "epsc"], writes=[("rbc", ri)])
                            S.op("act", lambda e: e.activation(rbc[:, ri, :], rbc[:, ri, :], AF.Exp, scale=-0.5),
                                 reads=[("rbc", ri)], writes=[("rbc", ri)])
                        for lc in range(2):
                            S.op("dve", lambda e: e.tensor_tensor(cqTn[:, lc, gs], latf[:, lc, :], rbc[:, 0, :], ALU.mult),
                                 reads=[("latf", lc), ("rbc", 0)], writes=["cqTn"])
                        S.op("dve", lambda e: e.tensor_tensor(ckvTn[:, gs], latf[:, 2, :], rbc[:, 1, :], ALU.mult),
                             reads=[("latf", 2), ("rbc", 1)], writes=["ckvTn"])
                        P = slice(64, 96)
                        S.dma("sp", lambda e: e.dma_start(out=posi[P, :], in_=positions[b:b + 1, gs].partition_broadcast(32)),
                              writes=["posi"])
                        S.op("dve", lambda e: e.tensor_copy(ang[P, :], posi[P, :]), reads=["posi"], writes=["ang"])
                        S.op("dve", lambda e: e.tensor_scalar(ang[P, :], ang[P, :], invf_sb[P, 0:1], None, ALU.mult),
                             reads=["ang", "invf"], writes=["ang"])
                        for (dst, dk, add) in ((sinT, "sinT", 0.0), (cosT, "cosT", float(np.pi / 2))):
                            S.op("dve", lambda e: e.tensor_scalar(ru[P, :], ang[P, :], add, None, ALU.add),
                                 reads=["ang"], writes=["ru"])
                            S.op("dve", lambda e: e.tensor_scalar(rtmp[P, :], ru[P, :], 1.0 / TWO_PI, None, ALU.mult),
                                 reads=["ru"], writes=["rtmp"])
                            S.op("dve", lambda e: e.tensor_copy(posi[P, :], rtmp[P, :]), reads=["rtmp"], writes=["posi"])
                            S.op("dve", lambda e: e.tensor_copy(rtmp[P, :], posi[P, :]), reads=["posi"], writes=["rtmp"])
                            S.op("dve", lambda e: e.scalar_tensor_tensor(rtmp[P, :], rtmp[P, :], -TWO_PI, ru[P, :], ALU.mult, ALU.add),
                                 reads=["rtmp", "ru"], writes=["rtmp"])
                            S.op("dve", lambda e: e.tensor_scalar(rtmp[P, :], rtmp[P, :], float(np.pi), float(-np.pi), ALU.min, ALU.max),
                                 reads=["rtmp"], writes=["rtmp"])
                            S.op("act", lambda e: e.activation(dst[P, gs], rtmp[P, :], AF.Sin), reads=["rtmp"], writes=[(dk, G)])
                        for (wt, wk, bn) in ((wkr, "wkr", 7), (wkr2, "wkr2", 5)):
                            for c in range(8):
                                S.op("pe", lambda e: e.matmul(bank(bn)[0:96, :], wt[:, c, :], hT[:, c, gs], start=(c == 0), stop=(c == 7)),
                                     reads=[wk, hTk], writes=[bk(bn)])
                        S.op("dve", lambda e: e.tensor_tensor(ta[P, :], bank(7)[P, :], cosT[P, gs], ALU.mult),
                             reads=[bk(7), ("cosT", G)], writes=["ta"])
                        S.op("dve", lambda e: e.tensor_tensor(tb[P, :], bank(5)[P, :], sinT[P, gs], ALU.mult),
                             reads=[bk(5), ("sinT", G)], writes=["tb"])
                        S.op("dve", lambda e: e.tensor_tensor(krope[P, gs], ta[P, :], tb[P, :], ALU.add),
                             reads=["ta", "tb"], writes=["krope"])
                    S.barrier()

                if debug and b == 0:
                    S.dma("sp", lambda e: e.dma_start(out=dbg_hT[:, :, :], in_=hT[:]), reads=[("hT", g) for g in range(4)], writes=["dbg_hT"])
                    S.barrier()
                def run_passes(proj_chunks, loop_iters, npass):
                    for ch in proj_chunks(0):
                        ch()
                    for j in range(npass):
                        nxt = S.record(proj_chunks(j + 1)) if j + 1 < npass else []
                        iters = loop_iters(j)
                        per = -(-len(nxt) // max(1, len(iters) - 6))
                        for idx, it in enumerate(iters):
                            it()
                            if idx >= 2:
                                for _ in range(per):
                                    if nxt:
                                        nxt.pop(0)()
                        while nxt:
                            nxt.pop(0)()

                with ExitStack() as sp_:
                    wsb2 = [sb(sp_, f"wsb{i}", [128, 8, 3, 128], BF16) for i in range(2)]
                    qs02 = [[sb(sp_, f"qs0_{i}_{h}", [128, 2048], BF16) for h in range(2)] for i in range(2)]
                    ksT2 = [sb(sp_, f"ksT{i}", [128, 2048], BF16) for i in range(2)]
                    vs2 = [sb(sp_, f"vs{i}", [128, 16, 128], BF16) for i in range(2)]
                    E1 = [sb(sp_, f"E1_{i}", [128, 512]) for i in range(3)]
                    Lb = [sb(sp_, f"Lb_{i}", [128, 512], BF16) for i in range(3)]
                    Xe = [sb(sp_, f"Xe_{i}", [128, 512]) for i in range(2)]
                    At = [sb(sp_, f"At_{i}", [128, 512], BF16) for i in range(3)]
                    S32 = [sb(sp_, f"S32_{i}", [128, 512]) for i in range(2)]
                    Sb = [sb(sp_, f"Sb_{i}", [128, 512], BF16) for i in range(2)]
                    wi_c = w_in.rearrange("(c p) n -> p c n", p=128)

                    def sb_proj_chunks(j):
                        pj = j % 2
                        wsb, qs0, ksT, vs = wsb2[pj], qs02[pj], ksT2[pj], vs2[pj]
                        chunks = []

                        def c_load():
                            for w in range(3):
                                S.dma("pool", lambda e: e.dma_start(out=wsb[:, :, w, :],
                                                                    in_=wi_c[:, :, w * 512 + j * 128:w * 512 + (j + 1) * 128]),
                                      writes=[("wsb", pj, w)])
                            S.op("dve", lambda e: e.memset(qs0[0][64:128, :], 0.0), writes=[("qs0", pj, 0, G) for G in range(4)])
                            S.op("dve", lambda e: e.memset(qs0[1][0:64, :], 0.0), writes=[("qs0", pj, 1, G) for G in range(4)])
                        chunks.append(c_load)
                        for G in range(4):
                            gs = slice(G * 512, (G + 1) * 512)

                            def c_q(G=G, gs=gs):
                                for c in range(8):
                                    S.op("pe", lambda e: e.matmul(bank(6), wsb[:, c, 0, :], hT[:, c, gs], start=(c == 0), stop=(c == 7)),
                                         reads=[("wsb", pj, 0), ("hT", G)], writes=[bk(6)])
                                S.op("dve", lambda e: e.tensor_scalar(qs0[0][0:64, gs], bank(6)[0:64, :], 0.125, None, ALU.mult),
                                     reads=[bk(6)], writes=[("qs0", pj, 0, G)])
                                S.op("dve", lambda e: e.tensor_scalar(qs0[1][64:128, gs], bank(6)[64:128, :], 0.125, None, ALU.mult),
                                     reads=[bk(6)], writes=[("qs0", pj, 1, G)])

                            def c_k(G=G, gs=gs):
                                for c in range(8):
                                    S.op("pe", lambda e: e.matmul(bank(7), wsb[:, c, 1, :], hT[:, c, gs], start=(c == 0), stop=(c == 7)),
                                         reads=[("wsb", pj, 1), ("hT", G)], writes=[bk(7)])
                                S.op("dve", lambda e: e.tensor_copy(ksT[:, gs], bank(7)), reads=[bk(7)], writes=[("ksT", pj, G)])

                            def c_v(G=G):
                                bn = 6 + (G % 2)
                                for tl in range(4):
                                    t = G * 4 + tl
                                    for c in range(8):
                                        S.op("pe", lambda e: e.matmul(bank(bn)[:, tl * 128:(tl + 1) * 128], hT[:, c, t * 128:(t + 1) * 128],
                                                                      wsb[:, c, 2, :], start=(c == 0), stop=(c == 7)),
                                             reads=[("wsb", pj, 2), ("hT", G)], writes=[bk(bn)])
                                S.op("dve", lambda e: e.tensor_copy(vs[:, G * 4:(G + 1) * 4, :], bank(bn).rearrange("p (t n) -> p t n", t=4)),
                                     reads=[bk(bn)], writes=[("vs", pj, G)])
                            chunks += [c_q, c_k, c_v]
                        return chunks

                    def sb_loop_iters(j):
                        pj = j % 2
                        qs0, ksT, vs = qs02[pj], ksT2[pj], vs2[pj]
                        units = [(hl, G, i) for G in range(4) for i in range(4 * G + 3, -1, -1) for hl in range(2)]
                        nU = len(units)

                        def geom(u):
                            hl, G, i = units[u]
                            q0 = max(i, 4 * G) * 128
                            off = q0 - G * 512
                            return dict(hl=hl, G=G, i=i, off=off, cs=slice(off, 512), qsl=slice(q0, (G + 1) * 512),
                                        ksl=slice(i * 128, (i + 1) * 128), diag=(i >= 4 * G), pr=slice(hl * 64, (hl + 1) * 64),
                                        first=(i == 4 * G + 3), last=(i == 0), gs=slice(G * 512, (G + 1) * 512))

                        def st0(u):
                            g = geom(u)
                            zb = u % 2
                            S.op("pe", lambda e: e.matmul(bank(zb)[:, g["cs"]], ksT[:, g["ksl"]], qs0[g["hl"]][:, g["qsl"]], start=True, stop=True),
                                 reads=[("ksT", pj, g["i"] // 4), ("qs0", pj, g["hl"], g["G"])], writes=[bk(zb)])

                        def st1a(u):
                            g = geom(u)
                            zb, cs, off = u % 2, g["cs"], g["off"]
                            e1, e1k = E1[u % 3], f"E1_{u % 3}"
                            S.op("act", lambda e: e.activation(e1[:, cs], bank(zb)[:, cs], AF.Exp), reads=[bk(zb)], writes=[e1k])
                            if g["diag"]:
                                S.op("dve", lambda e: e.tensor_tensor(e1[:, off:off + 128], e1[:, off:off + 128], mstrict, ALU.mult),
                                     reads=[e1k, "cst"], writes=[e1k])

                        def st1b(u):
                            g = geom(u)
                            cs = g["cs"]
                            e1, lb = E1[u % 3], Lb[u % 3]
                            e1k, lbk = f"E1_{u % 3}", f"Lb_{u % 3}"
                            S.op("act", lambda e: e.activation(lb[:, cs], e1[:, cs], AF.Ln, bias=onec[:], scale=1.0),
                                 reads=[e1k, "onec"], writes=[lbk])

                        def st2a(u):
                            g = geom(u)
                            cs, hl = g["cs"], g["hl"]
                            cbk = 2 + u % 2
                            lb, xe = Lb[u % 3], Xe[u % 2]
                            lbk, xek = f"Lb_{u % 3}", f"Xe_{u % 2}"
                            S.op("pe", lambda e: e.matmul(bank(cbk)[:, cs], tri, lb[:, cs], start=True, stop=g["first"]),
                                 reads=["cst", lbk], writes=[bk(cbk)])
                            if not g["first"]:
                                S.op("pe", lambda e: e.matmul(bank(cbk)[:, cs], ones_bf[:], Sb[hl][:, cs], start=False, stop=True),
                                     reads=["ones_bf", f"Sb_{hl}"], writes=[bk(cbk)])
                            S.op("act", lambda e: e.activation(xe[:, cs], bank(cbk)[:, cs], AF.Exp, scale=-1.0), reads=[bk(cbk)], writes=[xek])

                        def st2b(u):
                            g = geom(u)
                            cs, hl = g["cs"], g["hl"]
                            e1, lb, xe, at = E1[u % 3], Lb[u % 3], Xe[u % 2], At[u % 3]
                            e1k, lbk, xek, atk = f"E1_{u % 3}", f"Lb_{u % 3}", f"Xe_{u % 2}", f"At_{u % 3}"
                            if not g["last"]:
                                if g["first"]:
                                    S.op("dve", lambda e: e.memset(S32[hl][:], 0.0), writes=[f"S32_{hl}"])
                                S.op("dve", lambda e: e.tensor_tensor(S32[hl][:, cs], S32[hl][:, cs], lb[:, cs], ALU.add),
                                     reads=[f"S32_{hl}", lbk], writes=[f"S32_{hl}"])
                            S.op("dve", lambda e: e.tensor_tensor(at[:, cs], xe[:, cs], e1[:, cs], ALU.mult), reads=[xek, e1k], writes=[atk])
                            if not g["last"]:
                                S.op("dve", lambda e: e.tensor_copy(Sb[hl][:], S32[hl][:]), reads=[f"S32_{hl}"], writes=[f"Sb_{hl}"])

                        def st3(u):
                            g = geom(u)
                            cs = g["cs"]
                            ob = 4 + g["hl"]
                            at, atk = At[u % 3], f"At_{u % 3}"
                            S.op("pe", lambda e: e.matmul(bank(ob)[0:64, cs], vs[:, g["i"], g["pr"]], at[:, cs], start=g["first"], stop=g["last"],
                                                          skip_group_check=True),
                                 reads=[("vs", pj, g["i"] // 4), atk], writes=[bk(ob)])
                            if g["last"]:
                                S.op("act", lambda e: e.copy(oT[g["pr"], j, g["gs"]], bank(ob)[0:64, :]), reads=[bk(ob)], writes=[("oT", j)])

                        def mk(k):
                            def it():
                                if 0 <= k + 1 < nU:
                                    st0(k + 1)
                                if 0 <= k < nU:
                                    st1a(k)
                                if 0 <= k - 1 < nU:
                                    st2a(k - 1)
                                if 0 <= k < nU:
                                    st1b(k)
                                if 0 <= k - 1 < nU:
                                    st2b(k - 1)
                                if 0 <= k - 2 < nU:
                                    st3(k - 2)
                            return it
                        return [mk(k) for k in range(-1, nU + 2)]

                    run_passes(sb_proj_chunks, sb_loop_iters, 4)
                    S.barrier()

                with ExitStack() as sm:
                    qmT2 = [sb(sm, f"qmT{i}", [128, 2, 2048], BF16) for i in range(2)]
                    kmT2 = [sb(sm, f"kmT{i}", [128, 2, 2048], BF16) for i in range(2)]
                    vm2 = [sb(sm, f"vm{i}", [128, 16, 2, 65], BF16) for i in range(2)]
                    Et = [sb(sm, f"Et_{i}", [128, 512], BF16) for i in range(3)]
                    dr = sb(sm, "dr", [128, 512])
                    rb = sb(sm, "rb", [128, 512])
                    ta = sb(sm, "ta", [128, 512])
                    tb = sb(sm, "tb", [128, 512])
                    P = slice(64, 96)
                    scale = float(96 ** -0.5)

                    def mla_proj_chunks(m):
                        pm = m % 2
                        qmT, kmT, vm = qmT2[pm], kmT2[pm], vm2[pm]
                        chunks = []

                        def c_init():
                            S.op("dve", lambda e: e.memset(vm[:], 1.0), writes=[("vm", pm, G) for G in range(4)])
                            S.op("dve", lambda e: e.memset(qmT[96:128], 0.0), writes=[("qmT", pm, G) for G in range(4)])
                            S.op("dve", lambda e: e.memset(kmT[96:128], 0.0), writes=[("kmT", pm, G) for G in range(4)])
                        chunks.append(c_init)
                        for G in range(4):
                            gs = slice(G * 512, (G + 1) * 512)
                            for hl in range(2):
                                def c_qk(G=G, gs=gs, hl=hl):
                                    h = 2 * m + hl
                                    for (wt, wk, bn) in ((w_uq_sb, "w_uq", 6), (w2_sb, "w2", 7)):
                                        for c in range(2):
                                            S.op("pe", lambda e: e.matmul(bank(bn)[0:96, :], wt[:, c, h, :], cqTn[:, c, gs], start=(c == 0), stop=(c == 1)),
                                                 reads=[wk, "cqTn"], writes=[bk(bn)])
                                    S.op("dve", lambda e: e.tensor_copy(qmT[0:64, hl, gs], bank(6)[0:64, :]), reads=[bk(6)], writes=[("qmT", pm, G)])
                                    S.op("dve", lambda e: e.tensor_tensor(ta[P, :], bank(6)[P, :], cosT[P, gs], ALU.mult),
                                         reads=[bk(6), ("cosT", G)], writes=["ta"])
                                    S.op("dve", lambda e: e.tensor_tensor(tb[P, :], bank(7)[P, :], sinT[P, gs], ALU.mult),
                                         reads=[bk(7), ("sinT", G)], writes=["tb"])
                                    S.op("dve", lambda e: e.tensor_tensor(qmT[P, hl, gs], ta[P, :], tb[P, :], ALU.add),
                                         reads=["ta", "tb"], writes=[("qmT", pm, G)])
                                    S.op("pe", lambda e: e.matmul(bank(6)[0:64, :], w_ukv_sb[:, h * 128:h * 128 + 64], ckvTn[:, gs], start=True, stop=True),
                                         reads=["w_ukv", "ckvTn"], writes=[bk(6)])
                                    S.op("dve", lambda e: e.tensor_copy(kmT[0:64, hl, gs], bank(6)[0:64, :]), reads=[bk(6)], writes=[("kmT", pm, G)])
                                    S.op("dve", lambda e: e.tensor_copy(kmT[P, hl, gs], krope[P, gs]), reads=["krope"], writes=[("kmT", pm, G)])
                                chunks.append(c_qk)

                            def c_v(G=G):
                                bn = 7
                                for tl in range(4):
                                    t = G * 4 + tl
                                    S.op("pe", lambda e: e.matmul(bank(bn)[:, tl * 128:(tl + 1) * 128], ckvTn[:, t * 128:(t + 1) * 128],
                                                                  wv_sb[:, 2 * m:2 * m + 2, :], start=True, stop=True),
                                         reads=["wv", "ckvTn"], writes=[bk(bn)])
                                S.op("dve", lambda e: e.tensor_copy(vm[:, G * 4:(G + 1) * 4, :, 0:64],
                                                                    bank(bn).rearrange("p (t h d) -> p t h d", t=4, h=2)),
                                     reads=[bk(bn)], writes=[("vm", pm, G)])
                            chunks.append(c_v)
                        return chunks

                    def mla_loop_iters(m):
                        pm = m % 2
                        qmT, kmT, vm = qmT2[pm], kmT2[pm], vm2[pm]
                        units = [(hl, G, i) for G in range(4) for i in range(0, 4 * G + 4) for hl in range(2)]
                        nU = len(units)

                        def geom(u):
                            hl, G, i = units[u]
                            q0 = max(i, 4 * G) * 128
                            off = q0 - G * 512
                            return dict(hl=hl, G=G, i=i, off=off, cs=slice(off, 512), qsl=slice(q0, (G + 1) * 512),
                                        ksl=slice(i * 128, (i + 1) * 128), diag=(i >= 4 * G), pr=slice(hl * 64, (hl + 1) * 64),
                                        first=(i == 0), last=(i == 4 * G + 3), gs=slice(G * 512, (G + 1) * 512))

                        def st0(u):
                            g = geom(u)
                            zb = u % 3
                            S.op("pe", lambda e: e.matmul(bank(zb)[:, g["cs"]], kmT[:, g["hl"], g["ksl"]], qmT[:, g["hl"], g["qsl"]],
                                                          start=True, stop=True),
                                 reads=[("kmT", pm, g["i"] // 4), ("qmT", pm, g["G"])], writes=[bk(zb)])

                        def st1(u):
                            g = geom(u)
                            zb, cs, off = u % 3, g["cs"], g["off"]
                            et, etk = Et[u % 3], f"Et_{u % 3}"
                            S.op("act", lambda e: e.activation(et[:, cs], bank(zb)[:, cs], AF.Exp, scale=scale), reads=[bk(zb)], writes=[etk])
                            if g["diag"]:
                                S.op("dve", lambda e: e.tensor_tensor(et[:, off:off + 128], et[:, off:off + 128], mcausal, ALU.mult),
                                     reads=[etk, "cst"], writes=[etk])

                        def st2(u):
                            g = geom(u)
                            cs = g["cs"]
                            ob = 4 + g["hl"]
                            et, etk = Et[u % 3], f"Et_{u % 3}"
                            S.op("pe", lambda e: e.matmul(bank(ob)[0:65, cs], vm[:, g["i"], g["hl"], :], et[:, cs], start=g["first"], stop=g["last"],
                                                          skip_group_check=True),
                                 reads=[("vm", pm, g["i"] // 4), etk], writes=[bk(ob)])
                            if g["last"]:
                                S.op("act", lambda e: e.activation(dr[64:65, :], bank(ob)[64:65, :], AF.Ln), reads=[bk(ob)], writes=["dr"])

                                def fin(ob=ob, pr=g["pr"], gs=g["gs"]):
                                    S.op("act", lambda e: e.activation(dr[64:65, :], dr[64:65, :], AF.Exp, scale=-1.0), reads=["dr"], writes=["dr"])
                                    S.op("pe", lambda e: e.matmul(bank(3)[0:64, :], ones_f[64:65, 0:64], dr[64:65, :], start=True, stop=True),
                                         reads=["ones_f", "dr"], writes=[bk(3)])
                                    S.op("act", lambda e: e.copy(rb[0:64, :], bank(3)[0:64, :]), reads=[bk(3)], writes=["rb"])
                                    S.op("dve", lambda e: e.tensor_tensor(oT[pr, 4 + m, gs], bank(ob)[0:64, :], rb[0:64, :], ALU.mult),
                                         reads=[bk(ob), "rb"], writes=[("oT", 4 + m)])
                                deferred.append(fin)

                        deferred = []

                        def mk(k):
                            def it():
                                if 0 <= k + 1 < nU:
                                    st0(k + 1)
                                if 0 <= k < nU:
                                    st1(k)
                                pend = list(deferred)
                                del deferred[:]
                                for f in pend:
                                    f()
                                if 0 <= k - 1 < nU:
                                    st2(k - 1)
                                if k >= nU:
                                    for f in list(deferred):
                                        f()
                                    del deferred[:]
                            return it
                        return [mk(k) for k in range(-1, nU + 1)]

                    run_passes(mla_proj_chunks, mla_loop_iters, 4)
                    S.barrier()

                if debug and b == 0:
                    S.dma("sp", lambda e: e.dma_start(out=dbg_oT[:, :, :], in_=oT[:]), reads=[("oT", jj) for jj in range(8)], writes=["dbg_oT"])
                    S.dma("sp", lambda e: e.dma_start(out=dbg_lat[:, 0:2, :], in_=cqTn[:]), reads=["cqTn"], writes=["dbg_lat0"])
                    S.dma("sp", lambda e: e.dma_start(out=dbg_lat[:, 2, :], in_=ckvTn[:]), reads=["ckvTn"], writes=["dbg_lat1"])
                    S.dma("sp", lambda e: e.dma_start(out=dbg_lat[64:96, 3, :], in_=krope[64:96, :]), reads=["krope"], writes=["dbg_lat2"])
                    S.barrier()
                with ExitStack() as sd:
                    xts = [sb(sd, f"xd{i}", [128, 1024]) for i in range(2)]
                    x1t = [sb(sd, f"x1t{i}", [128, 1024]) for i in range(2)]
                    h2b = [sb(sd, f"h2b{i}", [128, 1024], BF16) for i in range(2)]
                    junk = sb(sd, "junkd", [128, 1024], BF16)
                    osq2 = [sb(sd, f"osq{i}", [128, 8, 128], BF16) for i in range(2)]
                    rsDall = sb(sd, "rsDall", [128, 32])
                    ssD = sb(sd, "ssD", [128, 2])
                    rsD = sb(sd, "rsD", [128, 2])
                    ss2 = sb(sd, "ss2", [128, 1])
                    rs2 = sb(sd, "rs2", [128, 1])
                    lgall = sb(sd, "lgall", [128, 16, 36])
                    lgraw = sb(sd, "lgraw", [128, 16, 36])
                    ss2all = sb(sd, "ss2all", [128, 16])
                    gmax = sb(sd, "gmax", [128, 16, 1])
                    ohg = sb(sd, "ohg", [128, 16, 4])
                    gsh = sb(sd, "gsh", [128, 16, 4])
                    gex = sb(sd, "gex", [128, 16, 4])
                    gsum = sb(sd, "gsum", [128, 16, 1])
                    gval = sb(sd, "gval", [128, 16, 1])
                    tmp4 = sb(sd, "tmp4", [128, 16, 4, 8])
                    loc = sb(sd, "loc", [128, 16, 8])
                    loc2 = sb(sd, "loc2", [128, 16, 8])
                    l1 = sb(sd, "l1", [128, 16, 1])
                    l2 = sb(sd, "l2", [128, 16, 1])
                    m1 = sb(sd, "m1", [128, 16, 8])
                    m2 = sb(sd, "m2", [128, 16, 8])
                    dd = sb(sd, "dd", [128, 16, 1])
                    s1 = sb(sd, "s1", [128, 16, 1])
                    w1 = sb(sd, "w1", [128, 16, 1])
                    w2 = sb(sd, "w2", [128, 16, 1])
                    wl = sb(sd, "wl", [128, 16, 8])
                    wl2 = sb(sd, "wl2", [128, 16, 8])
                    S.dma("sp", lambda e: e.dma_start(out=gbc[:], in_=ffn_norm[0:1, :].partition_broadcast(128)), writes=["gbc"])
                    def d_partA(t):
                        ts_ = slice(t * 128, (t + 1) * 128)
                        xt, xk = xts[t % 2], f"xd{t % 2}"
                        x1, x1k = x1t[t % 2], f"x1t{t % 2}"
                        hb, hk = h2b[t % 2], f"h2b{t % 2}"
                        S.dma("sp", lambda e: e.dma_start(out=xt[:], in_=x[b, ts_, :]), writes=[xk])
                        for grp in range(2):
                            for hh in range(2):
                                bn = grp * 2 + hh
                                for jl in range(4):
                                    jj = grp * 4 + jl
                                    S.op("pe", lambda e: e.matmul(bank(bn), oT[:, jj, ts_], w_out_sb[:, jj, hh * 512:(hh + 1) * 512],
                                                                  start=(jl == 0), stop=(jl == 3)),
                                         reads=[("oT", jj), "w_out"], writes=[bk(bn)])
                        S.op("dve", lambda e: e.scalar_tensor_tensor(x1[:], PS[0][:], rsDall[:, 2 * t:2 * t + 1], xt[:], ALU.mult, ALU.add),
                             reads=[bk(0), bk(1), "rsDall", xk], writes=[x1k])
                        S.op("dve", lambda e: e.scalar_tensor_tensor(x1[:], PS[1][:], rsDall[:, 2 * t + 1:2 * t + 2], x1[:], ALU.mult, ALU.add),
                             reads=[bk(2), bk(3), "rsDall", x1k], writes=[x1k])
                        S.dma("sp", lambda e: e.dma_start(out=x1s[b, ts_, :], in_=x1[:]), reads=[x1k], writes=[("x1s", b, t)])
                        S.op("act", lambda e: e.activation(junk[:], x1[:], AF.Square, accum_out=ss2all[:, t:t + 1]),
                             reads=[x1k, "ss2init"], writes=["junkd", ("ss2all", t)])
                        S.op("dve", lambda e: e.tensor_tensor(hb[:], x1[:], gbc[:], ALU.mult), reads=[x1k, "gbc"], writes=[hk])

                    def d_partB(t):
                        ts_ = slice(t * 128, (t + 1) * 128)
                        x1, x1k = x1t[t % 2], f"x1t{t % 2}"
                        hb, hk = h2b[t % 2], f"h2b{t % 2}"
                        transpose_to_hT(hb, hk, t, 5)
                        for c in range(8):
                            S.op("pe", lambda e: e.matmul(bank(6)[:, 0:36], hT[:, c, ts_], wr_sb[:, c, :], start=(c == 0), stop=(c == 7)),
                                 reads=[("hT", t // 4), ("hTt", t), "wr"], writes=[bk(6)])
                        S.op("dve", lambda e: e.tensor_copy(lgraw[:, t, :], bank(6)[:, 0:36]), reads=[bk(6)], writes=["lgraw"])

                    S.op("dve", lambda e: e.memset(ss2all[:], 0.0), writes=["ss2init"] + [("ss2all", t) for t in range(16)])
                    for t in range(16):
                        ts_ = slice(t * 128, (t + 1) * 128)
                        oq, oqk = osq2[t % 2], f"osq{t % 2}"
                        S.op("dve", lambda e: e.tensor_tensor(oq[:], oT[:, :, ts_], oT[:, :, ts_], ALU.mult),
                             reads=[("oT", jj) for jj in range(8)], writes=[oqk])
                        for grp in range(2):
                            for jl in range(4):
                                S.op("pe", lambda e: e.matmul(bank(4)[:, 2 * t + grp:2 * t + grp + 1], oq[:, grp * 4 + jl, :], ones_bf[:, 0:1],
                                                              start=(t == 0 and jl == 0 and grp == 0), stop=(t == 15 and grp == 1 and jl == 3),
                                                              skip_group_check=True),
                                     reads=[oqk, "ones_bf"], writes=[bk(4)])
                    rstd_from_ss(rsDall[:], bank(4)[:, 0:32], 512.0, "rsDall", bk(4))
                    for t in range(17):
                        if t < 16:
                            d_partA(t)
                        if t >= 1:
                            d_partB(t - 1)
                    S.op("act", lambda e: e.activation(rs2all[:], ss2all[:], AF.Sqrt, bias=epsc[:], scale=1.0 / 1024.0),
                         reads=[("ss2all", t) for t in range(16)] + ["epsc"], writes=["rs2all"])
                    S.op("dve", lambda e: e.reciprocal(rs2all[:], rs2all[:]), reads=["rs2all"], writes=["rs2all"])
                    rs2b = rs2all[:].rearrange("p (t o) -> p t o", o=1)
                    S.op("dve", lambda e: e.tensor_tensor(lgall[:], lgraw[:], rs2b.to_broadcast([128, 16, 36]), ALU.mult),
                         reads=["lgraw", "rs2all"], writes=["lgall"])
                    S.op("dve", lambda e: e.tensor_tensor(lgall[:], lgall[:], br_bc[:].rearrange("p (o n) -> p o n", o=1).to_broadcast([128, 16, 36]), ALU.add),
                         reads=["lgall", "br"], writes=["lgall"])
                    T4 = [128, 16, 4]
                    T8 = [128, 16, 8]
                    T48 = [128, 16, 4, 8]
                    glg = lgall[:, :, 0:4]
                    elg = lgall[:, :, 4:36].rearrange("p t (g k) -> p t g k", g=4)
                    ohg4 = ohg[:].rearrange("p t (g o) -> p t g o", o=1)
                    V = lambda fn, r, w: S.op("dve", fn, reads=r, writes=w)
                    V(lambda e: e.reduce_max(gmax[:], glg, axis=AX.X), ["lgall"], ["gmax"])
                    V(lambda e: e.tensor_tensor(ohg[:], glg, gmax[:].to_broadcast(T4), ALU.is_equal), ["lgall", "gmax"], ["ohg"])
                    V(lambda e: e.tensor_tensor(gsh[:], glg, gmax[:].to_broadcast(T4), ALU.subtract), ["lgall", "gmax"], ["gsh"])
                    S.op("act", lambda e: e.activation(gex[:], gsh[:], AF.Exp), reads=["gsh"], writes=["gex"])
                    V(lambda e: e.reduce_sum(gsum[:], gex[:], axis=AX.X), ["gex"], ["gsum"])
                    V(lambda e: e.reciprocal(gval[:], gsum[:]), ["gsum"], ["gval"])
                    V(lambda e: e.tensor_tensor(tmp4[:], elg, ohg4.to_broadcast(T48), ALU.mult), ["lgall", "ohg"], ["tmp4"])
                    V(lambda e: e.reduce_sum(loc[:].rearrange("p t (k o) -> p t k o", o=1), tmp4[:].rearrange("p t g k -> p t k g"), axis=AX.X),
                      ["tmp4"], ["loc"])
                    V(lambda e: e.reduce_max(l1[:], loc[:], axis=AX.X), ["loc"], ["l1"])
                    V(lambda e: e.tensor_tensor(m1[:], loc[:], l1[:].to_broadcast(T8), ALU.is_equal), ["loc", "l1"], ["m1"])
                    V(lambda e: e.scalar_tensor_tensor(loc2[:], m1[:], -1e30, loc[:], ALU.mult, ALU.add), ["m1", "loc"], ["loc2"])
                    V(lambda e: e.reduce_max(l2[:], loc2[:], axis=AX.X), ["loc2"], ["l2"])
                    V(lambda e: e.tensor_tensor(m2[:], loc2[:], l2[:].to_broadcast(T8), ALU.is_equal), ["loc2", "l2"], ["m2"])
                    V(lambda e: e.tensor_tensor(dd[:], l2[:], l1[:], ALU.subtract), ["l1", "l2"], ["dd"])
                    S.op("act", lambda e: e.activation(s1[:], dd[:], AF.Exp), reads=["dd"], writes=["s1"])
                    V(lambda e: e.tensor_scalar(s1[:], s1[:], 1.0, None, ALU.add), ["s1"], ["s1"])
                    V(lambda e: e.reciprocal(s1[:], s1[:]), ["s1"], ["s1"])
                    V(lambda e: e.tensor_tensor(w1[:], gval[:], s1[:], ALU.mult), ["gval", "s1"], ["w1"])
                    V(lambda e: e.tensor_tensor(w2[:], gval[:], w1[:], ALU.subtract), ["gval", "w1"], ["w2"])
                    V(lambda e: e.tensor_tensor(wl[:], m1[:], w1[:].to_broadcast(T8), ALU.mult), ["m1", "w1"], ["wl"])
                    V(lambda e: e.tensor_tensor(wl2[:], m2[:], w2[:].to_broadcast(T8), ALU.mult), ["m2", "w2"], ["wl2"])
                    V(lambda e: e.tensor_tensor(wl[:], wl[:], wl2[:], ALU.add), ["wl", "wl2"], ["wl"])
                    V(lambda e: e.tensor_tensor(Wfull[:].rearrange("p t (g k) -> p t g k", g=4), ohg4.to_broadcast(T48),
                                                wl[:].rearrange("p t (o k) -> p t o k", o=1).to_broadcast(T48), ALU.mult),
                      ["ohg", "wl"], [("Wfull", t) for t in range(16)])
                    V(lambda e: e.tensor_tensor(Wfull[:], Wfull[:], rs2b.to_broadcast([128, 16, 32]), ALU.mult),
                      [("Wfull", t) for t in range(16)] + ["rs2all"], [("Wfull", t) for t in range(16)])
                    S.barrier()

            if "moe" in phases:
              with ExitStack() as s2:
                acc = sb(s2, "acc", [128, 16, 1024])
                NW = 3
                wgu = [sb(s2, f"wgu{i}", [128, 8, 512], BF16) for i in range(NW)]
                wd = [sb(s2, f"wd{i}", [128, 2, 1024], BF16) for i in range(NW)]
                sg = [sb(s2, f"sg{i}", [128, 256]) for i in range(2)]
                hid = [sb(s2, f"hid{i}", [128, 256], BF16) for i in range(2)]
                hidT = [sb(s2, f"hidT{i}", [128, 256], BF16) for i in range(2)]
                junk = sb(s2, "junke", [128, 1024], BF16)
                ss3 = sb(s2, "ss3", [128, 2])
                rs3 = sb(s2, "rs3", [128, 2])
                yt = [sb(s2, f"yt{i}", [128, 1024]) for i in range(2)]
                S.dma("sp", lambda e: e.dma_start(out=gbc[:], in_=final_norm[0:1, :].partition_broadcast(128)), writes=["gbc"])
                for t in range(16):
                    S.dma("sp", lambda e: e.dma_start(out=acc[:, t, :], in_=x1s[b, t * 128:(t + 1) * 128, :]),
                          reads=[("x1s", b, t)], writes=[("acc", t)])

                def load_expert(ex):
                    sl = ex % NW
                    S.dma("pool", lambda e: e.dma_start(out=wgu[sl][:, :, 0:256], in_=w_gate[ex].rearrange("(c p) n -> p c n", p=128)),
                          writes=[f"wgu{sl}"])
                    S.dma("pool", lambda e: e.dma_start(out=wgu[sl][:, :, 256:512], in_=w_up[ex].rearrange("(c p) n -> p c n", p=128)),
                          writes=[f"wgu{sl}"])
                    S.dma("pool", lambda e: e.dma_start(out=wd[sl][:], in_=w_down[ex].rearrange("(c p) n -> p c n", p=128)),
                          writes=[f"wd{sl}"])

                for ex0 in range(min(NW, n_experts)):
                    load_expert(ex0)
                steps = [(ex, t) for ex in range(n_experts) for t in range(16)]
                nS = len(steps)

                def m_gu(k):
                    ex, t = steps[k]
                    sl, p2, ts_ = ex % NW, k % 2, slice(t * 128, (t + 1) * 128)
                    gb = p2
                    for c in range(8):
                        S.op("pe", lambda e: e.matmul(bank(gb), hT[:, c, ts_], wgu[sl][:, c, :], start=(c == 0), stop=(c == 7)),
                             reads=[("hT", t // 4), ("hTt", t), f"wgu{sl}"], writes=[bk(gb)])
                    S.op("act", lambda e: e.activation(sg[p2][:], bank(gb)[:, 0:256], AF.Silu, scale=rs2all[:, t:t + 1]), reads=[bk(gb), "rs2all"], writes=[f"sg{p2}"])
                    S.op("dve", lambda e: e.scalar_tensor_tensor(hid[p2][:], bank(gb)[:, 256:512], Wfull[:, t, ex:ex + 1], sg[p2][:],
                                                                 ALU.mult, ALU.mult),
                         reads=[bk(gb), ("Wfull", t), f"sg{p2}"], writes=[f"hid{p2}"])

                def m_tr(k):
                    p2 = k % 2
                    tbk = 2 + p2
                    tb_ = bank_bf(tbk)
                    for c in range(2):
                        S.op("pe", lambda e: e.transpose(tb_[:, c * 128:(c + 1) * 128], hid[p2][:, c * 128:(c + 1) * 128], ident),
                             reads=[f"hid{p2}", "cst"], writes=[bk(tbk)])
                    S.op("act", lambda e: e.copy(hidT[p2][:], tb_[:, 0:256]), reads=[bk(tbk)], writes=[f"hidT{p2}"])

                def m_dn(k):
                    ex, t = steps[k]
                    sl, p2 = ex % NW, k % 2
                    dps = 2 + p2
                    for hh in range(2):
                        for c in range(2):
                            S.op("pe", lambda e: e.matmul(PS[dps][:, hh * 512:(hh + 1) * 512], hidT[p2][:, c * 128:(c + 1) * 128],
                                                          wd[sl][:, c, hh * 512:(hh + 1) * 512], start=(c == 0), stop=(c == 1)),
                                 reads=[f"hidT{p2}", f"wd{sl}"], writes=[bk(2 * dps + hh)])
                    S.op("dve", lambda e: e.tensor_tensor(acc[:, t, :], acc[:, t, :], PS[dps][:], ALU.add),
                         reads=[("acc", t), bk(2 * dps), bk(2 * dps + 1)], writes=[("acc", t)])
                    if t == 15 and ex + NW < n_experts:
                        load_expert(ex + NW)

                for k in range(nS + 2):
                    if k < nS:
                        m_gu(k)
                    if 0 <= k - 1 < nS:
                        m_tr(k - 1)
                    if 0 <= k - 2 < nS:
                        m_dn(k - 2)
                for t in range(16):
                    sl = t % 2
                    S.op("dve", lambda e: e.memset(ss3[:, sl:sl + 1], 0.0), writes=[f"ss3{sl}"])
                    S.op("act", lambda e: e.activation(junk[:], acc[:, t, :], AF.Square, accum_out=ss3[:, sl:sl + 1]),
                         reads=[("acc", t)], writes=["junke", f"ss3{sl}"])
                    rstd_from_ss(rs3[:, sl:sl + 1], ss3[:, sl:sl + 1], 1024.0, f"rs3{sl}", f"ss3{sl}")
                    S.op("dve", lambda e: e.scalar_tensor_tensor(yt[sl][:], acc[:, t, :], rs3[:, sl:sl + 1], gbc[:], ALU.mult, ALU.mult),
                         reads=[("acc", t), f"rs3{sl}", "gbc"], writes=[f"yt{sl}"])
                    S.dma("sp", lambda e: e.dma_start(out=y[b, t * 128:(t + 1) * 128, :], in_=yt[sl][:]), reads=[f"yt{sl}"], writes=[("y", b, t)])
                S.barrier()
        S.final_wait("sp")
        print("instructions emitted:", S.nins, {e: S.cnt[e] for e in ENGS}, S.ndma)
    return nc


def _consts():
    k = np.arange(128)[:, None]
    q = np.arange(128)[None, :]
    ident = (k == q)
    tri = (k >= q)
    strict = (k < q)
    causal = (k <= q)
    c = np.concatenate([ident, tri, strict, causal, strict], axis=1).astype(np.float32)
    half = 16
    inv_freq = (np.float32(10000.0) ** (-np.arange(half, dtype=np.float32) / np.float32(half))).astype(np.float32)
    invf = np.zeros((128, 1), np.float32)
    for p in range(64, 96):
        invf[p, 0] = inv_freq[(p - 64) % 16]
    return c.astype(ml_dtypes.bfloat16), invf


def make_in_maps(inputs, n_cores, nseq):
    f = lambda a: np.ascontiguousarray(np.asarray(a))
    cst, invf = _consts()
    shared = {
        "attn_norm": f(inputs["attn_norm"]).reshape(1, 1024),
        "w_in": f(inputs["w_in"]).reshape(1024, 1952),
        "q_norm": f(inputs["q_norm"]).reshape(256, 1),
        "w_uq": f(inputs["w_uq"]).reshape(256, 768),
        "kv_norm": f(inputs["kv_norm"]).reshape(128, 1),
        "w_ukv": f(inputs["w_ukv"]).reshape(128, 1024),
        "out_norm": np.concatenate([f(inputs["sb_out_norm"]).reshape(-1), f(inputs["mla_out_norm"]).reshape(-1)]).reshape(1024, 1),
        "w_out": f(inputs["w_out"]).reshape(1024, 1024),
        "ffn_norm": f(inputs["ffn_norm"]).reshape(1, 1024),
        "w_group_router": f(inputs["w_group_router"]).reshape(1024, 4),
        "b_group_router": f(inputs["b_group_router"]).reshape(1, 4),
        "w_expert_router": f(inputs["w_expert_router"]).reshape(1024, 32),
        "b_expert_router": f(inputs["b_expert_router"]).reshape(1, 32),
        "w_gate": f(inputs["w_gate"]).reshape(32, 1024, 256),
        "w_up": f(inputs["w_up"]).reshape(32, 1024, 256),
        "w_down": f(inputs["w_down"]).reshape(32, 256, 1024),
        "final_norm": f(inputs["final_norm"]).reshape(1, 1024),
        "consts": cst,
        "invf": invf,
    }
    xs = f(inputs["x"])
    ps = f(inputs["positions"]).astype(np.int32)
    maps = []
    for c in range(n_cores):
        m = dict(shared)
        m["x"] = xs[c * nseq:(c + 1) * nseq]
        m["positions"] = ps[c * nseq:(c + 1) * nseq]
        maps.append(m)
    return maps


def kernel(**inputs):
    n_cores = 8
    nseq = 4
    nc = build(NSEQ=nseq)
    maps = make_in_maps(inputs, n_cores, nseq)
    res = run_bass_kernel_spmd(nc, maps, core_ids=list(range(n_cores)))
    out = np.concatenate([np.asarray(r["y"]) for r in res.results], axis=0)
    return out.astype(np.float32)
```

```python
import numpy as np
import ml_dtypes
from contextlib import ExitStack
import concourse.bass as bass
import concourse.mybir as mybir
from concourse.bass_utils import run_bass_kernel_spmd

F32 = mybir.dt.float32
BF16 = mybir.dt.bfloat16
I32 = mybir.dt.int32
AF = mybir.ActivationFunctionType
ALU = mybir.AluOpType
AX = mybir.AxisListType

ENGS = ["pe", "act", "dve", "pool", "sp"]
DMA_ENGS = ["sp", "pool"]
ND = 8
EPS = 1e-6
TWO_PI = float(2 * np.pi)


class _Proxy:
    def __init__(self):
        self.call = None

    def __getattr__(self, name):
        def f(*a, **k):
            self.call = (name, a, k)
            return self
        return f


class Sched:
    def __init__(self, nc, es):
        self.nc = nc
        self.eng = {"pe": nc.tensor, "act": nc.scalar, "dve": nc.vector, "pool": nc.gpsimd, "sp": nc.sync}
        self.sem = {e: es.enter_context(nc.semaphore("c_" + e)) for e in ENGS}
        self.dsem = {e: [es.enter_context(nc.semaphore(f"d_{e}_{i}")) for i in range(ND)] for e in DMA_ENGS}
        self.cnt = {e: 0 for e in ENGS}
        self.ndma = {e: 0 for e in DMA_ENGS}
        self.dlast = {e: {} for e in DMA_ENGS}
        self.lastw = {}
        self.readers = {}
        self.seen = {e: {} for e in ENGS}
        self.nins = 0
        self.rec = None

    def record(self, chunk_fns):
        self.rec = []
        for f in chunk_fns:
            f()
        out, self.rec = self.rec, None
        return out

    def _semobj(self, key):
        return self.sem[key[1]] if key[0] == "c" else self.dsem[key[1]][key[2]]

    def _collect(self, reads, writes):
        toks = {}

        def add(k, v):
            if toks.get(k, 0) < v:
                toks[k] = v
        for r in reads:
            t = self.lastw.get(r)
            if t is not None:
                add(*t)
        for w in writes:
            t = self.lastw.get(w)
            if t is not None:
                add(*t)
            for k, v in self.readers.get(w, {}).items():
                add(k, v)
        return toks

    def _wait(self, eng, toks):
        seen = self.seen[eng]
        for k, v in toks.items():
            if k[0] == "c" and k[1] == eng and eng == "pe":
                continue
            if seen.get(k, 0) >= v:
                continue
            self.eng[eng].wait_ge(self._semobj(k), v)
            seen[k] = v
            self.nins += 1

    def _record(self, tok, reads, writes):
        for w in writes:
            self.lastw[w] = tok
            self.readers[w] = {}
        for r in reads:
            d = self.readers.setdefault(r, {})
            if d.get(tok[0], 0) < tok[1]:
                d[tok[0]] = tok[1]

    def op(self, eng, fn, reads=(), writes=()):
        if self.rec is not None:
            p = _Proxy()
            fn(p)
            name, a, k = p.call
            self.rec.append(lambda: self.op(eng, lambda e: getattr(e, name)(*a, **k), reads, writes))
            return
        toks = self._collect(reads, writes)
        self._wait(eng, toks)
        ins = fn(self.eng[eng])
        self.cnt[eng] += 1
        ins.then_inc(self.sem[eng], 1)
        self.nins += 1
        tok = (("c", eng), self.cnt[eng])
        self._record(tok, reads, writes)

    def dma(self, eng, fn, reads=(), writes=()):
        if self.rec is not None:
            p = _Proxy()
            fn(p)
            name, a, k = p.call
            self.rec.append(lambda: self.dma(eng, lambda e: getattr(e, name)(*a, **k), reads, writes))
            return
        n = self.ndma[eng]
        self.ndma[eng] += 1
        slot = n % ND
        val = 16 * (n // ND + 1)
        toks = self._collect(reads, writes)
        key = ("d", eng, slot)
        if val > 16:
            toks[key] = max(toks.get(key, 0), val - 16)
        self._wait(eng, toks)
        ins = fn(self.eng[eng])
        ins.then_inc(self.dsem[eng][slot], 16)
        self.nins += 1
        self.dlast[eng][slot] = val
        self._record((key, val), reads, writes)

    def barrier(self):
        toks = {}
        for e in ENGS:
            if self.cnt[e] > 0:
                toks[("c", e)] = self.cnt[e]
        for q in DMA_ENGS:
            for slot, val in self.dlast[q].items():
                toks[("d", q, slot)] = val
        for e in ENGS:
            t = {k: v for k, v in toks.items() if not (k[0] == "c" and k[1] == e)}
            self._wait(e, t)

    def final_wait(self, eng="sp"):
        toks = {}
        for q in DMA_ENGS:
            for slot, val in self.dlast[q].items():
                toks[("d", q, slot)] = val
        for e in ENGS:
            if self.cnt[e] > 0 and e != eng:
                toks[("c", e)] = self.cnt[e]
        self._wait(eng, toks)


def build(NSEQ=4, debug=False, n_experts=32, phases=("attn", "moe"), nd_sb=0, nd_mla=0):
    nc = bass.Bass("TRN2", target_bir_lowering=False)

    def din(name, shape, dtype=F32):
        return nc.dram_tensor(name, shape, dtype, kind="ExternalInput").ap()

    x = din("x", [NSEQ, 2048, 1024])
    positions = din("positions", [NSEQ, 2048], I32)
    attn_norm = din("attn_norm", [1, 1024])
    w_in = din("w_in", [1024, 1952])
    q_norm = din("q_norm", [256, 1])
    w_uq = din("w_uq", [256, 768])
    kv_norm = din("kv_norm", [128, 1])
    w_ukv = din("w_ukv", [128, 1024])
    out_norm = din("out_norm", [1024, 1])
    w_out = din("w_out", [1024, 1024])
    ffn_norm = din("ffn_norm", [1, 1024])
    w_gr = din("w_group_router", [1024, 4])
    b_gr = din("b_group_router", [1, 4])
    w_er = din("w_expert_router", [1024, 32])
    b_er = din("b_expert_router", [1, 32])
    w_gate = din("w_gate", [32, 1024, 256])
    w_up = din("w_up", [32, 1024, 256])
    w_down = din("w_down", [32, 256, 1024])
    final_norm = din("final_norm", [1, 1024])
    consts = din("consts", [128, 640], BF16)
    invf = din("invf", [128, 1])
    y = nc.dram_tensor("y", [NSEQ, 2048, 1024], F32, kind="ExternalOutput").ap()
    x1s = nc.dram_tensor("x1s", [NSEQ, 2048, 1024], F32,
                         kind="ExternalOutput" if debug else "Internal").ap()

    dbg_oT = nc.dram_tensor("dbg_oT", [128, 8, 2048], BF16, kind="ExternalOutput").ap() if debug else None
    dbg_hT = nc.dram_tensor("dbg_hT", [128, 8, 2048], BF16, kind="ExternalOutput").ap() if debug else None
    dbg_lat = nc.dram_tensor("dbg_lat", [128, 4, 2048], BF16, kind="ExternalOutput").ap() if debug else None

    with ExitStack() as es:
        S = Sched(nc, es)

        uid = [0]

        def sb(stack, name, shape, dtype=F32):
            uid[0] += 1
            return stack.enter_context(nc.sbuf_tensor(f"{name}_{uid[0]}", shape, dtype))

        PS = [es.enter_context(nc.psum_tensor(f"ps{i}", [128, 1024], F32)) for i in range(4)]
        PSB = [p[:].bitcast(BF16) for p in PS]

        def bank(k):
            return PS[k // 2][:, (k % 2) * 512:(k % 2) * 512 + 512]

        def bank_bf(k):
            return PSB[k // 2][:, (k % 2) * 1024:(k % 2) * 1024 + 1024]

        def bk(k):
            return f"B{k}"

        cst = sb(es, "cst", [128, 640], BF16)
        ident = cst[:, 0:128]
        tri = cst[:, 128:256]
        mstrict = cst[:, 256:384]
        mcausal = cst[:, 384:512]
        slow = cst[:, 512:640]
        ones_bf = sb(es, "ones_bf", [128, 128], BF16)
        ones_f = sb(es, "ones_f", [128, 64], F32)
        epsc = sb(es, "epsc", [128, 1])
        onec = sb(es, "onec", [128, 1])
        invf_sb = sb(es, "invf_sb", [128, 1])
        gbc = sb(es, "gbc", [128, 1024])
        w_uq_sb = sb(es, "w_uq_sb", [128, 2, 8, 96], BF16)
        w2_sb = sb(es, "w2_sb", [128, 2, 8, 96], BF16)
        w_ukv_sb = sb(es, "w_ukv_sb", [128, 1024], BF16)
        wv_sb = sb(es, "wv_sb", [128, 8, 64], BF16)
        wkr = sb(es, "wkr", [128, 8, 96], BF16)
        wkr2 = sb(es, "wkr2", [128, 8, 96], BF16)
        w_out_sb = sb(es, "w_out_sb", [128, 8, 1024], BF16)
        wr_sb = sb(es, "wr_sb", [128, 8, 36], BF16)
        br_bc = sb(es, "br_bc", [128, 36])
        hT = sb(es, "hT", [128, 8, 2048], BF16)
        Wfull = sb(es, "Wfull", [128, 16, 32])
        rs2all = sb(es, "rs2all", [128, 16])

        S.dma("sp", lambda e: e.dma_start(out=cst[:], in_=consts[:, :]), writes=["cst"])
        S.dma("sp", lambda e: e.dma_start(out=invf_sb[:], in_=invf[:, :]), writes=["invf"])
        S.op("pool", lambda e: e.memset(ones_bf[:], 1.0), writes=["ones_bf"])
        S.op("pool", lambda e: e.memset(ones_f[:], 1.0), writes=["ones_f"])
        S.op("pool", lambda e: e.memset(epsc[:], EPS), writes=["epsc"])
        S.op("pool", lambda e: e.memset(onec[:], 1.0), writes=["onec"])
        S.op("pool", lambda e: e.memset(w2_sb[:], 0.0), writes=["w2"])
        S.op("pool", lambda e: e.memset(wkr[:], 0.0), writes=["wkr"])
        S.op("pool", lambda e: e.memset(wkr2[:], 0.0), writes=["wkr2"])
        with ExitStack() as ss:
            stg = sb(ss, "stg", [128, 2048])
            qn = sb(ss, "qn", [128, 2])
            kvn = sb(ss, "kvn", [128, 1])
            og = sb(ss, "og", [128, 8])
            S.dma("sp", lambda e: e.dma_start(out=stg[:, 0:1536].rearrange("p (c n) -> p c n", c=2),
                                              in_=w_uq.rearrange("(c p) n -> p c n", p=128)), writes=["stg"])
            for c in range(2):
                S.dma("sp", lambda e: e.dma_start(out=qn[:, c:c + 1], in_=q_norm[c * 128:(c + 1) * 128, :]), writes=["qn"])
            stv = stg[:, 0:1536].rearrange("p (c h d) -> p c h d", c=2, h=8)
            for c in range(2):
                S.op("dve", lambda e: e.tensor_scalar(w_uq_sb[:, c], stv[:, c], qn[:, c:c + 1], None, ALU.mult),
                     reads=["stg", "qn"], writes=["w_uq"])
                S.op("dve", lambda e: e.tensor_scalar(w2_sb[:, c, :, 64:80], stv[:, c, :, 80:96], qn[:, c:c + 1], -1.0,
                                                      ALU.mult, ALU.mult), reads=["stg", "qn"], writes=["w2"])
                S.op("dve", lambda e: e.tensor_scalar(w2_sb[:, c, :, 80:96], stv[:, c, :, 64:80], qn[:, c:c + 1], None,
                                                      ALU.mult), reads=["stg", "qn"], writes=["w2"])
            S.dma("sp", lambda e: e.dma_start(out=stg[:, 0:1024], in_=w_ukv[:, :]), writes=["stg"])
            S.dma("sp", lambda e: e.dma_start(out=kvn[:], in_=kv_norm[:, :]), writes=["kvn"])
            S.op("dve", lambda e: e.tensor_scalar(w_ukv_sb[:], stg[:, 0:1024], kvn[:, 0:1], None, ALU.mult),
                 reads=["stg", "kvn"], writes=["w_ukv"])
            S.op("dve", lambda e: e.tensor_scalar(wv_sb[:], stg[:, 0:1024].rearrange("p (h d) -> p h d", h=8)[:, :, 64:128],
                                                  kvn[:, 0:1], None, ALU.mult), reads=["stg", "kvn"], writes=["wv"])
            wi_c = w_in.rearrange("(c p) n -> p c n", p=128)
            S.dma("pool", lambda e: e.dma_start(out=wkr[:, :, 64:96], in_=wi_c[:, :, 1920:1952]), writes=["wkr"])
            S.dma("pool", lambda e: e.dma_start(out=wkr2[:, :, 80:96], in_=wi_c[:, :, 1920:1936]), writes=["wkr2"])
            S.dma("pool", lambda e: e.dma_start(out=wkr2[:, :, 64:80], in_=wi_c[:, :, 1936:1952]), writes=["wkr2"])
            S.op("pool", lambda e: e.tensor_scalar(wkr2[:, :, 64:80], wkr2[:, :, 64:80], -1.0, None, ALU.mult),
                 reads=["wkr2"], writes=["wkr2"])
            for j in range(8):
                S.dma("sp", lambda e: e.dma_start(out=og[:, j:j + 1], in_=out_norm[j * 128:(j + 1) * 128, :]), writes=["og"])
            wo_c = w_out.rearrange("(c p) n -> p c n", p=128)
            for jj in range(4):
                S.dma("sp", lambda e: e.dma_start(out=stg[:].rearrange("p (c n) -> p c n", c=2),
                                                  in_=wo_c[:, 2 * jj:2 * jj + 2, :]), writes=["stg"])
                for jl in range(2):
                    j = 2 * jj + jl
                    S.op("dve", lambda e: e.tensor_scalar(w_out_sb[:, j, :], stg[:, jl * 1024:(jl + 1) * 1024],
                                                          og[:, j:j + 1], None, ALU.mult),
                         reads=["stg", "og"], writes=["w_out"])
            S.dma("pool", lambda e: e.dma_start(out=wr_sb[:, :, 0:4], in_=w_gr.rearrange("(c p) n -> p c n", p=128)), writes=["wr"])
            S.dma("pool", lambda e: e.dma_start(out=wr_sb[:, :, 4:36], in_=w_er.rearrange("(c p) n -> p c n", p=128)), writes=["wr"])
            S.dma("sp", lambda e: e.dma_start(out=br_bc[:, 0:4], in_=b_gr[0:1, :].partition_broadcast(128)), writes=["br"])
            S.dma("sp", lambda e: e.dma_start(out=br_bc[:, 4:36], in_=b_er[0:1, :].partition_broadcast(128)), writes=["br"])
            S.barrier()

        def rstd_from_ss(rs, ss_ap, n, rkey, sskey):
            S.op("act", lambda e: e.activation(rs, ss_ap, AF.Sqrt, bias=epsc[:], scale=1.0 / n),
                 reads=[sskey, "epsc"], writes=[rkey])
            S.op("dve", lambda e: e.reciprocal(rs, rs), reads=[rkey], writes=[rkey])

        def transpose_to_hT(src_bf, srckey, t, bnk):
            pb = bank_bf(bnk)
            for c in range(8):
                S.op("pe", lambda e: e.transpose(pb[:, c * 128:(c + 1) * 128], src_bf[:, c * 128:(c + 1) * 128], ident),
                     reads=[srckey, "cst", ("hTt", t)] if c == 0 else [srckey, "cst"], writes=[bk(bnk)])
            S.op("act", lambda e: e.copy(hT[:, :, t * 128:(t + 1) * 128], pb.rearrange("p (c n) -> p c n", c=8)),
                 reads=[bk(bnk)], writes=[("hT", t // 4), ("hTt", t)])

        for b in range(NSEQ):
            if "attn" in phases:
              with ExitStack() as s1:
                cqTn = sb(s1, "cqTn", [128, 2, 2048], BF16)
                ckvTn = sb(s1, "ckvTn", [128, 2048], BF16)
                krope = sb(s1, "krope", [128, 2048], BF16)
                oT = sb(s1, "oT", [128, 8, 2048], BF16)
                sinT = sb(s1, "sinT", [128, 2048])
                cosT = sb(s1, "cosT", [128, 2048])

                with ExitStack() as sa:
                    xts = [sb(sa, f"xt{i}", [128, 1024]) for i in range(2)]
                    hbs = [sb(sa, f"hb{i}", [128, 1024], BF16) for i in range(2)]
                    junk = sb(sa, "junk", [128, 1024], BF16)
                    ssA = sb(sa, "ssA", [128, 2])
                    rsA = sb(sa, "rsA", [128, 2])
                    wlat = sb(sa, "wlat", [128, 8, 384], BF16)
                    latf = sb(sa, "latf", [128, 3, 512])
                    latsq = sb(sa, "latsq", [128, 3, 512], BF16)
                    rbc = sb(sa, "rbc", [128, 2, 512])
                    posi = sb(sa, "posi", [128, 512], I32)
                    ang = sb(sa, "ang", [128, 512])
                    rtmp = sb(sa, "rtmp", [128, 512])
                    ru = sb(sa, "ru", [128, 512])
                    ta = sb(sa, "ta", [128, 512])
                    tb = sb(sa, "tb", [128, 512])

                    S.dma("sp", lambda e: e.dma_start(out=gbc[:], in_=attn_norm[0:1, :].partition_broadcast(128)), writes=["gbc"])
                    S.dma("pool", lambda e: e.dma_start(out=wlat[:], in_=w_in.rearrange("(c p) n -> p c n", p=128)[:, :, 1536:1920]),
                          writes=["wlat"])
                    for t in range(16):
                        xt = xts[t % 2]
                        hb = hbs[t % 2]
                        xk, hk = f"xt{t % 2}", f"hb{t % 2}"
                        sl = t % 2
                        S.dma("sp", lambda e: e.dma_start(out=xt[:], in_=x[b, t * 128:(t + 1) * 128, :]), writes=[xk])
                        S.op("dve", lambda e: e.memset(ssA[:, sl:sl + 1], 0.0), writes=[f"ssA{sl}"])
                        S.op("act", lambda e: e.activation(junk[:], xt[:], AF.Square, accum_out=ssA[:, sl:sl + 1]),
                             reads=[xk], writes=["junk", f"ssA{sl}"])
                        rstd_from_ss(rsA[:, sl:sl + 1], ssA[:, sl:sl + 1], 1024.0, f"rsA{sl}", f"ssA{sl}")
                        S.op("dve", lambda e: e.scalar_tensor_tensor(hb[:], xt[:], rsA[:, sl:sl + 1], gbc[:], ALU.mult, ALU.mult),
                             reads=[xk, f"rsA{sl}", "gbc"], writes=[hk])
                        transpose_to_hT(hb, hk, t, t % 2)

                    for G in range(4):
                        gs = slice(G * 512, (G + 1) * 512)
                        hTk = ("hT", G)
                        for lc in range(3):
                            bn = 2 + lc
                            for c in range(8):
                                S.op("pe", lambda e: e.matmul(bank(bn), wlat[:, c, lc * 128:(lc + 1) * 128], hT[:, c, gs],
                                                              start=(c == 0), stop=(c == 7)),
                                     reads=["wlat", hTk], writes=[bk(bn)])
                            S.op("act", lambda e: e.copy(latf[:, lc, :], bank(bn)), reads=[bk(bn)], writes=[("latf", lc)])
                            S.op("dve", lambda e: e.tensor_tensor(latsq[:, lc, :], latf[:, lc, :], latf[:, lc, :], ALU.mult),
                                 reads=[("latf", lc)], writes=[("latsq", lc)])
                        S.op("pe", lambda e: e.matmul(bank(5), ones_bf[:], latsq[:, 0, :], start=True, stop=False),
                             reads=["ones_bf", ("latsq", 0)], writes=[bk(5)])
                        S.op("pe", lambda e: e.matmul(bank(5), ones_bf[:], latsq[:, 1, :], start=False, stop=True),
                             reads=["ones_bf", ("latsq", 1)], writes=[bk(5)])
                        S.op("pe", lambda e: e.matmul(bank(6), ones_bf[:], latsq[:, 2, :], start=True, stop=True),
                             reads=["ones_bf", ("latsq", 2)], writes=[bk(6)])
                        for (ri, bnk_, nn) in ((0, 5, 256.0), (1, 6, 128.0)):
                            S.op("act", lambda e: e.activation(rbc[:, ri, :], bank(bnk_), AF.Ln, bias=epsc[:], scale=1.0 / nn),
                                 reads=[bk(bnk_), "epsc"], writes=[("rbc", ri)])
                            S.op("act", lambda e: e.activation(rbc[:, ri, :], rbc[:, ri, :], AF.Exp, scale=-0.5),
                                 reads=[("rbc", ri)], writes=[("rbc", ri)])
                        for lc in range(2):
                            S.op("dve", lambda e: e.tensor_tensor(cqTn[:, lc, gs], latf[:, lc, :], rbc[:, 0, :], ALU.mult),
                                 reads=[("latf", lc), ("rbc", 0)], writes=["cqTn"])
                        S.op("dve", lambda e: e.tensor_tensor(ckvTn[:, gs], latf[:, 2, :], rbc[:, 1, :], ALU.mult),
                             reads=[("latf", 2), ("rbc", 1)], writes=["ckvTn"])
                        P = slice(64, 96)
                        S.dma("sp", lambda e: e.dma_start(out=posi[P, :], in_=positions[b:b + 1, gs].partition_broadcast(32)),
                              writes=["posi"])
                        S.op("dve", lambda e: e.tensor_copy(ang[P, :], posi[P, :]), reads=["posi"], writes=["ang"])
                        S.op("dve", lambda e: e.tensor_scalar(ang[P, :], ang[P, :], invf_sb[P, 0:1], None, ALU.mult),
                             reads=["ang", "invf"], writes=["ang"])
                        for (dst, dk, add) in ((sinT, "sinT", 0.0), (cosT, "cosT", float(np.pi / 2))):
                            S.op("dve", lambda e: e.tensor_scalar(ru[P, :], ang[P, :], add, None, ALU.add),
                                 reads=["ang"], writes=["ru"])
                            S.op("dve", lambda e: e.tensor_scalar(rtmp[P, :], ru[P, :], 1.0 / TWO_PI, None, ALU.mult),
                                 reads=["ru"], writes=["rtmp"])
                            S.op("dve", lambda e: e.tensor_copy(posi[P, :], rtmp[P, :]), reads=["rtmp"], writes=["posi"])
                            S.op("dve", lambda e: e.tensor_copy(rtmp[P, :], posi[P, :]), reads=["posi"], writes=["rtmp"])
                            S.op("dve", lambda e: e.scalar_tensor_tensor(rtmp[P, :], rtmp[P, :], -TWO_PI, ru[P, :], ALU.mult, ALU.add),
                                 reads=["rtmp", "ru"], writes=["rtmp"])
                            S.op("dve", lambda e: e.tensor_scalar(rtmp[P, :], rtmp[P, :], float(np.pi), float(-np.pi), ALU.min, ALU.max),
                                 reads=["rtmp"], writes=["rtmp"])
                            S.op("act", lambda e: e.activation(dst[P, gs], rtmp[P, :], AF.Sin), reads=["rtmp"], writes=[(dk, G)])
                        for (wt, wk, bn) in ((wkr, "wkr", 7), (wkr2, "wkr2", 5)):
                            for c in range(8):
                                S.op("pe", lambda e: e.matmul(bank(bn)[0:96, :], wt[:, c, :], hT[:, c, gs], start=(c == 0), stop=(c == 7)),
                                     reads=[wk, hTk], writes=[bk(bn)])
                        S.op("dve", lambda e: e.tensor_tensor(ta[P, :], bank(7)[P, :], cosT[P, gs], ALU.mult),
                             reads=[bk(7), ("cosT", G)], writes=["ta"])
                        S.op("dve", lambda e: e.tensor_tensor(tb[P, :], bank(5)[P, :], sinT[P, gs], ALU.mult),
                             reads=[bk(5), ("sinT", G)], writes=["tb"])
                        S.op("dve", lambda e: e.tensor_tensor(krope[P, gs], ta[P, :], tb[P, :], ALU.add),
                             reads=["ta", "tb"], writes=["krope"])
                    S.barrier()

                if debug and b == 0:
                    S.dma("sp", lambda e: e.dma_start(out=dbg_hT[:, :, :], in_=hT[:]), reads=[("hT", g) for g in range(4)], writes=["dbg_hT"])
                    S.barrier()
                def run_passes(proj_chunks, loop_iters, npass):
                    for ch in proj_chunks(0):
                        ch()
                    for j in range(npass):
                        nxt = S.record(proj_chunks(j + 1)) if j + 1 < npass else []
                        iters = loop_iters(j)
                        per = -(-len(nxt) // max(1, len(iters) - 6))
                        for idx, it in enumerate(iters):
                            it()
                            if idx >= 2:
                                for _ in range(per):
                                    if nxt:
                                        nxt.pop(0)()
                        while nxt:
                            nxt.pop(0)()

                with ExitStack() as sp_:
                    wsb2 = [sb(sp_, f"wsb{i}", [128, 8, 3, 128], BF16) for i in range(2)]
                    qs02 = [[sb(sp_, f"qs0_{i}_{h}", [128, 2048], BF16) for h in range(2)] for i in range(2)]
                    ksT2 = [sb(sp_, f"ksT{i}", [128, 2048], BF16) for i in range(2)]
                    vs2 = [sb(sp_, f"vs{i}", [128, 16, 128], BF16) for i in range(2)]
                    E1 = [sb(sp_, f"E1_{i}", [128, 512]) for i in range(3)]
                    Lb = [sb(sp_, f"Lb_{i}", [128, 512], BF16) for i in range(3)]
                    Xe = [sb(sp_, f"Xe_{i}", [128, 512]) for i in range(2)]
                    At = [sb(sp_, f"At_{i}", [128, 512], BF16) for i in range(3)]
                    S32 = [sb(sp_, f"S32_{i}", [128, 512]) for i in range(2)]
                    Sb = [sb(sp_, f"Sb_{i}", [128, 512], BF16) for i in range(2)]
                    wi_c = w_in.rearrange("(c p) n -> p c n", p=128)

                    def sb_proj_chunks(j):
                        pj = j % 2
                        wsb, qs0, ksT, vs = wsb2[pj], qs02[pj], ksT2[pj], vs2[pj]
                        chunks = []

                        def c_load():
                            for w in range(3):
                                S.dma("pool", lambda e: e.dma_start(out=wsb[:, :, w, :],
                                                                    in_=wi_c[:, :, w * 512 + j * 128:w * 512 + (j + 1) * 128]),
                                      writes=[("wsb", pj, w)])
                            S.op("dve", lambda e: e.memset(qs0[0][64:128, :], 0.0), writes=[("qs0", pj, 0, G) for G in range(4)])
                            S.op("dve", lambda e: e.memset(qs0[1][0:64, :], 0.0), writes=[("qs0", pj, 1, G) for G in range(4)])
                        chunks.append(c_load)
                        for G in range(4):
                            gs = slice(G * 512, (G + 1) * 512)

                            def c_q(G=G, gs=gs):
                                for c in range(8):
                                    S.op("pe", lambda e: e.matmul(bank(6), wsb[:, c, 0, :], hT[:, c, gs], start=(c == 0), stop=(c == 7)),
                                         reads=[("wsb", pj, 0), ("hT", G)], writes=[bk(6)])
                                S.op("dve", lambda e: e.tensor_scalar(qs0[0][0:64, gs], bank(6)[0:64, :], 0.125, None, ALU.mult),
                                     reads=[bk(6)], writes=[("qs0", pj, 0, G)])
                                S.op("dve", lambda e: e.tensor_scalar(qs0[1][64:128, gs], bank(6)[64:128, :], 0.125, None, ALU.mult),
                                     reads=[bk(6)], writes=[("qs0", pj, 1, G)])

                            def c_k(G=G, gs=gs):
                                for c in range(8):
                                    S.op("pe", lambda e: e.matmul(bank(7), wsb[:, c, 1, :], hT[:, c, gs], start=(c == 0), stop=(c == 7)),
                                         reads=[("wsb", pj, 1), ("hT", G)], writes=[bk(7)])
                                S.op("dve", lambda e: e.tensor_copy(ksT[:, gs], bank(7)), reads=[bk(7)], writes=[("ksT", pj, G)])

                            def c_v(G=G):
                                bn = 6 + (G % 2)
                                for tl in range(4):
                                    t = G * 4 + tl
                                    for c in range(8):
                                        S.op("pe", lambda e: e.matmul(bank(bn)[:, tl * 128:(tl + 1) * 128], hT[:, c, t * 128:(t + 1) * 128],
                                                                      wsb[:, c, 2, :], start=(c == 0), stop=(c == 7)),
                                             reads=[("wsb", pj, 2), ("hT", G)], writes=[bk(bn)])
                                S.op("dve", lambda e: e.tensor_copy(vs[:, G * 4:(G + 1) * 4, :], bank(bn).rearrange("p (t n) -> p t n", t=4)),
                                     reads=[bk(bn)], writes=[("vs", pj, G)])
                            chunks += [c_q, c_k, c_v]
                        return chunks

                    def sb_loop_iters(j):
                        pj = j % 2
                        qs0, ksT, vs = qs02[pj], ksT2[pj], vs2[pj]
                        units = [(hl, G, i) for G in range(4) for i in range(4 * G + 3, -1, -1) for hl in range(2)]
                        nU = len(units)

                        def geom(u):
                            hl, G, i = units[u]
                            q0 = max(i, 4 * G) * 128
                            off = q0 - G * 512
                            return dict(hl=hl, G=G, i=i, off=off, cs=slice(off, 512), qsl=slice(q0, (G + 1) * 512),
                                        ksl=slice(i * 128, (i + 1) * 128), diag=(i >= 4 * G), pr=slice(hl * 64, (hl + 1) * 64),
                                        first=(i == 4 * G + 3), last=(i == 0), gs=slice(G * 512, (G + 1) * 512))

                        def st0(u):
                            g = geom(u)
                            zb = u % 2
                            S.op("pe", lambda e: e.matmul(bank(zb)[:, g["cs"]], ksT[:, g["ksl"]], qs0[g["hl"]][:, g["qsl"]], start=True, stop=True),
                                 reads=[("ksT", pj, g["i"] // 4), ("qs0", pj, g["hl"], g["G"])], writes=[bk(zb)])

                        def st1a(u):
                            g = geom(u)
                            zb, cs, off = u % 2, g["cs"], g["off"]
                            e1, e1k = E1[u % 3], f"E1_{u % 3}"
                            S.op("act", lambda e: e.activation(e1[:, cs], bank(zb)[:, cs], AF.Exp), reads=[bk(zb)], writes=[e1k])
                            if g["diag"]:
                                S.op("dve", lambda e: e.tensor_tensor(e1[:, off:off + 128], e1[:, off:off + 128], mstrict, ALU.mult),
                                     reads=[e1k, "cst"], writes=[e1k])

                        def st1b(u):
                            g = geom(u)
                            cs = g["cs"]
                            e1, lb = E1[u % 3], Lb[u % 3]
                            e1k, lbk = f"E1_{u % 3}", f"Lb_{u % 3}"
                            S.op("act", lambda e: e.activation(lb[:, cs], e1[:, cs], AF.Ln, bias=onec[:], scale=1.0),
                                 reads=[e1k, "onec"], writes=[lbk])

                        def st2a(u):
                            g = geom(u)
                            cs, hl = g["cs"], g["hl"]
                            cbk = 2 + u % 2
                            lb, xe = Lb[u % 3], Xe[u % 2]
                            lbk, xek = f"Lb_{u % 3}", f"Xe_{u % 2}"
                            S.op("pe", lambda e: e.matmul(bank(cbk)[:, cs], tri, lb[:, cs], start=True, stop=g["first"]),
                                 reads=["cst", lbk], writes=[bk(cbk)])
                            if not g["first"]:
                                S.op("pe", lambda e: e.matmul(bank(cbk)[:, cs], ones_bf[:], Sb[hl][:, cs], start=False, stop=True),
                                     reads=["ones_bf", f"Sb_{hl}"], writes=[bk(cbk)])
                            S.op("act", lambda e: e.activation(xe[:, cs], bank(cbk)[:, cs], AF.Exp, scale=-1.0), reads=[bk(cbk)], writes=[xek])

                        def st2b(u):
                            g = geom(u)
                            cs, hl = g["cs"], g["hl"]
                            e1, lb, xe, at = E1[u % 3], Lb[u % 3], Xe[u % 2], At[u % 3]
                            e1k, lbk, xek, atk = f"E1_{u % 3}", f"Lb_{u % 3}", f"Xe_{u % 2}", f"At_{u % 3}"
                            if not g["last"]:
                                if g["first"]:
                                    S.op("dve", lambda e: e.memset(S32[hl][:], 0.0), writes=[f"S32_{hl}"])
                                S.op("dve", lambda e: e.tensor_tensor(S32[hl][:, cs], S32[hl][:, cs], lb[:, cs], ALU.add),
                                     reads=[f"S32_{hl}", lbk], writes=[f"S32_{hl}"])
                            S.op("dve", lambda e: e.tensor_tensor(at[:, cs], xe[:, cs], e1[:, cs], ALU.mult), reads=[xek, e1k], writes=[atk])
                            if not g["last"]:
                                S.op("dve", lambda e: e.tensor_copy(Sb[hl][:], S32[hl][:]), reads=[f"S32_{hl}"], writes=[f"Sb_{hl}"])

                        def st3(u):
                            g = geom(u)
                            cs = g["cs"]
                            ob = 4 + g["hl"]
                            at, atk = At[u % 3], f"At_{u % 3}"
                            S.op("pe", lambda e: e.matmul(bank(ob)[0:64, cs], vs[:, g["i"], g["pr"]], at[:, cs], start=g["first"], stop=g["last"],
                                                          skip_group_check=True),
                                 reads=[("vs", pj, g["i"] // 4), atk], writes=[bk(ob)])
                            if g["last"]:
                                S.op("act", lambda e: e.copy(oT[g["pr"], j, g["gs"]], bank(ob)[0:64, :]), reads=[bk(ob)], writes=[("oT", j)])

                        def mk(k):
                            def it():
                                if 0 <= k < nU:
                                    st1b(k)
                                if 0 <= k + 1 < nU:
                                    st1a(k + 1)
                                if 0 <= k - 1 < nU:
                                    st2a(k - 1)
                                if 0 <= k + 2 < nU:
                                    st0(k + 2)
                                if 0 <= k - 1 < nU:
                                    st2b(k - 1)
                                if 0 <= k - 2 < nU:
                                    st3(k - 2)
                            return it
                        return [mk(k) for k in range(-2, nU + 2)]

                    run_passes(sb_proj_chunks, sb_loop_iters, 4)
                    S.barrier()

                with ExitStack() as sm:
                    qmT2 = [sb(sm, f"qmT{i}", [128, 2, 2048], BF16) for i in range(2)]
                    kmT2 = [sb(sm, f"kmT{i}", [128, 2, 2048], BF16) for i in range(2)]
                    vm2 = [sb(sm, f"vm{i}", [128, 16, 2, 65], BF16) for i in range(2)]
                    Et = [sb(sm, f"Et_{i}", [128, 512], BF16) for i in range(3)]
                    dr = sb(sm, "dr", [128, 512])
                    rb = sb(sm, "rb", [128, 512])
                    ta = sb(sm, "ta", [128, 512])
                    tb = sb(sm, "tb", [128, 512])
                    P = slice(64, 96)
                    scale = float(96 ** -0.5)

                    def mla_proj_chunks(m):
                        pm = m % 2
                        qmT, kmT, vm = qmT2[pm], kmT2[pm], vm2[pm]
                        chunks = []

                        def c_init():
                            S.op("dve", lambda e: e.memset(vm[:], 1.0), writes=[("vm", pm, G) for G in range(4)])
                            S.op("dve", lambda e: e.memset(qmT[96:128], 0.0), writes=[("qmT", pm, G) for G in range(4)])
                            S.op("dve", lambda e: e.memset(kmT[96:128], 0.0), writes=[("kmT", pm, G) for G in range(4)])
                        chunks.append(c_init)
                        for G in range(4):
                            gs = slice(G * 512, (G + 1) * 512)
                            for hl in range(2):
                                def c_qk(G=G, gs=gs, hl=hl):
                                    h = 2 * m + hl
                                    for (wt, wk, bn) in ((w_uq_sb, "w_uq", 6), (w2_sb, "w2", 7)):
                                        for c in range(2):
                                            S.op("pe", lambda e: e.matmul(bank(bn)[0:96, :], wt[:, c, h, :], cqTn[:, c, gs], start=(c == 0), stop=(c == 1)),
                                                 reads=[wk, "cqTn"], writes=[bk(bn)])
                                    S.op("dve", lambda e: e.tensor_copy(qmT[0:64, hl, gs], bank(6)[0:64, :]), reads=[bk(6)], writes=[("qmT", pm, G)])
                                    S.op("dve", lambda e: e.tensor_tensor(ta[P, :], bank(6)[P, :], cosT[P, gs], ALU.mult),
                                         reads=[bk(6), ("cosT", G)], writes=["ta"])
                                    S.op("dve", lambda e: e.tensor_tensor(tb[P, :], bank(7)[P, :], sinT[P, gs], ALU.mult),
                                         reads=[bk(7), ("sinT", G)], writes=["tb"])
                                    S.op("dve", lambda e: e.tensor_tensor(qmT[P, hl, gs], ta[P, :], tb[P, :], ALU.add),
                                         reads=["ta", "tb"], writes=[("qmT", pm, G)])
                                    S.op("pe", lambda e: e.matmul(bank(6)[0:64, :], w_ukv_sb[:, h * 128:h * 128 + 64], ckvTn[:, gs], start=True, stop=True),
                                         reads=["w_ukv", "ckvTn"], writes=[bk(6)])
                                    S.op("dve", lambda e: e.tensor_copy(kmT[0:64, hl, gs], bank(6)[0:64, :]), reads=[bk(6)], writes=[("kmT", pm, G)])
                                    S.op("dve", lambda e: e.tensor_copy(kmT[P, hl, gs], krope[P, gs]), reads=["krope"], writes=[("kmT", pm, G)])
                                chunks.append(c_qk)

                            def c_v(G=G):
                                bn = 7
                                for tl in range(4):
                                    t = G * 4 + tl
                                    S.op("pe", lambda e: e.matmul(bank(bn)[:, tl * 128:(tl + 1) * 128], ckvTn[:, t * 128:(t + 1) * 128],
                                                                  wv_sb[:, 2 * m:2 * m + 2, :], start=True, stop=True),
                                         reads=["wv", "ckvTn"], writes=[bk(bn)])
                                S.op("dve", lambda e: e.tensor_copy(vm[:, G * 4:(G + 1) * 4, :, 0:64],
                                                                    bank(bn).rearrange("p (t h d) -> p t h d", t=4, h=2)),
                                     reads=[bk(bn)], writes=[("vm", pm, G)])
                            chunks.append(c_v)
                        return chunks

                    def mla_loop_iters(m):
                        pm = m % 2
                        qmT, kmT, vm = qmT2[pm], kmT2[pm], vm2[pm]
                        units = [(hl, G, i) for G in range(4) for i in range(0, 4 * G + 4) for hl in range(2)]
                        nU = len(units)

                        def geom(u):
                            hl, G, i = units[u]
                            q0 = max(i, 4 * G) * 128
                            off = q0 - G * 512
                            return dict(hl=hl, G=G, i=i, off=off, cs=slice(off, 512), qsl=slice(q0, (G + 1) * 512),
                                        ksl=slice(i * 128, (i + 1) * 128), diag=(i >= 4 * G), pr=slice(hl * 64, (hl + 1) * 64),
                                        first=(i == 0), last=(i == 4 * G + 3), gs=slice(G * 512, (G + 1) * 512))

                        def st0(u):
                            g = geom(u)
                            zb = u % 3
                            S.op("pe", lambda e: e.matmul(bank(zb)[:, g["cs"]], kmT[:, g["hl"], g["ksl"]], qmT[:, g["hl"], g["qsl"]],
                                                          start=True, stop=True),
                                 reads=[("kmT", pm, g["i"] // 4), ("qmT", pm, g["G"])], writes=[bk(zb)])

                        def st1(u):
                            g = geom(u)
                            zb, cs, off = u % 3, g["cs"], g["off"]
                            et, etk = Et[u % 3], f"Et_{u % 3}"
                            S.op("act", lambda e: e.activation(et[:, cs], bank(zb)[:, cs], AF.Exp, scale=scale), reads=[bk(zb)], writes=[etk])
                            if g["diag"]:
                                S.op("dve", lambda e: e.tensor_tensor(et[:, off:off + 128], et[:, off:off + 128], mcausal, ALU.mult),
                                     reads=[etk, "cst"], writes=[etk])

                        def st2(u):
                            g = geom(u)
                            cs = g["cs"]
                            ob = 4 + g["hl"]
                            et, etk = Et[u % 3], f"Et_{u % 3}"
                            S.op("pe", lambda e: e.matmul(bank(ob)[0:65, cs], vm[:, g["i"], g["hl"], :], et[:, cs], start=g["first"], stop=g["last"],
                                                          skip_group_check=True),
                                 reads=[("vm", pm, g["i"] // 4), etk], writes=[bk(ob)])
                            if g["last"]:
                                S.op("act", lambda e: e.activation(dr[64:65, :], bank(ob)[64:65, :], AF.Ln), reads=[bk(ob)], writes=["dr"])

                                def fin(ob=ob, pr=g["pr"], gs=g["gs"]):
                                    S.op("act", lambda e: e.activation(dr[64:65, :], dr[64:65, :], AF.Exp, scale=-1.0), reads=["dr"], writes=["dr"])
                                    S.op("pe", lambda e: e.matmul(bank(3)[0:64, :], ones_f[64:65, 0:64], dr[64:65, :], start=True, stop=True),
                                         reads=["ones_f", "dr"], writes=[bk(3)])
                                    S.op("act", lambda e: e.copy(rb[0:64, :], bank(3)[0:64, :]), reads=[bk(3)], writes=["rb"])
                                    S.op("dve", lambda e: e.tensor_tensor(oT[pr, 4 + m, gs], bank(ob)[0:64, :], rb[0:64, :], ALU.mult),
                                         reads=[bk(ob), "rb"], writes=[("oT", 4 + m)])
                                deferred.append(fin)

                        deferred = []

                        def mk(k):
                            def it():
                                if 0 <= k + 1 < nU:
                                    st0(k + 1)
                                if 0 <= k < nU:
                                    st1(k)
                                pend = list(deferred)
                                del deferred[:]
                                for f in pend:
                                    f()
                                if 0 <= k - 1 < nU:
                                    st2(k - 1)
                                if k >= nU:
                                    for f in list(deferred):
                                        f()
                                    del deferred[:]
                            return it
                        return [mk(k) for k in range(-1, nU + 1)]

                    run_passes(mla_proj_chunks, mla_loop_iters, 4)
                    S.barrier()

                if debug and b == 0:
                    S.dma("sp", lambda e: e.dma_start(out=dbg_oT[:, :, :], in_=oT[:]), reads=[("oT", jj) for jj in range(8)], writes=["dbg_oT"])
                    S.dma("sp", lambda e: e.dma_start(out=dbg_lat[:, 0:2, :], in_=cqTn[:]), reads=["cqTn"], writes=["dbg_lat0"])
                    S.dma("sp", lambda e: e.dma_start(out=dbg_lat[:, 2, :], in_=ckvTn[:]), reads=["ckvTn"], writes=["dbg_lat1"])
                    S.dma("sp", lambda e: e.dma_start(out=dbg_lat[64:96, 3, :], in_=krope[64:96, :]), reads=["krope"], writes=["dbg_lat2"])
                    S.barrier()
                with ExitStack() as sd:
                    xts = [sb(sd, f"xd{i}", [128, 1024]) for i in range(2)]
                    x1t = [sb(sd, f"x1t{i}", [128, 1024]) for i in range(2)]
                    h2b = [sb(sd, f"h2b{i}", [128, 1024], BF16) for i in range(2)]
                    junk = sb(sd, "junkd", [128, 1024], BF16)
                    osq2 = [sb(sd, f"osq{i}", [128, 8, 128], BF16) for i in range(2)]
                    rsDall = sb(sd, "rsDall", [128, 32])
                    ssD = sb(sd, "ssD", [128, 2])
                    rsD = sb(sd, "rsD", [128, 2])
                    ss2 = sb(sd, "ss2", [128, 1])
                    rs2 = sb(sd, "rs2", [128, 1])
                    lgall = sb(sd, "lgall", [128, 16, 36])
                    lgraw = sb(sd, "lgraw", [128, 16, 36])
                    ss2all = sb(sd, "ss2all", [128, 16])
                    gmax = sb(sd, "gmax", [128, 16, 1])
                    ohg = sb(sd, "ohg", [128, 16, 4])
                    gsh = sb(sd, "gsh", [128, 16, 4])
                    gex = sb(sd, "gex", [128, 16, 4])
                    gsum = sb(sd, "gsum", [128, 16, 1])
                    gval = sb(sd, "gval", [128, 16, 1])
                    tmp4 = sb(sd, "tmp4", [128, 16, 4, 8])
                    loc = sb(sd, "loc", [128, 16, 8])
                    loc2 = sb(sd, "loc2", [128, 16, 8])
                    l1 = sb(sd, "l1", [128, 16, 1])
                    l2 = sb(sd, "l2", [128, 16, 1])
                    m1 = sb(sd, "m1", [128, 16, 8])
                    m2 = sb(sd, "m2", [128, 16, 8])
                    dd = sb(sd, "dd", [128, 16, 1])
                    s1 = sb(sd, "s1", [128, 16, 1])
                    w1 = sb(sd, "w1", [128, 16, 1])
                    w2 = sb(sd, "w2", [128, 16, 1])
                    wl = sb(sd, "wl", [128, 16, 8])
                    wl2 = sb(sd, "wl2", [128, 16, 8])
                    S.dma("sp", lambda e: e.dma_start(out=gbc[:], in_=ffn_norm[0:1, :].partition_broadcast(128)), writes=["gbc"])
                    def d_partA(t):
                        ts_ = slice(t * 128, (t + 1) * 128)
                        xt, xk = xts[t % 2], f"xd{t % 2}"
                        x1, x1k = x1t[t % 2], f"x1t{t % 2}"
                        hb, hk = h2b[t % 2], f"h2b{t % 2}"
                        S.dma("sp", lambda e: e.dma_start(out=xt[:], in_=x[b, ts_, :]), writes=[xk])
                        for grp in range(2):
                            for hh in range(2):
                                bn = grp * 2 + hh
                                for jl in range(4):
                                    jj = grp * 4 + jl
                                    S.op("pe", lambda e: e.matmul(bank(bn), oT[:, jj, ts_], w_out_sb[:, jj, hh * 512:(hh + 1) * 512],
                                                                  start=(jl == 0), stop=(jl == 3)),
                                         reads=[("oT", jj), "w_out"], writes=[bk(bn)])
                        S.op("dve", lambda e: e.scalar_tensor_tensor(x1[:], PS[0][:], rsDall[:, 2 * t:2 * t + 1], xt[:], ALU.mult, ALU.add),
                             reads=[bk(0), bk(1), "rsDall", xk], writes=[x1k])
                        S.op("dve", lambda e: e.scalar_tensor_tensor(x1[:], PS[1][:], rsDall[:, 2 * t + 1:2 * t + 2], x1[:], ALU.mult, ALU.add),
                             reads=[bk(2), bk(3), "rsDall", x1k], writes=[x1k])
                        S.dma("sp", lambda e: e.dma_start(out=x1s[b, ts_, :], in_=x1[:]), reads=[x1k], writes=[("x1s", b, t)])
                        S.op("act", lambda e: e.activation(junk[:], x1[:], AF.Square, accum_out=ss2all[:, t:t + 1]),
                             reads=[x1k, "ss2init"], writes=["junkd", ("ss2all", t)])
                        S.op("dve", lambda e: e.tensor_tensor(hb[:], x1[:], gbc[:], ALU.mult), reads=[x1k, "gbc"], writes=[hk])

                    def d_partB(t):
                        ts_ = slice(t * 128, (t + 1) * 128)
                        x1, x1k = x1t[t % 2], f"x1t{t % 2}"
                        hb, hk = h2b[t % 2], f"h2b{t % 2}"
                        transpose_to_hT(hb, hk, t, 5)
                        for c in range(8):
                            S.op("pe", lambda e: e.matmul(bank(6)[:, 0:36], hT[:, c, ts_], wr_sb[:, c, :], start=(c == 0), stop=(c == 7)),
                                 reads=[("hT", t // 4), ("hTt", t), "wr"], writes=[bk(6)])
                        S.op("dve", lambda e: e.tensor_copy(lgraw[:, t, :], bank(6)[:, 0:36]), reads=[bk(6)], writes=["lgraw"])

                    S.op("dve", lambda e: e.memset(ss2all[:], 0.0), writes=["ss2init"] + [("ss2all", t) for t in range(16)])
                    for t in range(16):
                        ts_ = slice(t * 128, (t + 1) * 128)
                        oq, oqk = osq2[t % 2], f"osq{t % 2}"
                        S.op("dve", lambda e: e.tensor_tensor(oq[:], oT[:, :, ts_], oT[:, :, ts_], ALU.mult),
                             reads=[("oT", jj) for jj in range(8)], writes=[oqk])
                        for grp in range(2):
                            for jl in range(4):
                                S.op("pe", lambda e: e.matmul(bank(4)[:, 2 * t + grp:2 * t + grp + 1], oq[:, grp * 4 + jl, :], ones_bf[:, 0:1],
                                                              start=(t == 0 and jl == 0 and grp == 0), stop=(t == 15 and grp == 1 and jl == 3),
                                                              skip_group_check=True),
                                     reads=[oqk, "ones_bf"], writes=[bk(4)])
                    rstd_from_ss(rsDall[:], bank(4)[:, 0:32], 512.0, "rsDall", bk(4))
                    for t in range(17):
                        if t < 16:
                            d_partA(t)
                        if t >= 1:
                            d_partB(t - 1)
                    S.op("act", lambda e: e.activation(rs2all[:], ss2all[:], AF.Sqrt, bias=epsc[:], scale=1.0 / 1024.0),
                         reads=[("ss2all", t) for t in range(16)] + ["epsc"], writes=["rs2all"])
                    S.op("dve", lambda e: e.reciprocal(rs2all[:], rs2all[:]), reads=["rs2all"], writes=["rs2all"])
                    rs2b = rs2all[:].rearrange("p (t o) -> p t o", o=1)
                    S.op("dve", lambda e: e.tensor_tensor(lgall[:], lgraw[:], rs2b.to_broadcast([128, 16, 36]), ALU.mult),
                         reads=["lgraw", "rs2all"], writes=["lgall"])
                    S.op("dve", lambda e: e.tensor_tensor(lgall[:], lgall[:], br_bc[:].rearrange("p (o n) -> p o n", o=1).to_broadcast([128, 16, 36]), ALU.add),
                         reads=["lgall", "br"], writes=["lgall"])
                    T4 = [128, 16, 4]
                    T8 = [128, 16, 8]
                    T48 = [128, 16, 4, 8]
                    glg = lgall[:, :, 0:4]
                    elg = lgall[:, :, 4:36].rearrange("p t (g k) -> p t g k", g=4)
                    ohg4 = ohg[:].rearrange("p t (g o) -> p t g o", o=1)
                    V = lambda fn, r, w: S.op("dve", fn, reads=r, writes=w)
                    V(lambda e: e.reduce_max(gmax[:], glg, axis=AX.X), ["lgall"], ["gmax"])
                    V(lambda e: e.tensor_tensor(ohg[:], glg, gmax[:].to_broadcast(T4), ALU.is_equal), ["lgall", "gmax"], ["ohg"])
                    V(lambda e: e.tensor_tensor(gsh[:], glg, gmax[:].to_broadcast(T4), ALU.subtract), ["lgall", "gmax"], ["gsh"])
                    S.op("act", lambda e: e.activation(gex[:], gsh[:], AF.Exp), reads=["gsh"], writes=["gex"])
                    V(lambda e: e.reduce_sum(gsum[:], gex[:], axis=AX.X), ["gex"], ["gsum"])
                    V(lambda e: e.reciprocal(gval[:], gsum[:]), ["gsum"], ["gval"])
                    V(lambda e: e.tensor_tensor(tmp4[:], elg, ohg4.to_broadcast(T48), ALU.mult), ["lgall", "ohg"], ["tmp4"])
                    V(lambda e: e.reduce_sum(loc[:].rearrange("p t (k o) -> p t k o", o=1), tmp4[:].rearrange("p t g k -> p t k g"), axis=AX.X),
                      ["tmp4"], ["loc"])
                    V(lambda e: e.reduce_max(l1[:], loc[:], axis=AX.X), ["loc"], ["l1"])
                    V(lambda e: e.tensor_tensor(m1[:], loc[:], l1[:].to_broadcast(T8), ALU.is_equal), ["loc", "l1"], ["m1"])
                    V(lambda e: e.scalar_tensor_tensor(loc2[:], m1[:], -1e30, loc[:], ALU.mult, ALU.add), ["m1", "loc"], ["loc2"])
                    V(lambda e: e.reduce_max(l2[:], loc2[:], axis=AX.X), ["loc2"], ["l2"])
                    V(lambda e: e.tensor_tensor(m2[:], loc2[:], l2[:].to_broadcast(T8), ALU.is_equal), ["loc2", "l2"], ["m2"])
                    V(lambda e: e.tensor_tensor(dd[:], l2[:], l1[:], ALU.subtract), ["l1", "l2"], ["dd"])
                    S.op("act", lambda e: e.activation(s1[:], dd[:], AF.Exp), reads=["dd"], writes=["s1"])
                    V(lambda e: e.tensor_scalar(s1[:], s1[:], 1.0, None, ALU.add), ["s1"], ["s1"])
                    V(lambda e: e.reciprocal(s1[:], s1[:]), ["s1"], ["s1"])
                    V(lambda e: e.tensor_tensor(w1[:], gval[:], s1[:], ALU.mult), ["gval", "s1"], ["w1"])
                    V(lambda e: e.tensor_tensor(w2[:], gval[:], w1[:], ALU.subtract), ["gval", "w1"], ["w2"])
                    V(lambda e: e.tensor_tensor(wl[:], m1[:], w1[:].to_broadcast(T8), ALU.mult), ["m1", "w1"], ["wl"])
                    V(lambda e: e.tensor_tensor(wl2[:], m2[:], w2[:].to_broadcast(T8), ALU.mult), ["m2", "w2"], ["wl2"])
                    V(lambda e: e.tensor_tensor(wl[:], wl[:], wl2[:], ALU.add), ["wl", "wl2"], ["wl"])
                    V(lambda e: e.tensor_tensor(Wfull[:].rearrange("p t (g k) -> p t g k", g=4), ohg4.to_broadcast(T48),
                                                wl[:].rearrange("p t (o k) -> p t o k", o=1).to_broadcast(T48), ALU.mult),
                      ["ohg", "wl"], [("Wfull", t) for t in range(16)])
                    V(lambda e: e.tensor_tensor(Wfull[:], Wfull[:], rs2b.to_broadcast([128, 16, 32]), ALU.mult),
                      [("Wfull", t) for t in range(16)] + ["rs2all"], [("Wfull", t) for t in range(16)])
                    S.barrier()

            if "moe" in phases:
              with ExitStack() as s2:
                acc = sb(s2, "acc", [128, 16, 1024])
                NW = 3
                wgu = [sb(s2, f"wgu{i}", [128, 8, 512], BF16) for i in range(NW)]
                wd = [sb(s2, f"wd{i}", [128, 2, 1024], BF16) for i in range(NW)]
                sg = [sb(s2, f"sg{i}", [128, 256]) for i in range(2)]
                hid = [sb(s2, f"hid{i}", [128, 256], BF16) for i in range(2)]
                hidT = [sb(s2, f"hidT{i}", [128, 256], BF16) for i in range(2)]
                junk = sb(s2, "junke", [128, 1024], BF16)
                ss3 = sb(s2, "ss3", [128, 2])
                rs3 = sb(s2, "rs3", [128, 2])
                yt = [sb(s2, f"yt{i}", [128, 1024]) for i in range(2)]
                S.dma("sp", lambda e: e.dma_start(out=gbc[:], in_=final_norm[0:1, :].partition_broadcast(128)), writes=["gbc"])
                for t in range(16):
                    S.dma("sp", lambda e: e.dma_start(out=acc[:, t, :], in_=x1s[b, t * 128:(t + 1) * 128, :]),
                          reads=[("x1s", b, t)], writes=[("acc", t)])

                def load_expert(ex):
                    sl = ex % NW
                    S.dma("pool", lambda e: e.dma_start(out=wgu[sl][:, :, 0:256], in_=w_gate[ex].rearrange("(c p) n -> p c n", p=128)),
                          writes=[f"wgu{sl}"])
                    S.dma("pool", lambda e: e.dma_start(out=wgu[sl][:, :, 256:512], in_=w_up[ex].rearrange("(c p) n -> p c n", p=128)),
                          writes=[f"wgu{sl}"])
                    S.dma("pool", lambda e: e.dma_start(out=wd[sl][:], in_=w_down[ex].rearrange("(c p) n -> p c n", p=128)),
                          writes=[f"wd{sl}"])

                for ex0 in range(min(NW, n_experts)):
                    load_expert(ex0)
                steps = [(ex, t) for ex in range(n_experts) for t in range(16)]
                nS = len(steps)

                def m_gu(k):
                    ex, t = steps[k]
                    sl, p2, ts_ = ex % NW, k % 2, slice(t * 128, (t + 1) * 128)
                    gb = p2
                    for c in range(8):
                        S.op("pe", lambda e: e.matmul(bank(gb), hT[:, c, ts_], wgu[sl][:, c, :], start=(c == 0), stop=(c == 7)),
                             reads=[("hT", t // 4), ("hTt", t), f"wgu{sl}"], writes=[bk(gb)])
                    S.op("act", lambda e: e.activation(sg[p2][:], bank(gb)[:, 0:256], AF.Silu, scale=rs2all[:, t:t + 1]), reads=[bk(gb), "rs2all"], writes=[f"sg{p2}"])
                    S.op("dve", lambda e: e.scalar_tensor_tensor(hid[p2][:], bank(gb)[:, 256:512], Wfull[:, t, ex:ex + 1], sg[p2][:],
                                                                 ALU.mult, ALU.mult),
                         reads=[bk(gb), ("Wfull", t), f"sg{p2}"], writes=[f"hid{p2}"])

                def m_tr(k):
                    p2 = k % 2
                    tbk = 2 + p2
                    tb_ = bank_bf(tbk)
                    for c in range(2):
                        S.op("pe", lambda e: e.transpose(tb_[:, c * 128:(c + 1) * 128], hid[p2][:, c * 128:(c + 1) * 128], ident),
                             reads=[f"hid{p2}", "cst"], writes=[bk(tbk)])
                    S.op("act", lambda e: e.copy(hidT[p2][:], tb_[:, 0:256]), reads=[bk(tbk)], writes=[f"hidT{p2}"])

                def m_dn(k):
                    ex, t = steps[k]
                    sl, p2 = ex % NW, k % 2
                    dps = 2 + p2
                    for hh in range(2):
                        for c in range(2):
                            S.op("pe", lambda e: e.matmul(PS[dps][:, hh * 512:(hh + 1) * 512], hidT[p2][:, c * 128:(c + 1) * 128],
                                                          wd[sl][:, c, hh * 512:(hh + 1) * 512], start=(c == 0), stop=(c == 1)),
                                 reads=[f"hidT{p2}", f"wd{sl}"], writes=[bk(2 * dps + hh)])
                    S.op("dve", lambda e: e.tensor_tensor(acc[:, t, :], acc[:, t, :], PS[dps][:], ALU.add),
                         reads=[("acc", t), bk(2 * dps), bk(2 * dps + 1)], writes=[("acc", t)])
                    if t == 15 and ex + NW < n_experts:
                        load_expert(ex + NW)

                for k in range(nS + 2):
                    if k < nS:
                        m_gu(k)
                    if 0 <= k - 1 < nS:
                        m_tr(k - 1)
                    if 0 <= k - 2 < nS:
                        m_dn(k - 2)
                for t in range(16):
                    sl = t % 2
                    S.op("dve", lambda e: e.memset(ss3[:, sl:sl + 1], 0.0), writes=[f"ss3{sl}"])
                    S.op("act", lambda e: e.activation(junk[:], acc[:, t, :], AF.Square, accum_out=ss3[:, sl:sl + 1]),
                         reads=[("acc", t)], writes=["junke", f"ss3{sl}"])
                    rstd_from_ss(rs3[:, sl:sl + 1], ss3[:, sl:sl + 1], 1024.0, f"rs3{sl}", f"ss3{sl}")
                    S.op("dve", lambda e: e.scalar_tensor_tensor(yt[sl][:], acc[:, t, :], rs3[:, sl:sl + 1], gbc[:], ALU.mult, ALU.mult),
                         reads=[("acc", t), f"rs3{sl}", "gbc"], writes=[f"yt{sl}"])
                    S.dma("sp", lambda e: e.dma_start(out=y[b, t * 128:(t + 1) * 128, :], in_=yt[sl][:]), reads=[f"yt{sl}"], writes=[("y", b, t)])
                S.barrier()
        S.final_wait("sp")
        print("instructions emitted:", S.nins, {e: S.cnt[e] for e in ENGS}, S.ndma)
    return nc


def _consts():
    k = np.arange(128)[:, None]
    q = np.arange(128)[None, :]
    ident = (k == q)
    tri = (k >= q)
    strict = (k < q)
    causal = (k <= q)
    c = np.concatenate([ident, tri, strict, causal, strict], axis=1).astype(np.float32)
    half = 16
    inv_freq = (np.float32(10000.0) ** (-np.arange(half, dtype=np.float32) / np.float32(half))).astype(np.float32)
    invf = np.zeros((128, 1), np.float32)
    for p in range(64, 96):
        invf[p, 0] = inv_freq[(p - 64) % 16]
    return c.astype(ml_dtypes.bfloat16), invf


def make_in_maps(inputs, n_cores, nseq):
    f = lambda a: np.ascontiguousarray(np.asarray(a))
    cst, invf = _consts()
    shared = {
        "attn_norm": f(inputs["attn_norm"]).reshape(1, 1024),
        "w_in": f(inputs["w_in"]).reshape(1024, 1952),
        "q_norm": f(inputs["q_norm"]).reshape(256, 1),
        "w_uq": f(inputs["w_uq"]).reshape(256, 768),
        "kv_norm": f(inputs["kv_norm"]).reshape(128, 1),
        "w_ukv": f(inputs["w_ukv"]).reshape(128, 1024),
        "out_norm": np.concatenate([f(inputs["sb_out_norm"]).reshape(-1), f(inputs["mla_out_norm"]).reshape(-1)]).reshape(1024, 1),
        "w_out": f(inputs["w_out"]).reshape(1024, 1024),
        "ffn_norm": f(inputs["ffn_norm"]).reshape(1, 1024),
        "w_group_router": f(inputs["w_group_router"]).reshape(1024, 4),
        "b_group_router": f(inputs["b_group_router"]).reshape(1, 4),
        "w_expert_router": f(inputs["w_expert_router"]).reshape(1024, 32),
        "b_expert_router": f(inputs["b_expert_router"]).reshape(1, 32),
        "w_gate": f(inputs["w_gate"]).reshape(32, 1024, 256),
        "w_up": f(inputs["w_up"]).reshape(32, 1024, 256),
        "w_down": f(inputs["w_down"]).reshape(32, 256, 1024),
        "final_norm": f(inputs["final_norm"]).reshape(1, 1024),
        "consts": cst,
        "invf": invf,
    }
    xs = f(inputs["x"])
    ps = f(inputs["positions"]).astype(np.int32)
    maps = []
    for c in range(n_cores):
        m = dict(shared)
        m["x"] = xs[c * nseq:(c + 1) * nseq]
        m["positions"] = ps[c * nseq:(c + 1) * nseq]
        maps.append(m)
    return maps


def kernel(**inputs):
    n_cores = 8
    nseq = 4
    nc = build(NSEQ=nseq)
    maps = make_in_maps(inputs, n_cores, nseq)
    res = run_bass_kernel_spmd(nc, maps, core_ids=list(range(n_cores)))
    out = np.concatenate([np.asarray(r["y"]) for r in res.results], axis=0)
    return out.astype(np.float32)
```

```python
import numpy as np
import ml_dtypes
from contextlib import ExitStack
import concourse.bass as bass
import concourse.mybir as mybir
from concourse.bass_utils import run_bass_kernel_spmd

F32 = mybir.dt.float32
BF16 = mybir.dt.bfloat16
I32 = mybir.dt.int32
AF = mybir.ActivationFunctionType
ALU = mybir.AluOpType
AX = mybir.AxisListType

ENGS = ["pe", "act", "dve", "pool", "sp"]
DMA_ENGS = ["sp", "pool"]
ND = 8
EPS = 1e-6
TWO_PI = float(2 * np.pi)


class _Proxy:
    def __init__(self):
        self.call = None

    def __getattr__(self, name):
        def f(*a, **k):
            self.call = (name, a, k)
            return self
        return f


class Sched:
    def __init__(self, nc, es):
        self.nc = nc
        self.eng = {"pe": nc.tensor, "act": nc.scalar, "dve": nc.vector, "pool": nc.gpsimd, "sp": nc.sync}
        self.sem = {e: es.enter_context(nc.semaphore("c_" + e)) for e in ENGS}
        self.dsem = {e: [es.enter_context(nc.semaphore(f"d_{e}_{i}")) for i in range(ND)] for e in DMA_ENGS}
        self.cnt = {e: 0 for e in ENGS}
        self.ndma = {e: 0 for e in DMA_ENGS}
        self.dlast = {e: {} for e in DMA_ENGS}
        self.lastw = {}
        self.readers = {}
        self.seen = {e: {} for e in ENGS}
        self.nins = 0
        self.rec = None

    def record(self, chunk_fns):
        self.rec = []
        for f in chunk_fns:
            f()
        out, self.rec = self.rec, None
        return out

    def _semobj(self, key):
        return self.sem[key[1]] if key[0] == "c" else self.dsem[key[1]][key[2]]

    def _collect(self, reads, writes):
        toks = {}

        def add(k, v):
            if toks.get(k, 0) < v:
                toks[k] = v
        for r in reads:
            t = self.lastw.get(r)
            if t is not None:
                add(*t)
        for w in writes:
            t = self.lastw.get(w)
            if t is not None:
                add(*t)
            for k, v in self.readers.get(w, {}).items():
                add(k, v)
        return toks

    def _wait(self, eng, toks):
        seen = self.seen[eng]
        for k, v in toks.items():
            if k[0] == "c" and k[1] == eng and eng == "pe":
                continue
            if seen.get(k, 0) >= v:
                continue
            self.eng[eng].wait_ge(self._semobj(k), v)
            seen[k] = v
            self.nins += 1

    def _record(self, tok, reads, writes):
        for w in writes:
            self.lastw[w] = tok
            self.readers[w] = {}
        for r in reads:
            d = self.readers.setdefault(r, {})
            if d.get(tok[0], 0) < tok[1]:
                d[tok[0]] = tok[1]

    def op(self, eng, fn, reads=(), writes=()):
        if self.rec is not None:
            p = _Proxy()
            fn(p)
            name, a, k = p.call
            self.rec.append(lambda: self.op(eng, lambda e: getattr(e, name)(*a, **k), reads, writes))
            return
        toks = self._collect(reads, writes)
        self._wait(eng, toks)
        ins = fn(self.eng[eng])
        self.cnt[eng] += 1
        ins.then_inc(self.sem[eng], 1)
        self.nins += 1
        tok = (("c", eng), self.cnt[eng])
        self._record(tok, reads, writes)

    def dma(self, eng, fn, reads=(), writes=()):
        if self.rec is not None:
            p = _Proxy()
            fn(p)
            name, a, k = p.call
            self.rec.append(lambda: self.dma(eng, lambda e: getattr(e, name)(*a, **k), reads, writes))
            return
        n = self.ndma[eng]
        self.ndma[eng] += 1
        slot = n % ND
        val = 16 * (n // ND + 1)
        toks = self._collect(reads, writes)
        key = ("d", eng, slot)
        if val > 16:
            toks[key] = max(toks.get(key, 0), val - 16)
        self._wait(eng, toks)
        ins = fn(self.eng[eng])
        ins.then_inc(self.dsem[eng][slot], 16)
        self.nins += 1
        self.dlast[eng][slot] = val
        self._record((key, val), reads, writes)

    def barrier(self):
        toks = {}
        for e in ENGS:
            if self.cnt[e] > 0:
                toks[("c", e)] = self.cnt[e]
        for q in DMA_ENGS:
            for slot, val in self.dlast[q].items():
                toks[("d", q, slot)] = val
        for e in ENGS:
            t = {k: v for k, v in toks.items() if not (k[0] == "c" and k[1] == e)}
            self._wait(e, t)

    def final_wait(self, eng="sp"):
        toks = {}
        for q in DMA_ENGS:
            for slot, val in self.dlast[q].items():
                toks[("d", q, slot)] = val
        for e in ENGS:
            if self.cnt[e] > 0 and e != eng:
                toks[("c", e)] = self.cnt[e]
        self._wait(eng, toks)


def build(NSEQ=4, debug=False, n_experts=32, phases=("attn", "moe"), nd_sb=0, nd_mla=0):
    nc = bass.Bass("TRN2", target_bir_lowering=False)

    def din(name, shape, dtype=F32):
        return nc.dram_tensor(name, shape, dtype, kind="ExternalInput").ap()

    x = din("x", [NSEQ, 2048, 1024])
    positions = din("positions", [NSEQ, 2048], I32)
    attn_norm = din("attn_norm", [1, 1024])
    w_in = din("w_in", [1024, 1952])
    q_norm = din("q_norm", [256, 1])
    w_uq = din("w_uq", [256, 768])
    kv_norm = din("kv_norm", [128, 1])
    w_ukv = din("w_ukv", [128, 1024])
    out_norm = din("out_norm", [1024, 1])
    w_out = din("w_out", [1024, 1024])
    ffn_norm = din("ffn_norm", [1, 1024])
    w_gr = din("w_group_router", [1024, 4])
    b_gr = din("b_group_router", [1, 4])
    w_er = din("w_expert_router", [1024, 32])
    b_er = din("b_expert_router", [1, 32])
    w_gate = din("w_gate", [32, 1024, 256])
    w_up = din("w_up", [32, 1024, 256])
    w_down = din("w_down", [32, 256, 1024])
    final_norm = din("final_norm", [1, 1024])
    consts = din("consts", [128, 640], BF16)
    invf = din("invf", [128, 1])
    y = nc.dram_tensor("y", [NSEQ, 2048, 1024], F32, kind="ExternalOutput").ap()
    x1s = nc.dram_tensor("x1s", [NSEQ, 2048, 1024], F32,
                         kind="ExternalOutput" if debug else "Internal").ap()

    dbg_oT = nc.dram_tensor("dbg_oT", [128, 8, 2048], BF16, kind="ExternalOutput").ap() if debug else None
    dbg_hT = nc.dram_tensor("dbg_hT", [128, 8, 2048], BF16, kind="ExternalOutput").ap() if debug else None
    dbg_lat = nc.dram_tensor("dbg_lat", [128, 4, 2048], BF16, kind="ExternalOutput").ap() if debug else None

    with ExitStack() as es:
        S = Sched(nc, es)

        uid = [0]

        def sb(stack, name, shape, dtype=F32):
            uid[0] += 1
            return stack.enter_context(nc.sbuf_tensor(f"{name}_{uid[0]}", shape, dtype))

        PS = [es.enter_context(nc.psum_tensor(f"ps{i}", [128, 1024], F32)) for i in range(4)]
        PSB = [p[:].bitcast(BF16) for p in PS]

        def bank(k):
            return PS[k // 2][:, (k % 2) * 512:(k % 2) * 512 + 512]

        def bank_bf(k):
            return PSB[k // 2][:, (k % 2) * 1024:(k % 2) * 1024 + 1024]

        def bk(k):
            return f"B{k}"

        cst = sb(es, "cst", [128, 640], BF16)
        ident = cst[:, 0:128]
        tri = cst[:, 128:256]
        mstrict = cst[:, 256:384]
        mcausal = cst[:, 384:512]
        slow = cst[:, 512:640]
        ones_bf = sb(es, "ones_bf", [128, 128], BF16)
        ones_f = sb(es, "ones_f", [128, 64], F32)
        epsc = sb(es, "epsc", [128, 1])
        onec = sb(es, "onec", [128, 1])
        invf_sb = sb(es, "invf_sb", [128, 1])
        gbc = sb(es, "gbc", [128, 1024])
        w_uq_sb = sb(es, "w_uq_sb", [128, 2, 8, 96], BF16)
        w2_sb = sb(es, "w2_sb", [128, 2, 8, 96], BF16)
        w_ukv_sb = sb(es, "w_ukv_sb", [128, 1024], BF16)
        wv_sb = sb(es, "wv_sb", [128, 8, 64], BF16)
        wkr = sb(es, "wkr", [128, 8, 96], BF16)
        wkr2 = sb(es, "wkr2", [128, 8, 96], BF16)
        w_out_sb = sb(es, "w_out_sb", [128, 8, 1024], BF16)
        wr_sb = sb(es, "wr_sb", [128, 8, 36], BF16)
        br_bc = sb(es, "br_bc", [128, 36])
        hT = sb(es, "hT", [128, 8, 2048], BF16)
        Wfull = sb(es, "Wfull", [128, 16, 32])
        rs2all = sb(es, "rs2all", [128, 16])

        S.dma("sp", lambda e: e.dma_start(out=cst[:], in_=consts[:, :]), writes=["cst"])
        S.dma("sp", lambda e: e.dma_start(out=invf_sb[:], in_=invf[:, :]), writes=["invf"])
        S.op("pool", lambda e: e.memset(ones_bf[:], 1.0), writes=["ones_bf"])
        S.op("pool", lambda e: e.memset(ones_f[:], 1.0), writes=["ones_f"])
        S.op("pool", lambda e: e.memset(epsc[:], EPS), writes=["epsc"])
        S.op("pool", lambda e: e.memset(onec[:], 1.0), writes=["onec"])
        S.op("pool", lambda e: e.memset(w2_sb[:], 0.0), writes=["w2"])
        S.op("pool", lambda e: e.memset(wkr[:], 0.0), writes=["wkr"])
        S.op("pool", lambda e: e.memset(wkr2[:], 0.0), writes=["wkr2"])
        with ExitStack() as ss:
            stg = sb(ss, "stg", [128, 2048])
            qn = sb(ss, "qn", [128, 2])
            kvn = sb(ss, "kvn", [128, 1])
            og = sb(ss, "og", [128, 8])
            S.dma("sp", lambda e: e.dma_start(out=stg[:, 0:1536].rearrange("p (c n) -> p c n", c=2),
                                              in_=w_uq.rearrange("(c p) n -> p c n", p=128)), writes=["stg"])
            for c in range(2):
                S.dma("sp", lambda e: e.dma_start(out=qn[:, c:c + 1], in_=q_norm[c * 128:(c + 1) * 128, :]), writes=["qn"])
            stv = stg[:, 0:1536].rearrange("p (c h d) -> p c h d", c=2, h=8)
            for c in range(2):
                S.op("dve", lambda e: e.tensor_scalar(w_uq_sb[:, c], stv[:, c], qn[:, c:c + 1], None, ALU.mult),
                     reads=["stg", "qn"], writes=["w_uq"])
                S.op("dve", lambda e: e.tensor_scalar(w2_sb[:, c, :, 64:80], stv[:, c, :, 80:96], qn[:, c:c + 1], -1.0,
                                                      ALU.mult, ALU.mult), reads=["stg", "qn"], writes=["w2"])
                S.op("dve", lambda e: e.tensor_scalar(w2_sb[:, c, :, 80:96], stv[:, c, :, 64:80], qn[:, c:c + 1], None,
                                                      ALU.mult), reads=["stg", "qn"], writes=["w2"])
            S.dma("sp", lambda e: e.dma_start(out=stg[:, 0:1024], in_=w_ukv[:, :]), writes=["stg"])
            S.dma("sp", lambda e: e.dma_start(out=kvn[:], in_=kv_norm[:, :]), writes=["kvn"])
            S.op("dve", lambda e: e.tensor_scalar(w_ukv_sb[:], stg[:, 0:1024], kvn[:, 0:1], None, ALU.mult),
                 reads=["stg", "kvn"], writes=["w_ukv"])
            S.op("dve", lambda e: e.tensor_scalar(wv_sb[:], stg[:, 0:1024].rearrange("p (h d) -> p h d", h=8)[:, :, 64:128],
                                                  kvn[:, 0:1], None, ALU.mult), reads=["stg", "kvn"], writes=["wv"])
            wi_c = w_in.rearrange("(c p) n -> p c n", p=128)
            S.dma("pool", lambda e: e.dma_start(out=wkr[:, :, 64:96], in_=wi_c[:, :, 1920:1952]), writes=["wkr"])
            S.dma("pool", lambda e: e.dma_start(out=wkr2[:, :, 80:96], in_=wi_c[:, :, 1920:1936]), writes=["wkr2"])
            S.dma("pool", lambda e: e.dma_start(out=wkr2[:, :, 64:80], in_=wi_c[:, :, 1936:1952]), writes=["wkr2"])
            S.op("pool", lambda e: e.tensor_scalar(wkr2[:, :, 64:80], wkr2[:, :, 64:80], -1.0, None, ALU.mult),
                 reads=["wkr2"], writes=["wkr2"])
            for j in range(8):
                S.dma("sp", lambda e: e.dma_start(out=og[:, j:j + 1], in_=out_norm[j * 128:(j + 1) * 128, :]), writes=["og"])
            wo_c = w_out.rearrange("(c p) n -> p c n", p=128)
            for jj in range(4):
                S.dma("sp", lambda e: e.dma_start(out=stg[:].rearrange("p (c n) -> p c n", c=2),
                                                  in_=wo_c[:, 2 * jj:2 * jj + 2, :]), writes=["stg"])
                for jl in range(2):
                    j = 2 * jj + jl
                    S.op("dve", lambda e: e.tensor_scalar(w_out_sb[:, j, :], stg[:, jl * 1024:(jl + 1) * 1024],
                                                          og[:, j:j + 1], None, ALU.mult),
                         reads=["stg", "og"], writes=["w_out"])
            S.dma("pool", lambda e: e.dma_start(out=wr_sb[:, :, 0:4], in_=w_gr.rearrange("(c p) n -> p c n", p=128)), writes=["wr"])
            S.dma("pool", lambda e: e.dma_start(out=wr_sb[:, :, 4:36], in_=w_er.rearrange("(c p) n -> p c n", p=128)), writes=["wr"])
            S.dma("sp", lambda e: e.dma_start(out=br_bc[:, 0:4], in_=b_gr[0:1, :].partition_broadcast(128)), writes=["br"])
            S.dma("sp", lambda e: e.dma_start(out=br_bc[:, 4:36], in_=b_er[0:1, :].partition_broadcast(128)), writes=["br"])
            S.barrier()

        def rstd_from_ss(rs, ss_ap, n, rkey, sskey):
            S.op("act", lambda e: e.activation(rs, ss_ap, AF.Sqrt, bias=epsc[:], scale=1.0 / n),
                 reads=[sskey, "epsc"], writes=[rkey])
            S.op("dve", lambda e: e.reciprocal(rs, rs), reads=[rkey], writes=[rkey])

        def transpose_to_hT(src_bf, srckey, t, bnk):
            pb = bank_bf(bnk)
            for c in range(8):
                S.op("pe", lambda e: e.transpose(pb[:, c * 128:(c + 1) * 128], src_bf[:, c * 128:(c + 1) * 128], ident),
                     reads=[srckey, "cst", ("hTt", t)] if c == 0 else [srckey, "cst"], writes=[bk(bnk)])
            S.op("act", lambda e: e.copy(hT[:, :, t * 128:(t + 1) * 128], pb.rearrange("p (c n) -> p c n", c=8)),
                 reads=[bk(bnk)], writes=[("hT", t // 4), ("hTt", t)])

        for b in range(NSEQ):
            if "attn" in phases:
              with ExitStack() as s1:
                cqTn = sb(s1, "cqTn", [128, 2, 2048], BF16)
                ckvTn = sb(s1, "ckvTn", [128, 2048], BF16)
                krope = sb(s1, "krope", [128, 2048], BF16)
                oT = sb(s1, "oT", [128, 8, 2048], BF16)
                sinT = sb(s1, "sinT", [128, 2048])
                cosT = sb(s1, "cosT", [128, 2048])

                with ExitStack() as sa:
                    xts = [sb(sa, f"xt{i}", [128, 1024]) for i in range(2)]
                    hbs = [sb(sa, f"hb{i}", [128, 1024], BF16) for i in range(2)]
                    junk = sb(sa, "junk", [128, 1024], BF16)
                    ssA = sb(sa, "ssA", [128, 2])
                    rsA = sb(sa, "rsA", [128, 2])
                    wlat = sb(sa, "wlat", [128, 8, 384], BF16)
                    latf = sb(sa, "latf", [128, 3, 512])
                    latsq = sb(sa, "latsq", [128, 3, 512], BF16)
                    rbc = sb(sa, "rbc", [128, 2, 512])
                    posi = sb(sa, "posi", [128, 512], I32)
                    ang = sb(sa, "ang", [128, 512])
                    rtmp = sb(sa, "rtmp", [128, 512])
                    ru = sb(sa, "ru", [128, 512])
                    ta = sb(sa, "ta", [128, 512])
                    tb = sb(sa, "tb", [128, 512])

                    S.dma("sp", lambda e: e.dma_start(out=gbc[:], in_=attn_norm[0:1, :].partition_broadcast(128)), writes=["gbc"])
                    S.dma("pool", lambda e: e.dma_start(out=wlat[:], in_=w_in.rearrange("(c p) n -> p c n", p=128)[:, :, 1536:1920]),
                          writes=["wlat"])
                    for t in range(16):
                        xt = xts[t % 2]
                        hb = hbs[t % 2]
                        xk, hk = f"xt{t % 2}", f"hb{t % 2}"
                        sl = t % 2
                        S.dma("sp", lambda e: e.dma_start(out=xt[:], in_=x[b, t * 128:(t + 1) * 128, :]), writes=[xk])
                        S.op("dve", lambda e: e.memset(ssA[:, sl:sl + 1], 0.0), writes=[f"ssA{sl}"])
                        S.op("act", lambda e: e.activation(junk[:], xt[:], AF.Square, accum_out=ssA[:, sl:sl + 1]),
                             reads=[xk], writes=["junk", f"ssA{sl}"])
                        rstd_from_ss(rsA[:, sl:sl + 1], ssA[:, sl:sl + 1], 1024.0, f"rsA{sl}", f"ssA{sl}")
                        S.op("dve", lambda e: e.scalar_tensor_tensor(hb[:], xt[:], rsA[:, sl:sl + 1], gbc[:], ALU.mult, ALU.mult),
                             reads=[xk, f"rsA{sl}", "gbc"], writes=[hk])
                        transpose_to_hT(hb, hk, t, t % 2)

                    for G in range(4):
                        gs = slice(G * 512, (G + 1) * 512)
                        hTk = ("hT", G)
                        for lc in range(3):
                            bn = 2 + lc
                            for c in range(8):
                                S.op("pe", lambda e: e.matmul(bank(bn), wlat[:, c, lc * 128:(lc + 1) * 128], hT[:, c, gs],
                                                              start=(c == 0), stop=(c == 7)),
                                     reads=["wlat", hTk], writes=[bk(bn)])
                            S.op("act", lambda e: e.copy(latf[:, lc, :], bank(bn)), reads=[bk(bn)], writes=[("latf", lc)])
                            S.op("dve", lambda e: e.tensor_tensor(latsq[:, lc, :], latf[:, lc, :], latf[:, lc, :], ALU.mult),
                                 reads=[("latf", lc)], writes=[("latsq", lc)])
                        S.op("pe", lambda e: e.matmul(bank(5), ones_bf[:], latsq[:, 0, :], start=True, stop=False),
                             reads=["ones_bf", ("latsq", 0)], writes=[bk(5)])
                        S.op("pe", lambda e: e.matmul(bank(5), ones_bf[:], latsq[:, 1, :], start=False, stop=True),
                             reads=["ones_bf", ("latsq", 1)], writes=[bk(5)])
                        S.op("pe", lambda e: e.matmul(bank(6), ones_bf[:], latsq[:, 2, :], start=True, stop=True),
                             reads=["ones_bf", ("latsq", 2)], writes=[bk(6)])
                        for (ri, bnk_, nn) in ((0, 5, 256.0), (1, 6, 128.0)):
                            S.op("act", lambda e: e.activation(rbc[:, ri, :], bank(bnk_), AF.Ln, bias=epsc[:], scale=1.0 / nn),
                                 reads=[bk(bnk_), "epsc"], writes=[("rbc", ri)])
                            S.op("act", lambda e: e.activation(rbc[:, ri, :], rbc[:, ri, :], AF.Exp, scale=-0.5),
                                 reads=[("rbc", ri)], writes=[("rbc", ri)])
                        for lc in range(2):
                            S.op("dve", lambda e: e.tensor_tensor(cqTn[:, lc, gs], latf[:, lc, :], rbc[:, 0, :], ALU.mult),
                                 reads=[("latf", lc), ("rbc", 0)], writes=["cqTn"])
                        S.op("dve", lambda e: e.tensor_tensor(ckvTn[:, gs], latf[:, 2, :], rbc[:, 1, :], ALU.mult),
                             reads=[("latf", 2), ("rbc", 1)], writes=["ckvTn"])
                        P = slice(64, 96)
                        S.dma("sp", lambda e: e.dma_start(out=posi[P, :], in_=positions[b:b + 1, gs].partition_broadcast(32)),
                              writes=["posi"])
                        S.op("dve", lambda e: e.tensor_copy(ang[P, :], posi[P, :]), reads=["posi"], writes=["ang"])
                        S.op("dve", lambda e: e.tensor_scalar(ang[P, :], ang[P, :], invf_sb[P, 0:1], None, ALU.mult),
                             reads=["ang", "invf"], writes=["ang"])
                        for (dst, dk, add) in ((sinT, "sinT", 0.0), (cosT, "cosT", float(np.pi / 2))):
                            S.op("dve", lambda e: e.tensor_scalar(ru[P, :], ang[P, :], add, None, ALU.add),
                                 reads=["ang"], writes=["ru"])
                            S.op("dve", lambda e: e.tensor_scalar(rtmp[P, :], ru[P, :], 1.0 / TWO_PI, None, ALU.mult),
                                 reads=["ru"], writes=["rtmp"])
                            S.op("dve", lambda e: e.tensor_copy(posi[P, :], rtmp[P, :]), reads=["rtmp"], writes=["posi"])
                            S.op("dve", lambda e: e.tensor_copy(rtmp[P, :], posi[P, :]), reads=["posi"], writes=["rtmp"])
                            S.op("dve", lambda e: e.scalar_tensor_tensor(rtmp[P, :], rtmp[P, :], -TWO_PI, ru[P, :], ALU.mult, ALU.add),
                                 reads=["rtmp", "ru"], writes=["rtmp"])
                            S.op("dve", lambda e: e.tensor_scalar(rtmp[P, :], rtmp[P, :], float(np.pi), float(-np.pi), ALU.min, ALU.max),
                                 reads=["rtmp"], writes=["rtmp"])
                            S.op("act", lambda e: e.activation(dst[P, gs], rtmp[P, :], AF.Sin), reads=["rtmp"], writes=[(dk, G)])
                        for (wt, wk, bn) in ((wkr, "wkr", 7), (wkr2, "wkr2", 5)):
                            for c in range(8):
                                S.op("pe", lambda e: e.matmul(bank(bn)[0:96, :], wt[:, c, :], hT[:, c, gs], start=(c == 0), stop=(c == 7)),
                                     reads=[wk, hTk], writes=[bk(bn)])
                        S.op("dve", lambda e: e.tensor_tensor(ta[P, :], bank(7)[P, :], cosT[P, gs], ALU.mult),
                             reads=[bk(7), ("cosT", G)], writes=["ta"])
                        S.op("dve", lambda e: e.tensor_tensor(tb[P, :], bank(5)[P, :], sinT[P, gs], ALU.mult),
                             reads=[bk(5), ("sinT", G)], writes=["tb"])
                        S.op("dve", lambda e: e.tensor_tensor(krope[P, gs], ta[P, :], tb[P, :], ALU.add),
                             reads=["ta", "tb"], writes=["krope"])
                    S.barrier()

                if debug and b == 0:
                    S.dma("sp", lambda e: e.dma_start(out=dbg_hT[:, :, :], in_=hT[:]), reads=[("hT", g) for g in range(4)], writes=["dbg_hT"])
                    S.barrier()
                def run_passes(proj_chunks, loop_iters, npass):
                    for ch in proj_chunks(0):
                        ch()
                    for j in range(npass):
                        nxt = S.record(proj_chunks(j + 1)) if j + 1 < npass else []
                        iters = loop_iters(j)
                        per = -(-len(nxt) // max(1, len(iters) - 6))
                        for idx, it in enumerate(iters):
                            it()
                            if idx >= 2:
                                for _ in range(per):
                                    if nxt:
                                        nxt.pop(0)()
                        while nxt:
                            nxt.pop(0)()

                with ExitStack() as sp_:
                    wsb2 = [sb(sp_, f"wsb{i}", [128, 8, 3, 128], BF16) for i in range(2)]
                    qs02 = [[sb(sp_, f"qs0_{i}_{h}", [128, 2048], BF16) for h in range(2)] for i in range(2)]
                    ksT2 = [sb(sp_, f"ksT{i}", [128, 2048], BF16) for i in range(2)]
                    vs2 = [sb(sp_, f"vs{i}", [128, 16, 128], BF16) for i in range(2)]
                    E1 = [sb(sp_, f"E1_{i}", [128, 512]) for i in range(4)]
                    Lb = [sb(sp_, f"Lb_{i}", [128, 512], BF16) for i in range(2)]
                    Xe = [sb(sp_, f"Xe_{i}", [128, 512]) for i in range(2)]
                    At = [sb(sp_, f"At_{i}", [128, 512], BF16) for i in range(2)]
                    S32 = [sb(sp_, f"S32_{i}", [128, 512]) for i in range(2)]
                    Sb = [sb(sp_, f"Sb_{i}", [128, 512], BF16) for i in range(2)]
                    wi_c = w_in.rearrange("(c p) n -> p c n", p=128)

                    def sb_proj_chunks(j):
                        pj = j % 2
                        wsb, qs0, ksT, vs = wsb2[pj], qs02[pj], ksT2[pj], vs2[pj]
                        chunks = []

                        def c_load():
                            for w in range(3):
                                S.dma("pool", lambda e: e.dma_start(out=wsb[:, :, w, :],
                                                                    in_=wi_c[:, :, w * 512 + j * 128:w * 512 + (j + 1) * 128]),
                                      writes=[("wsb", pj, w)])
                            S.op("dve", lambda e: e.memset(qs0[0][64:128, :], 0.0), writes=[("qs0", pj, 0, G) for G in range(4)])
                            S.op("dve", lambda e: e.memset(qs0[1][0:64, :], 0.0), writes=[("qs0", pj, 1, G) for G in range(4)])
                        chunks.append(c_load)
                        for G in range(4):
                            gs = slice(G * 512, (G + 1) * 512)

                            def c_q(G=G, gs=gs):
                                for c in range(8):
                                    S.op("pe", lambda e: e.matmul(bank(6), wsb[:, c, 0, :], hT[:, c, gs], start=(c == 0), stop=(c == 7)),
                                         reads=[("wsb", pj, 0), ("hT", G)], writes=[bk(6)])
                                S.op("dve", lambda e: e.tensor_scalar(qs0[0][0:64, gs], bank(6)[0:64, :], 0.125, None, ALU.mult),
                                     reads=[bk(6)], writes=[("qs0", pj, 0, G)])
                                S.op("dve", lambda e: e.tensor_scalar(qs0[1][64:128, gs], bank(6)[64:128, :], 0.125, None, ALU.mult),
                                     reads=[bk(6)], writes=[("qs0", pj, 1, G)])

                            def c_k(G=G, gs=gs):
                                for c in range(8):
                                    S.op("pe", lambda e: e.matmul(bank(7), wsb[:, c, 1, :], hT[:, c, gs], start=(c == 0), stop=(c == 7)),
                                         reads=[("wsb", pj, 1), ("hT", G)], writes=[bk(7)])
                                S.op("dve", lambda e: e.tensor_copy(ksT[:, gs], bank(7)), reads=[bk(7)], writes=[("ksT", pj, G)])

                            def c_v(G=G):
                                bn = 6 + (G % 2)
                                for tl in range(4):
                                    t = G * 4 + tl
                                    for c in range(8):
                                        S.op("pe", lambda e: e.matmul(bank(bn)[:, tl * 128:(tl + 1) * 128], hT[:, c, t * 128:(t + 1) * 128],
                                                                      wsb[:, c, 2, :], start=(c == 0), stop=(c == 7)),
                                             reads=[("wsb", pj, 2), ("hT", G)], writes=[bk(bn)])
                                S.op("dve", lambda e: e.tensor_copy(vs[:, G * 4:(G + 1) * 4, :], bank(bn).rearrange("p (t n) -> p t n", t=4)),
                                     reads=[bk(bn)], writes=[("vs", pj, G)])
                            chunks += [c_q, c_k, c_v]
                        return chunks

                    def sb_loop_iters(j):
                        pj = j % 2
                        qs0, ksT, vs = qs02[pj], ksT2[pj], vs2[pj]
                        units = [(hl, G, i) for G in range(4) for i in range(4 * G + 3, -1, -1) for hl in range(2)]
                        nU = len(units)

                        def geom(u):
                            hl, G, i = units[u]
                            q0 = max(i, 4 * G) * 128
                            off = q0 - G * 512
                            return dict(hl=hl, G=G, i=i, off=off, cs=slice(off, 512), qsl=slice(q0, (G + 1) * 512),
                                        ksl=slice(i * 128, (i + 1) * 128), diag=(i >= 4 * G), pr=slice(hl * 64, (hl + 1) * 64),
                                        first=(i == 4 * G + 3), last=(i == 0), gs=slice(G * 512, (G + 1) * 512))

                        def st0(u):
                            g = geom(u)
                            zb = u % 2
                            S.op("pe", lambda e: e.matmul(bank(zb)[:, g["cs"]], ksT[:, g["ksl"]], qs0[g["hl"]][:, g["qsl"]], start=True, stop=True),
                                 reads=[("ksT", pj, g["i"] // 4), ("qs0", pj, g["hl"], g["G"])], writes=[bk(zb)])

                        def st1a(u):
                            g = geom(u)
                            zb, cs, off = u % 2, g["cs"], g["off"]
                            e1, e1k = E1[u % 4], f"E1_{u % 4}"
                            S.op("act", lambda e: e.activation(e1[:, cs], bank(zb)[:, cs], AF.Exp), reads=[bk(zb)], writes=[e1k])
                            if g["diag"]:
                                S.op("dve", lambda e: e.tensor_tensor(e1[:, off:off + 128], e1[:, off:off + 128], mstrict, ALU.mult),
                                     reads=[e1k, "cst"], writes=[e1k])

                        def st1b(u):
                            g = geom(u)
                            cs = g["cs"]
                            e1, lb = E1[u % 4], Lb[u % 2]
                            e1k, lbk = f"E1_{u % 4}", f"Lb_{u % 2}"
                            S.op("act", lambda e: e.activation(lb[:, cs], e1[:, cs], AF.Ln, bias=onec[:], scale=1.0),
                                 reads=[e1k, "onec"], writes=[lbk])

                        def st2a(u):
                            g = geom(u)
                            cs, hl = g["cs"], g["hl"]
                            cbk = 2 + u % 2
                            lb, xe = Lb[u % 2], Xe[u % 2]
                            lbk, xek = f"Lb_{u % 2}", f"Xe_{u % 2}"
                            S.op("pe", lambda e: e.matmul(bank(cbk)[:, cs], tri, lb[:, cs], start=True, stop=g["first"]),
                                 reads=["cst", lbk], writes=[bk(cbk)])
                            if not g["first"]:
                                S.op("pe", lambda e: e.matmul(bank(cbk)[:, cs], ones_bf[:], Sb[hl][:, cs], start=False, stop=True),
                                     reads=["ones_bf", f"Sb_{hl}"], writes=[bk(cbk)])
                            S.op("act", lambda e: e.activation(xe[:, cs], bank(cbk)[:, cs], AF.Exp, scale=-1.0), reads=[bk(cbk)], writes=[xek])

                        def st2b(u):
                            g = geom(u)
                            cs, hl = g["cs"], g["hl"]
                            e1, lb, xe, at = E1[u % 4], Lb[u % 2], Xe[u % 2], At[u % 2]
                            e1k, lbk, xek, atk = f"E1_{u % 4}", f"Lb_{u % 2}", f"Xe_{u % 2}", f"At_{u % 2}"
                            if not g["last"]:
                                if g["first"]:
                                    S.op("dve", lambda e: e.memset(S32[hl][:], 0.0), writes=[f"S32_{hl}"])
                                S.op("dve", lambda e: e.tensor_tensor(S32[hl][:, cs], S32[hl][:, cs], lb[:, cs], ALU.add),
                                     reads=[f"S32_{hl}", lbk], writes=[f"S32_{hl}"])
                            S.op("dve", lambda e: e.tensor_tensor(at[:, cs], xe[:, cs], e1[:, cs], ALU.mult), reads=[xek, e1k], writes=[atk])
                            if not g["last"]:
                                S.op("dve", lambda e: e.tensor_copy(Sb[hl][:], S32[hl][:]), reads=[f"S32_{hl}"], writes=[f"Sb_{hl}"])

                        def st3(u):
                            g = geom(u)
                            cs = g["cs"]
                            ob = 4 + g["hl"]
                            at, atk = At[u % 2], f"At_{u % 2}"
                            S.op("pe", lambda e: e.matmul(bank(ob)[0:64, cs], vs[:, g["i"], g["pr"]], at[:, cs], start=g["first"], stop=g["last"],
                                                          skip_group_check=True),
                                 reads=[("vs", pj, g["i"] // 4), atk], writes=[bk(ob)])
                            if g["last"]:
                                S.op("act", lambda e: e.copy(oT[g["pr"], j, g["gs"]], bank(ob)[0:64, :]), reads=[bk(ob)], writes=[("oT", j)])

                        def mk(k):
                            def it():
                                if 0 <= k < nU:
                                    st1b(k)
                                if 0 <= k + 1 < nU:
                                    st1a(k + 1)
                                if 0 <= k - 1 < nU:
                                    st2a(k - 1)
                                if 0 <= k + 2 < nU:
                                    st0(k + 2)
                                if 0 <= k - 1 < nU:
                                    st2b(k - 1)
                                if 0 <= k - 2 < nU:
                                    st3(k - 2)
                            return it
                        return [mk(k) for k in range(-2, nU + 2)]

                    run_passes(sb_proj_chunks, sb_loop_iters, 4)
                    S.barrier()

                with ExitStack() as sm:
                    qmT2 = [sb(sm, f"qmT{i}", [128, 2, 2048], BF16) for i in range(2)]
                    kmT2 = [sb(sm, f"kmT{i}", [128, 2, 2048], BF16) for i in range(2)]
                    vm2 = [sb(sm, f"vm{i}", [128, 16, 2, 65], BF16) for i in range(2)]
                    Et = [sb(sm, f"Et_{i}", [128, 512], BF16) for i in range(3)]
                    dr = sb(sm, "dr", [128, 512])
                    rb = sb(sm, "rb", [128, 512])
                    ta = sb(sm, "ta", [128, 512])
                    tb = sb(sm, "tb", [128, 512])
                    P = slice(64, 96)
                    scale = float(96 ** -0.5)

                    def mla_proj_chunks(m):
                        pm = m % 2
                        qmT, kmT, vm = qmT2[pm], kmT2[pm], vm2[pm]
                        chunks = []

                        def c_init():
                            S.op("dve", lambda e: e.memset(vm[:], 1.0), writes=[("vm", pm, G) for G in range(4)])
                            S.op("dve", lambda e: e.memset(qmT[96:128], 0.0), writes=[("qmT", pm, G) for G in range(4)])
                            S.op("dve", lambda e: e.memset(kmT[96:128], 0.0), writes=[("kmT", pm, G) for G in range(4)])
                        chunks.append(c_init)
                        for G in range(4):
                            gs = slice(G * 512, (G + 1) * 512)
                            for hl in range(2):
                                def c_qk(G=G, gs=gs, hl=hl):
                                    h = 2 * m + hl
                                    for (wt, wk, bn) in ((w_uq_sb, "w_uq", 6), (w2_sb, "w2", 7)):
                                        for c in range(2):
                                            S.op("pe", lambda e: e.matmul(bank(bn)[0:96, :], wt[:, c, h, :], cqTn[:, c, gs], start=(c == 0), stop=(c == 1)),
                                                 reads=[wk, "cqTn"], writes=[bk(bn)])
                                    S.op("dve", lambda e: e.tensor_copy(qmT[0:64, hl, gs], bank(6)[0:64, :]), reads=[bk(6)], writes=[("qmT", pm, G)])
                                    S.op("dve", lambda e: e.tensor_tensor(ta[P, :], bank(6)[P, :], cosT[P, gs], ALU.mult),
                                         reads=[bk(6), ("cosT", G)], writes=["ta"])
                                    S.op("dve", lambda e: e.tensor_tensor(tb[P, :], bank(7)[P, :], sinT[P, gs], ALU.mult),
                                         reads=[bk(7), ("sinT", G)], writes=["tb"])
                                    S.op("dve", lambda e: e.tensor_tensor(qmT[P, hl, gs], ta[P, :], tb[P, :], ALU.add),
                                         reads=["ta", "tb"], writes=[("qmT", pm, G)])
                                    S.op("pe", lambda e: e.matmul(bank(6)[0:64, :], w_ukv_sb[:, h * 128:h * 128 + 64], ckvTn[:, gs], start=True, stop=True),
                                         reads=["w_ukv", "ckvTn"], writes=[bk(6)])
                                    S.op("dve", lambda e: e.tensor_copy(kmT[0:64, hl, gs], bank(6)[0:64, :]), reads=[bk(6)], writes=[("kmT", pm, G)])
                                    S.op("dve", lambda e: e.tensor_copy(kmT[P, hl, gs], krope[P, gs]), reads=["krope"], writes=[("kmT", pm, G)])
                                chunks.append(c_qk)

                            def c_v(G=G):
                                bn = 7
                                for tl in range(4):
                                    t = G * 4 + tl
                                    S.op("pe", lambda e: e.matmul(bank(bn)[:, tl * 128:(tl + 1) * 128], ckvTn[:, t * 128:(t + 1) * 128],
                                                                  wv_sb[:, 2 * m:2 * m + 2, :], start=True, stop=True),
                                         reads=["wv", "ckvTn"], writes=[bk(bn)])
                                S.op("dve", lambda e: e.tensor_copy(vm[:, G * 4:(G + 1) * 4, :, 0:64],
                                                                    bank(bn).rearrange("p (t h d) -> p t h d", t=4, h=2)),
                                     reads=[bk(bn)], writes=[("vm", pm, G)])
                            chunks.append(c_v)
                        return chunks

                    def mla_loop_iters(m):
                        pm = m % 2
                        qmT, kmT, vm = qmT2[pm], kmT2[pm], vm2[pm]
                        units = [(hl, G, i) for G in range(4) for i in range(0, 4 * G + 4) for hl in range(2)]
                        nU = len(units)

                        def geom(u):
                            hl, G, i = units[u]
                            q0 = max(i, 4 * G) * 128
                            off = q0 - G * 512
                            return dict(hl=hl, G=G, i=i, off=off, cs=slice(off, 512), qsl=slice(q0, (G + 1) * 512),
                                        ksl=slice(i * 128, (i + 1) * 128), diag=(i >= 4 * G), pr=slice(hl * 64, (hl + 1) * 64),
                                        first=(i == 0), last=(i == 4 * G + 3), gs=slice(G * 512, (G + 1) * 512))

                        def st0(u):
                            g = geom(u)
                            zb = u % 3
                            S.op("pe", lambda e: e.matmul(bank(zb)[:, g["cs"]], kmT[:, g["hl"], g["ksl"]], qmT[:, g["hl"], g["qsl"]],
                                                          start=True, stop=True),
                                 reads=[("kmT", pm, g["i"] // 4), ("qmT", pm, g["G"])], writes=[bk(zb)])

                        def st1(u):
                            g = geom(u)
                            zb, cs, off = u % 3, g["cs"], g["off"]
                            et, etk = Et[u % 3], f"Et_{u % 3}"
                            S.op("act", lambda e: e.activation(et[:, cs], bank(zb)[:, cs], AF.Exp, scale=scale), reads=[bk(zb)], writes=[etk])
                            if g["diag"]:
                                S.op("dve", lambda e: e.tensor_tensor(et[:, off:off + 128], et[:, off:off + 128], mcausal, ALU.mult),
                                     reads=[etk, "cst"], writes=[etk])

                        def st2(u):
                            g = geom(u)
                            cs = g["cs"]
                            ob = 4 + g["hl"]
                            et, etk = Et[u % 3], f"Et_{u % 3}"
                            S.op("pe", lambda e: e.matmul(bank(ob)[0:65, cs], vm[:, g["i"], g["hl"], :], et[:, cs], start=g["first"], stop=g["last"],
                                                          skip_group_check=True),
                                 reads=[("vm", pm, g["i"] // 4), etk], writes=[bk(ob)])
                            if g["last"]:
                                S.op("act", lambda e: e.activation(dr[64:65, :], bank(ob)[64:65, :], AF.Ln), reads=[bk(ob)], writes=["dr"])

                                def fin(ob=ob, pr=g["pr"], gs=g["gs"]):
                                    S.op("act", lambda e: e.activation(dr[64:65, :], dr[64:65, :], AF.Exp, scale=-1.0), reads=["dr"], writes=["dr"])
                                    S.op("pe", lambda e: e.matmul(bank(3)[0:64, :], ones_f[64:65, 0:64], dr[64:65, :], start=True, stop=True),
                                         reads=["ones_f", "dr"], writes=[bk(3)])
                                    S.op("act", lambda e: e.copy(rb[0:64, :], bank(3)[0:64, :]), reads=[bk(3)], writes=["rb"])
                                    S.op("dve", lambda e: e.tensor_tensor(oT[pr, 4 + m, gs], bank(ob)[0:64, :], rb[0:64, :], ALU.mult),
                                         reads=[bk(ob), "rb"], writes=[("oT", 4 + m)])
                                deferred.append(fin)

                        deferred = []

                        def mk(k):
                            def it():
                                if 0 <= k + 1 < nU:
                                    st0(k + 1)
                                if 0 <= k < nU:
                                    st1(k)
                                pend = list(deferred)
                                del deferred[:]
                                for f in pend:
                                    f()
                                if 0 <= k - 1 < nU:
                                    st2(k - 1)
                                if k >= nU:
                                    for f in list(deferred):
                                        f()
                                    del deferred[:]
                            return it
                        return [mk(k) for k in range(-1, nU + 1)]

                    run_passes(mla_proj_chunks, mla_loop_iters, 4)
                    S.barrier()

                if debug and b == 0:
                    S.dma("sp", lambda e: e.dma_start(out=dbg_oT[:, :, :], in_=oT[:]), reads=[("oT", jj) for jj in range(8)], writes=["dbg_oT"])
                    S.dma("sp", lambda e: e.dma_start(out=dbg_lat[:, 0:2, :], in_=cqTn[:]), reads=["cqTn"], writes=["dbg_lat0"])
                    S.dma("sp", lambda e: e.dma_start(out=dbg_lat[:, 2, :], in_=ckvTn[:]), reads=["ckvTn"], writes=["dbg_lat1"])
                    S.dma("sp", lambda e: e.dma_start(out=dbg_lat[64:96, 3, :], in_=krope[64:96, :]), reads=["krope"], writes=["dbg_lat2"])
                    S.barrier()
                with ExitStack() as sd:
                    xts = [sb(sd, f"xd{i}", [128, 1024]) for i in range(2)]
                    x1t = [sb(sd, f"x1t{i}", [128, 1024]) for i in range(2)]
                    h2b = [sb(sd, f"h2b{i}", [128, 1024], BF16) for i in range(2)]
                    junk = sb(sd, "junkd", [128, 1024], BF16)
                    osq2 = [sb(sd, f"osq{i}", [128, 8, 128], BF16) for i in range(2)]
                    rsDall = sb(sd, "rsDall", [128, 32])
                    ssD = sb(sd, "ssD", [128, 2])
                    rsD = sb(sd, "rsD", [128, 2])
                    ss2 = sb(sd, "ss2", [128, 1])
                    rs2 = sb(sd, "rs2", [128, 1])
                    lgall = sb(sd, "lgall", [128, 16, 36])
                    lgraw = sb(sd, "lgraw", [128, 16, 36])
                    ss2all = sb(sd, "ss2all", [128, 16])
                    gmax = sb(sd, "gmax", [128, 16, 1])
                    ohg = sb(sd, "ohg", [128, 16, 4])
                    gsh = sb(sd, "gsh", [128, 16, 4])
                    gex = sb(sd, "gex", [128, 16, 4])
                    gsum = sb(sd, "gsum", [128, 16, 1])
                    gval = sb(sd, "gval", [128, 16, 1])
                    tmp4 = sb(sd, "tmp4", [128, 16, 4, 8])
                    loc = sb(sd, "loc", [128, 16, 8])
                    loc2 = sb(sd, "loc2", [128, 16, 8])
                    l1 = sb(sd, "l1", [128, 16, 1])
                    l2 = sb(sd, "l2", [128, 16, 1])
                    m1 = sb(sd, "m1", [128, 16, 8])
                    m2 = sb(sd, "m2", [128, 16, 8])
                    dd = sb(sd, "dd", [128, 16, 1])
                    s1 = sb(sd, "s1", [128, 16, 1])
                    w1 = sb(sd, "w1", [128, 16, 1])
                    w2 = sb(sd, "w2", [128, 16, 1])
                    wl = sb(sd, "wl", [128, 16, 8])
                    wl2 = sb(sd, "wl2", [128, 16, 8])
                    S.dma("sp", lambda e: e.dma_start(out=gbc[:], in_=ffn_norm[0:1, :].partition_broadcast(128)), writes=["gbc"])
                    def d_partA(t):
                        ts_ = slice(t * 128, (t + 1) * 128)
                        xt, xk = xts[t % 2], f"xd{t % 2}"
                        x1, x1k = x1t[t % 2], f"x1t{t % 2}"
                        hb, hk = h2b[t % 2], f"h2b{t % 2}"
                        S.dma("sp", lambda e: e.dma_start(out=xt[:], in_=x[b, ts_, :]), writes=[xk])
                        for grp in range(2):
                            for hh in range(2):
                                bn = grp * 2 + hh
                                for jl in range(4):
                                    jj = grp * 4 + jl
                                    S.op("pe", lambda e: e.matmul(bank(bn), oT[:, jj, ts_], w_out_sb[:, jj, hh * 512:(hh + 1) * 512],
                                                                  start=(jl == 0), stop=(jl == 3)),
                                         reads=[("oT", jj), "w_out"], writes=[bk(bn)])
                        S.op("dve", lambda e: e.scalar_tensor_tensor(x1[:], PS[0][:], rsDall[:, 2 * t:2 * t + 1], xt[:], ALU.mult, ALU.add),
                             reads=[bk(0), bk(1), "rsDall", xk], writes=[x1k])
                        S.op("dve", lambda e: e.scalar_tensor_tensor(x1[:], PS[1][:], rsDall[:, 2 * t + 1:2 * t + 2], x1[:], ALU.mult, ALU.add),
                             reads=[bk(2), bk(3), "rsDall", x1k], writes=[x1k])
                        S.dma("sp", lambda e: e.dma_start(out=x1s[b, ts_, :], in_=x1[:]), reads=[x1k], writes=[("x1s", b, t)])
                        S.op("act", lambda e: e.activation(junk[:], x1[:], AF.Square, accum_out=ss2all[:, t:t + 1]),
                             reads=[x1k, "ss2init"], writes=["junkd", ("ss2all", t)])
                        S.op("dve", lambda e: e.tensor_tensor(hb[:], x1[:], gbc[:], ALU.mult), reads=[x1k, "gbc"], writes=[hk])

                    def d_partB(t):
                        ts_ = slice(t * 128, (t + 1) * 128)
                        x1, x1k = x1t[t % 2], f"x1t{t % 2}"
                        hb, hk = h2b[t % 2], f"h2b{t % 2}"
                        transpose_to_hT(hb, hk, t, 5)
                        for c in range(8):
                            S.op("pe", lambda e: e.matmul(bank(6)[:, 0:36], hT[:, c, ts_], wr_sb[:, c, :], start=(c == 0), stop=(c == 7)),
                                 reads=[("hT", t // 4), ("hTt", t), "wr"], writes=[bk(6)])
                        S.op("dve", lambda e: e.tensor_copy(lgraw[:, t, :], bank(6)[:, 0:36]), reads=[bk(6)], writes=["lgraw"])

                    S.op("dve", lambda e: e.memset(ss2all[:], 0.0), writes=["ss2init"] + [("ss2all", t) for t in range(16)])
                    for t in range(16):
                        ts_ = slice(t * 128, (t + 1) * 128)
                        oq, oqk = osq2[t % 2], f"osq{t % 2}"
                        S.op("dve", lambda e: e.tensor_tensor(oq[:], oT[:, :, ts_], oT[:, :, ts_], ALU.mult),
                             reads=[("oT", jj) for jj in range(8)], writes=[oqk])
                        for grp in range(2):
                            for jl in range(4):
                                S.op("pe", lambda e: e.matmul(bank(4)[:, 2 * t + grp:2 * t + grp + 1], oq[:, grp * 4 + jl, :], ones_bf[:, 0:1],
                                                              start=(t == 0 and jl == 0 and grp == 0), stop=(t == 15 and grp == 1 and jl == 3),
                                                              skip_group_check=True),
                                     reads=[oqk, "ones_bf"], writes=[bk(4)])
                    rstd_from_ss(rsDall[:], bank(4)[:, 0:32], 512.0, "rsDall", bk(4))
                    for t in range(17):
                        if t < 16:
                            d_partA(t)
                        if t >= 1:
                            d_partB(t - 1)
                    S.op("act", lambda e: e.activation(rs2all[:], ss2all[:], AF.Sqrt, bias=epsc[:], scale=1.0 / 1024.0),
                         reads=[("ss2all", t) for t in range(16)] + ["epsc"], writes=["rs2all"])
                    S.op("dve", lambda e: e.reciprocal(rs2all[:], rs2all[:]), reads=["rs2all"], writes=["rs2all"])
                    rs2b = rs2all[:].rearrange("p (t o) -> p t o", o=1)
                    S.op("dve", lambda e: e.tensor_tensor(lgall[:], lgraw[:], rs2b.to_broadcast([128, 16, 36]), ALU.mult),
                         reads=["lgraw", "rs2all"], writes=["lgall"])
                    S.op("dve", lambda e: e.tensor_tensor(lgall[:], lgall[:], br_bc[:].rearrange("p (o n) -> p o n", o=1).to_broadcast([128, 16, 36]), ALU.add),
                         reads=["lgall", "br"], writes=["lgall"])
                    T4 = [128, 16, 4]
                    T8 = [128, 16, 8]
                    T48 = [128, 16, 4, 8]
                    glg = lgall[:, :, 0:4]
                    elg = lgall[:, :, 4:36].rearrange("p t (g k) -> p t g k", g=4)
                    ohg4 = ohg[:].rearrange("p t (g o) -> p t g o", o=1)
                    V = lambda fn, r, w: S.op("dve", fn, reads=r, writes=w)
                    V(lambda e: e.reduce_max(gmax[:], glg, axis=AX.X), ["lgall"], ["gmax"])
                    V(lambda e: e.tensor_tensor(ohg[:], glg, gmax[:].to_broadcast(T4), ALU.is_equal), ["lgall", "gmax"], ["ohg"])
                    V(lambda e: e.tensor_tensor(gsh[:], glg, gmax[:].to_broadcast(T4), ALU.subtract), ["lgall", "gmax"], ["gsh"])
                    S.op("act", lambda e: e.activation(gex[:], gsh[:], AF.Exp), reads=["gsh"], writes=["gex"])
                    V(lambda e: e.reduce_sum(gsum[:], gex[:], axis=AX.X), ["gex"], ["gsum"])
                    V(lambda e: e.reciprocal(gval[:], gsum[:]), ["gsum"], ["gval"])
                    V(lambda e: e.tensor_tensor(tmp4[:], elg, ohg4.to_broadcast(T48), ALU.mult), ["lgall", "ohg"], ["tmp4"])
                    V(lambda e: e.reduce_sum(loc[:].rearrange("p t (k o) -> p t k o", o=1), tmp4[:].rearrange("p t g k -> p t k g"), axis=AX.X),
                      ["tmp4"], ["loc"])
                    V(lambda e: e.reduce_max(l1[:], loc[:], axis=AX.X), ["loc"], ["l1"])
                    V(lambda e: e.tensor_tensor(m1[:], loc[:], l1[:].to_broadcast(T8), ALU.is_equal), ["loc", "l1"], ["m1"])
                    V(lambda e: e.scalar_tensor_tensor(loc2[:], m1[:], -1e30, loc[:], ALU.mult, ALU.add), ["m1", "loc"], ["loc2"])
                    V(lambda e: e.reduce_max(l2[:], loc2[:], axis=AX.X), ["loc2"], ["l2"])
                    V(lambda e: e.tensor_tensor(m2[:], loc2[:], l2[:].to_broadcast(T8), ALU.is_equal), ["loc2", "l2"], ["m2"])
                    V(lambda e: e.tensor_tensor(dd[:], l2[:], l1[:], ALU.subtract), ["l1", "l2"], ["dd"])
                    S.op("act", lambda e: e.activation(s1[:], dd[:], AF.Exp), reads=["dd"], writes=["s1"])
                    V(lambda e: e.tensor_scalar(s1[:], s1[:], 1.0, None, ALU.add), ["s1"], ["s1"])
                    V(lambda e: e.reciprocal(s1[:], s1[:]), ["s1"], ["s1"])
                    V(lambda e: e.tensor_tensor(w1[:], gval[:], s1[:], ALU.mult), ["gval", "s1"], ["w1"])
                    V(lambda e: e.tensor_tensor(w2[:], gval[:], w1[:], ALU.subtract), ["gval", "w1"], ["w2"])
                    V(lambda e: e.tensor_tensor(wl[:], m1[:], w1[:].to_broadcast(T8), ALU.mult), ["m1", "w1"], ["wl"])
                    V(lambda e: e.tensor_tensor(wl2[:], m2[:], w2[:].to_broadcast(T8), ALU.mult), ["m2", "w2"], ["wl2"])
                    V(lambda e: e.tensor_tensor(wl[:], wl[:], wl2[:], ALU.add), ["wl", "wl2"], ["wl"])
                    V(lambda e: e.tensor_tensor(Wfull[:].rearrange("p t (g k) -> p t g k", g=4), ohg4.to_broadcast(T48),
                                                wl[:].rearrange("p t (o k) -> p t o k", o=1).to_broadcast(T48), ALU.mult),
                      ["ohg", "wl"], [("Wfull", t) for t in range(16)])
                    V(lambda e: e.tensor_tensor(Wfull[:], Wfull[:], rs2b.to_broadcast([128, 16, 32]), ALU.mult),
                      [("Wfull", t) for t in range(16)] + ["rs2all"], [("Wfull", t) for t in range(16)])
                    S.barrier()

            if "moe" in phases:
              with ExitStack() as s2:
                acc = sb(s2, "acc", [128, 16, 1024])
                NW = 3
                wgu = [sb(s2, f"wgu{i}", [128, 8, 512], BF16) for i in range(NW)]
                wd = [sb(s2, f"wd{i}", [128, 2, 1024], BF16) for i in range(NW)]
                sg = [sb(s2, f"sg{i}", [128, 256]) for i in range(2)]
                hid = [sb(s2, f"hid{i}", [128, 256], BF16) for i in range(2)]
                hidT = [sb(s2, f"hidT{i}", [128, 256], BF16) for i in range(2)]
                junk = sb(s2, "junke", [128, 1024], BF16)
                ss3 = sb(s2, "ss3", [128, 2])
                rs3 = sb(s2, "rs3", [128, 2])
                yt = [sb(s2, f"yt{i}", [128, 1024]) for i in range(2)]
                S.dma("sp", lambda e: e.dma_start(out=gbc[:], in_=final_norm[0:1, :].partition_broadcast(128)), writes=["gbc"])
                for t in range(16):
                    S.dma("sp", lambda e: e.dma_start(out=acc[:, t, :], in_=x1s[b, t * 128:(t + 1) * 128, :]),
                          reads=[("x1s", b, t)], writes=[("acc", t)])

                def load_expert(ex):
                    sl = ex % NW
                    S.dma("pool", lambda e: e.dma_start(out=wgu[sl][:, :, 0:256], in_=w_gate[ex].rearrange("(c p) n -> p c n", p=128)),
                          writes=[f"wgu{sl}"])
                    S.dma("pool", lambda e: e.dma_start(out=wgu[sl][:, :, 256:512], in_=w_up[ex].rearrange("(c p) n -> p c n", p=128)),
                          writes=[f"wgu{sl}"])
                    S.dma("pool", lambda e: e.dma_start(out=wd[sl][:], in_=w_down[ex].rearrange("(c p) n -> p c n", p=128)),
                          writes=[f"wd{sl}"])

                for ex0 in range(min(NW, n_experts)):
                    load_expert(ex0)
                steps = [(ex, t) for ex in range(n_experts) for t in range(16)]
                nS = len(steps)

                def m_gu(k):
                    ex, t = steps[k]
                    sl, p2, ts_ = ex % NW, k % 2, slice(t * 128, (t + 1) * 128)
                    gb = p2
                    for c in range(8):
                        S.op("pe", lambda e: e.matmul(bank(gb), hT[:, c, ts_], wgu[sl][:, c, :], start=(c == 0), stop=(c == 7)),
                             reads=[("hT", t // 4), ("hTt", t), f"wgu{sl}"], writes=[bk(gb)])
                    S.op("act", lambda e: e.activation(sg[p2][:], bank(gb)[:, 0:256], AF.Silu, scale=rs2all[:, t:t + 1]), reads=[bk(gb), "rs2all"], writes=[f"sg{p2}"])
                    S.op("dve", lambda e: e.scalar_tensor_tensor(hid[p2][:], bank(gb)[:, 256:512], Wfull[:, t, ex:ex + 1], sg[p2][:],
                                                                 ALU.mult, ALU.mult),
                         reads=[bk(gb), ("Wfull", t), f"sg{p2}"], writes=[f"hid{p2}"])

                def m_tr(k):
                    p2 = k % 2
                    tbk = 2 + p2
                    tb_ = bank_bf(tbk)
                    for c in range(2):
                        S.op("pe", lambda e: e.transpose(tb_[:, c * 128:(c + 1) * 128], hid[p2][:, c * 128:(c + 1) * 128], ident),
                             reads=[f"hid{p2}", "cst"], writes=[bk(tbk)])
                    S.op("act", lambda e: e.copy(hidT[p2][:], tb_[:, 0:256]), reads=[bk(tbk)], writes=[f"hidT{p2}"])

                def m_dn(k):
                    ex, t = steps[k]
                    sl, p2 = ex % NW, k % 2
                    dps = 2 + p2
                    for hh in range(2):
                        for c in range(2):
                            S.op("pe", lambda e: e.matmul(PS[dps][:, hh * 512:(hh + 1) * 512], hidT[p2][:, c * 128:(c + 1) * 128],
                                                          wd[sl][:, c, hh * 512:(hh + 1) * 512], start=(c == 0), stop=(c == 1)),
                                 reads=[f"hidT{p2}", f"wd{sl}"], writes=[bk(2 * dps + hh)])
                    S.op("dve", lambda e: e.tensor_tensor(acc[:, t, :], acc[:, t, :], PS[dps][:], ALU.add),
                         reads=[("acc", t), bk(2 * dps), bk(2 * dps + 1)], writes=[("acc", t)])
                    if t == 15 and ex + NW < n_experts:
                        load_expert(ex + NW)

                for k in range(nS + 2):
                    if k < nS:
                        m_gu(k)
                    if 0 <= k - 1 < nS:
                        m_tr(k - 1)
                    if 0 <= k - 2 < nS:
                        m_dn(k - 2)
                for t in range(16):
                    sl = t % 2
                    S.op("dve", lambda e: e.memset(ss3[:, sl:sl + 1], 0.0), writes=[f"ss3{sl}"])
                    S.op("act", lambda e: e.activation(junk[:], acc[:, t, :], AF.Square, accum_out=ss3[:, sl:sl + 1]),
                         reads=[("acc", t)], writes=["junke", f"ss3{sl}"])
                    rstd_from_ss(rs3[:, sl:sl + 1], ss3[:, sl:sl + 1], 1024.0, f"rs3{sl}", f"ss3{sl}")
                    S.op("dve", lambda e: e.scalar_tensor_tensor(yt[sl][:], acc[:, t, :], rs3[:, sl:sl + 1], gbc[:], ALU.mult, ALU.mult),
                         reads=[("acc", t), f"rs3{sl}", "gbc"], writes=[f"yt{sl}"])
                    S.dma("sp", lambda e: e.dma_start(out=y[b, t * 128:(t + 1) * 128, :], in_=yt[sl][:]), reads=[f"yt{sl}"], writes=[("y", b, t)])
                S.barrier()
        S.final_wait("sp")
        print("instructions emitted:", S.nins, {e: S.cnt[e] for e in ENGS}, S.ndma)
    return nc


def _consts():
    k = np.arange(128)[:, None]
    q = np.arange(128)[None, :]
    ident = (k == q)
    tri = (k >= q)
    strict = (k < q)
    causal = (k <= q)
    c = np.concatenate([ident, tri, strict, causal, strict], axis=1).astype(np.float32)
    half = 16
    inv_freq = (np.float32(10000.0) ** (-np.arange(half, dtype=np.float32) / np.float32(half))).astype(np.float32)
    invf = np.zeros((128, 1), np.float32)
    for p in range(64, 96):
        invf[p, 0] = inv_freq[(p - 64) % 16]
    return c.astype(ml_dtypes.bfloat16), invf


def make_in_maps(inputs, n_cores, nseq):
    f = lambda a: np.ascontiguousarray(np.asarray(a))
    cst, invf = _consts()
    shared = {
        "attn_norm": f(inputs["attn_norm"]).reshape(1, 1024),
        "w_in": f(inputs["w_in"]).reshape(1024, 1952),
        "q_norm": f(inputs["q_norm"]).reshape(256, 1),
        "w_uq": f(inputs["w_uq"]).reshape(256, 768),
        "kv_norm": f(inputs["kv_norm"]).reshape(128, 1),
        "w_ukv": f(inputs["w_ukv"]).reshape(128, 1024),
        "out_norm": np.concatenate([f(inputs["sb_out_norm"]).reshape(-1), f(inputs["mla_out_norm"]).reshape(-1)]).reshape(1024, 1),
        "w_out": f(inputs["w_out"]).reshape(1024, 1024),
        "ffn_norm": f(inputs["ffn_norm"]).reshape(1, 1024),
        "w_group_router": f(inputs["w_group_router"]).reshape(1024, 4),
        "b_group_router": f(inputs["b_group_router"]).reshape(1, 4),
        "w_expert_router": f(inputs["w_expert_router"]).reshape(1024, 32),
        "b_expert_router": f(inputs["b_expert_router"]).reshape(1, 32),
        "w_gate": f(inputs["w_gate"]).reshape(32, 1024, 256),
        "w_up": f(inputs["w_up"]).reshape(32, 1024, 256),
        "w_down": f(inputs["w_down"]).reshape(32, 256, 1024),
        "final_norm": f(inputs["final_norm"]).reshape(1, 1024),
        "consts": cst,
        "invf": invf,
    }
    xs = f(inputs["x"])
    ps = f(inputs["positions"]).astype(np.int32)
    maps = []
    for c in range(n_cores):
        m = dict(shared)
        m["x"] = xs[c * nseq:(c + 1) * nseq]
        m["positions"] = ps[c * nseq:(c + 1) * nseq]
        maps.append(m)
    return maps


def kernel(**inputs):
    n_cores = 8
    nseq = 4
    nc = build(NSEQ=nseq)
    maps = make_in_maps(inputs, n_cores, nseq)
    res = run_bass_kernel_spmd(nc, maps, core_ids=list(range(n_cores)))
    out = np.concatenate([np.asarray(r["y"]) for r in res.results], axis=0)
    return out.astype(np.float32)
```

```python
import numpy as np
import ml_dtypes
from contextlib import ExitStack
import concourse.bass as bass
import concourse.mybir as mybir
from concourse.bass_utils import run_bass_kernel_spmd

F32 = mybir.dt.float32
BF16 = mybir.dt.bfloat16
I32 = mybir.dt.int32
AF = mybir.ActivationFunctionType
ALU = mybir.AluOpType
AX = mybir.AxisListType

ENGS = ["pe", "act", "dve", "pool", "sp"]
DMA_ENGS = ["sp", "pool"]
ND = 8
EPS = 1e-6
TWO_PI = float(2 * np.pi)


class _Proxy:
    def __init__(self):
        self.call = None

    def __getattr__(self, name):
        def f(*a, **k):
            self.call = (name, a, k)
            return self
        return f


class Sched:
    def __init__(self, nc, es):
        self.nc = nc
        self.eng = {"pe": nc.tensor, "act": nc.scalar, "dve": nc.vector, "pool": nc.gpsimd, "sp": nc.sync}
        self.sem = {e: es.enter_context(nc.semaphore("c_" + e)) for e in ENGS}
        self.dsem = {e: [es.enter_context(nc.semaphore(f"d_{e}_{i}")) for i in range(ND)] for e in DMA_ENGS}
        self.cnt = {e: 0 for e in ENGS}
        self.ndma = {e: 0 for e in DMA_ENGS}
        self.dlast = {e: {} for e in DMA_ENGS}
        self.lastw = {}
        self.readers = {}
        self.seen = {e: {} for e in ENGS}
        self.nins = 0
        self.rec = None

    def record(self, chunk_fns):
        self.rec = []
        for f in chunk_fns:
            f()
        out, self.rec = self.rec, None
        return out

    def _semobj(self, key):
        return self.sem[key[1]] if key[0] == "c" else self.dsem[key[1]][key[2]]

    def _collect(self, reads, writes):
        toks = {}

        def add(k, v):
            if toks.get(k, 0) < v:
                toks[k] = v
        for r in reads:
            t = self.lastw.get(r)
            if t is not None:
                add(*t)
        for w in writes:
            t = self.lastw.get(w)
            if t is not None:
                add(*t)
            for k, v in self.readers.get(w, {}).items():
                add(k, v)
        return toks

    def _wait(self, eng, toks):
        seen = self.seen[eng]
        for k, v in toks.items():
            if k[0] == "c" and k[1] == eng and eng == "pe":
                continue
            if seen.get(k, 0) >= v:
                continue
            self.eng[eng].wait_ge(self._semobj(k), v)
            seen[k] = v
            self.nins += 1

    def _record(self, tok, reads, writes):
        for w in writes:
            self.lastw[w] = tok
            self.readers[w] = {}
        for r in reads:
            d = self.readers.setdefault(r, {})
            if d.get(tok[0], 0) < tok[1]:
                d[tok[0]] = tok[1]

    def op(self, eng, fn, reads=(), writes=()):
        if self.rec is not None:
            p = _Proxy()
            fn(p)
            name, a, k = p.call
            self.rec.append(lambda: self.op(eng, lambda e: getattr(e, name)(*a, **k), reads, writes))
            return
        toks = self._collect(reads, writes)
        self._wait(eng, toks)
        ins = fn(self.eng[eng])
        self.cnt[eng] += 1
        ins.then_inc(self.sem[eng], 1)
        self.nins += 1
        tok = (("c", eng), self.cnt[eng])
        self._record(tok, reads, writes)

    def dma(self, eng, fn, reads=(), writes=()):
        if self.rec is not None:
            p = _Proxy()
            fn(p)
            name, a, k = p.call
            self.rec.append(lambda: self.dma(eng, lambda e: getattr(e, name)(*a, **k), reads, writes))
            return
        n = self.ndma[eng]
        self.ndma[eng] += 1
        slot = n % ND
        val = 16 * (n // ND + 1)
        toks = self._collect(reads, writes)
        key = ("d", eng, slot)
        if val > 16:
            toks[key] = max(toks.get(key, 0), val - 16)
        self._wait(eng, toks)
        ins = fn(self.eng[eng])
        ins.then_inc(self.dsem[eng][slot], 16)
        self.nins += 1
        self.dlast[eng][slot] = val
        self._record((key, val), reads, writes)

    def barrier(self):
        toks = {}
        for e in ENGS:
            if self.cnt[e] > 0:
                toks[("c", e)] = self.cnt[e]
        for q in DMA_ENGS:
            for slot, val in self.dlast[q].items():
                toks[("d", q, slot)] = val
        for e in ENGS:
            t = {k: v for k, v in toks.items() if not (k[0] == "c" and k[1] == e)}
            self._wait(e, t)

    def final_wait(self, eng="sp"):
        toks = {}
        for q in DMA_ENGS:
            for slot, val in self.dlast[q].items():
                toks[("d", q, slot)] = val
        for e in ENGS:
            if self.cnt[e] > 0 and e != eng:
                toks[("c", e)] = self.cnt[e]
        self._wait(eng, toks)


def build(NSEQ=4, debug=False, n_experts=32, phases=("attn", "moe"), nd_sb=0, nd_mla=0):
    nc = bass.Bass("TRN2", target_bir_lowering=False)

    def din(name, shape, dtype=F32):
        return nc.dram_tensor(name, shape, dtype, kind="ExternalInput").ap()

    x = din("x", [NSEQ, 2048, 1024])
    positions = din("positions", [NSEQ, 2048], I32)
    attn_norm = din("attn_norm", [1, 1024])
    w_in = din("w_in", [1024, 1952])
    q_norm = din("q_norm", [256, 1])
    w_uq = din("w_uq", [256, 768])
    kv_norm = din("kv_norm", [128, 1])
    w_ukv = din("w_ukv", [128, 1024])
    out_norm = din("out_norm", [1024, 1])
    w_out = din("w_out", [1024, 1024])
    ffn_norm = din("ffn_norm", [1, 1024])
    w_gr = din("w_group_router", [1024, 4])
    b_gr = din("b_group_router", [1, 4])
    w_er = din("w_expert_router", [1024, 32])
    b_er = din("b_expert_router", [1, 32])
    w_gate = din("w_gate", [32, 1024, 256])
    w_up = din("w_up", [32, 1024, 256])
    w_down = din("w_down", [32, 256, 1024])
    final_norm = din("final_norm", [1, 1024])
    consts = din("consts", [128, 640], BF16)
    invf = din("invf", [128, 1])
    y = nc.dram_tensor("y", [NSEQ, 2048, 1024], F32, kind="ExternalOutput").ap()
    x1s = nc.dram_tensor("x1s", [NSEQ, 2048, 1024], F32,
                         kind="ExternalOutput" if debug else "Internal").ap()

    dbg_oT = nc.dram_tensor("dbg_oT", [128, 8, 2048], BF16, kind="ExternalOutput").ap() if debug else None
    dbg_hT = nc.dram_tensor("dbg_hT", [128, 8, 2048], BF16, kind="ExternalOutput").ap() if debug else None
    dbg_lat = nc.dram_tensor("dbg_lat", [128, 4, 2048], BF16, kind="ExternalOutput").ap() if debug else None

    with ExitStack() as es:
        S = Sched(nc, es)

        uid = [0]

        def sb(stack, name, shape, dtype=F32):
            uid[0] += 1
            return stack.enter_context(nc.sbuf_tensor(f"{name}_{uid[0]}", shape, dtype))

        PS = [es.enter_context(nc.psum_tensor(f"ps{i}", [128, 1024], F32)) for i in range(4)]
        PSB = [p[:].bitcast(BF16) for p in PS]

        def bank(k):
            return PS[k // 2][:, (k % 2) * 512:(k % 2) * 512 + 512]

        def bank_bf(k):
            return PSB[k // 2][:, (k % 2) * 1024:(k % 2) * 1024 + 1024]

        def bk(k):
            return f"B{k}"

        cst = sb(es, "cst", [128, 640], BF16)
        ident = cst[:, 0:128]
        tri = cst[:, 128:256]
        mstrict = cst[:, 256:384]
        mcausal = cst[:, 384:512]
        slow = cst[:, 512:640]
        ones_bf = sb(es, "ones_bf", [128, 128], BF16)
        ones_f = sb(es, "ones_f", [128, 64], F32)
        epsc = sb(es, "epsc", [128, 1])
        onec = sb(es, "onec", [128, 1])
        invf_sb = sb(es, "invf_sb", [128, 1])
        w_uq_sb = sb(es, "w_uq_sb", [128, 2, 8, 96], BF16)
        w2_sb = sb(es, "w2_sb", [128, 2, 8, 96], BF16)
        w_ukv_sb = sb(es, "w_ukv_sb", [128, 1024], BF16)
        wv_sb = sb(es, "wv_sb", [128, 8, 64], BF16)
        wkr = sb(es, "wkr", [128, 8, 96], BF16)
        wkr2 = sb(es, "wkr2", [128, 8, 96], BF16)
        w_out_sb = sb(es, "w_out_sb", [128, 8, 1024], BF16)
        wr_sb = sb(es, "wr_sb", [128, 8, 36], BF16)
        br_bc = sb(es, "br_bc", [128, 36])
        hT = sb(es, "hT", [128, 8, 2048], BF16)
        Wfull = sb(es, "Wfull", [128, 16, 32])
        rs2all = sb(es, "rs2all", [128, 16])

        S.dma("sp", lambda e: e.dma_start(out=cst[:], in_=consts[:, :]), writes=["cst"])
        S.dma("sp", lambda e: e.dma_start(out=invf_sb[:], in_=invf[:, :]), writes=["invf"])
        S.op("pool", lambda e: e.memset(ones_bf[:], 1.0), writes=["ones_bf"])
        S.op("pool", lambda e: e.memset(ones_f[:], 1.0), writes=["ones_f"])
        S.op("pool", lambda e: e.memset(epsc[:], EPS), writes=["epsc"])
        S.op("pool", lambda e: e.memset(onec[:], 1.0), writes=["onec"])
        S.op("pool", lambda e: e.memset(w2_sb[:], 0.0), writes=["w2"])
        S.op("pool", lambda e: e.memset(wkr[:], 0.0), writes=["wkr"])
        S.op("pool", lambda e: e.memset(wkr2[:], 0.0), writes=["wkr2"])
        with ExitStack() as ss:
            stg = sb(ss, "stg", [128, 2048])
            qn = sb(ss, "qn", [128, 2])
            kvn = sb(ss, "kvn", [128, 1])
            og = sb(ss, "og", [128, 8])
            S.dma("sp", lambda e: e.dma_start(out=stg[:, 0:1536].rearrange("p (c n) -> p c n", c=2),
                                              in_=w_uq.rearrange("(c p) n -> p c n", p=128)), writes=["stg"])
            for c in range(2):
                S.dma("sp", lambda e: e.dma_start(out=qn[:, c:c + 1], in_=q_norm[c * 128:(c + 1) * 128, :]), writes=["qn"])
            stv = stg[:, 0:1536].rearrange("p (c h d) -> p c h d", c=2, h=8)
            for c in range(2):
                S.op("dve", lambda e: e.tensor_scalar(w_uq_sb[:, c], stv[:, c], qn[:, c:c + 1], None, ALU.mult),
                     reads=["stg", "qn"], writes=["w_uq"])
                S.op("dve", lambda e: e.tensor_scalar(w2_sb[:, c, :, 64:80], stv[:, c, :, 80:96], qn[:, c:c + 1], -1.0,
                                                      ALU.mult, ALU.mult), reads=["stg", "qn"], writes=["w2"])
                S.op("dve", lambda e: e.tensor_scalar(w2_sb[:, c, :, 80:96], stv[:, c, :, 64:80], qn[:, c:c + 1], None,
                                                      ALU.mult), reads=["stg", "qn"], writes=["w2"])
            S.dma("sp", lambda e: e.dma_start(out=stg[:, 0:1024], in_=w_ukv[:, :]), writes=["stg"])
            S.dma("sp", lambda e: e.dma_start(out=kvn[:], in_=kv_norm[:, :]), writes=["kvn"])
            S.op("dve", lambda e: e.tensor_scalar(w_ukv_sb[:], stg[:, 0:1024], kvn[:, 0:1], None, ALU.mult),
                 reads=["stg", "kvn"], writes=["w_ukv"])
            S.op("dve", lambda e: e.tensor_scalar(wv_sb[:], stg[:, 0:1024].rearrange("p (h d) -> p h d", h=8)[:, :, 64:128],
                                                  kvn[:, 0:1], None, ALU.mult), reads=["stg", "kvn"], writes=["wv"])
            wi_c = w_in.rearrange("(c p) n -> p c n", p=128)
            S.dma("pool", lambda e: e.dma_start(out=wkr[:, :, 64:96], in_=wi_c[:, :, 1920:1952]), writes=["wkr"])
            S.dma("pool", lambda e: e.dma_start(out=wkr2[:, :, 80:96], in_=wi_c[:, :, 1920:1936]), writes=["wkr2"])
            S.dma("pool", lambda e: e.dma_start(out=wkr2[:, :, 64:80], in_=wi_c[:, :, 1936:1952]), writes=["wkr2"])
            S.op("pool", lambda e: e.tensor_scalar(wkr2[:, :, 64:80], wkr2[:, :, 64:80], -1.0, None, ALU.mult),
                 reads=["wkr2"], writes=["wkr2"])
            for j in range(8):
                S.dma("sp", lambda e: e.dma_start(out=og[:, j:j + 1], in_=out_norm[j * 128:(j + 1) * 128, :]), writes=["og"])
            wo_c = w_out.rearrange("(c p) n -> p c n", p=128)
            for jj in range(4):
                S.dma("sp", lambda e: e.dma_start(out=stg[:].rearrange("p (c n) -> p c n", c=2),
                                                  in_=wo_c[:, 2 * jj:2 * jj + 2, :]), writes=["stg"])
                for jl in range(2):
                    j = 2 * jj + jl
                    S.op("dve", lambda e: e.tensor_scalar(w_out_sb[:, j, :], stg[:, jl * 1024:(jl + 1) * 1024],
                                                          og[:, j:j + 1], None, ALU.mult),
                         reads=["stg", "og"], writes=["w_out"])
            S.dma("pool", lambda e: e.dma_start(out=wr_sb[:, :, 0:4], in_=w_gr.rearrange("(c p) n -> p c n", p=128)), writes=["wr"])
            S.dma("pool", lambda e: e.dma_start(out=wr_sb[:, :, 4:36], in_=w_er.rearrange("(c p) n -> p c n", p=128)), writes=["wr"])
            S.dma("sp", lambda e: e.dma_start(out=br_bc[:, 0:4], in_=b_gr[0:1, :].partition_broadcast(128)), writes=["br"])
            S.dma("sp", lambda e: e.dma_start(out=br_bc[:, 4:36], in_=b_er[0:1, :].partition_broadcast(128)), writes=["br"])
            S.barrier()

        def rstd_from_ss(rs, ss_ap, n, rkey, sskey):
            S.op("act", lambda e: e.activation(rs, ss_ap, AF.Sqrt, bias=epsc[:], scale=1.0 / n),
                 reads=[sskey, "epsc"], writes=[rkey])
            S.op("dve", lambda e: e.reciprocal(rs, rs), reads=[rkey], writes=[rkey])

        def transpose_to_hT(src_bf, srckey, t, bnk):
            pb = bank_bf(bnk)
            for c in range(8):
                S.op("pe", lambda e: e.transpose(pb[:, c * 128:(c + 1) * 128], src_bf[:, c * 128:(c + 1) * 128], ident),
                     reads=[srckey, "cst", ("hTt", t)] if c == 0 else [srckey, "cst"], writes=[bk(bnk)])
            S.op("act", lambda e: e.copy(hT[:, :, t * 128:(t + 1) * 128], pb.rearrange("p (c n) -> p c n", c=8)),
                 reads=[bk(bnk)], writes=[("hT", t // 4), ("hTt", t)])

        for b in range(NSEQ):
            if "attn" in phases:
              with ExitStack() as s1:
                cqTn = sb(s1, "cqTn", [128, 2, 2048], BF16)
                ckvTn = sb(s1, "ckvTn", [128, 2048], BF16)
                krope = sb(s1, "krope", [128, 2048], BF16)
                oT = sb(s1, "oT", [128, 8, 2048], BF16)
                sinT = sb(s1, "sinT", [128, 2048])
                cosT = sb(s1, "cosT", [128, 2048])

                with ExitStack() as sa:
                    xts = [sb(sa, f"xt{i}", [128, 1024]) for i in range(2)]
                    hbs = [sb(sa, f"hb{i}", [128, 1024], BF16) for i in range(2)]
                    junk = sb(sa, "junk", [128, 1024], BF16)
                    ssA = sb(sa, "ssA", [128, 2])
                    rsA = sb(sa, "rsA", [128, 2])
                    wlat = sb(sa, "wlat", [128, 8, 384], BF16)
                    latf = sb(sa, "latf", [128, 3, 512])
                    latsq = sb(sa, "latsq", [128, 3, 512], BF16)
                    rbc = sb(sa, "rbc", [128, 2, 512])
                    posi = sb(sa, "posi", [128, 512], I32)
                    ang = sb(sa, "ang", [128, 512])
                    rtmp = sb(sa, "rtmp", [128, 512])
                    ru = sb(sa, "ru", [128, 512])
                    ta = sb(sa, "ta", [128, 512])
                    tb = sb(sa, "tb", [128, 512])

                    gbc = sb(sa, "gbc", [128, 1024])
                    S.dma("sp", lambda e: e.dma_start(out=gbc[:], in_=attn_norm[0:1, :].partition_broadcast(128)), writes=["gbc"])
                    S.dma("pool", lambda e: e.dma_start(out=wlat[:], in_=w_in.rearrange("(c p) n -> p c n", p=128)[:, :, 1536:1920]),
                          writes=["wlat"])
                    for t in range(16):
                        xt = xts[t % 2]
                        hb = hbs[t % 2]
                        xk, hk = f"xt{t % 2}", f"hb{t % 2}"
                        sl = t % 2
                        S.dma("sp", lambda e: e.dma_start(out=xt[:], in_=x[b, t * 128:(t + 1) * 128, :]), writes=[xk])
                        S.op("dve", lambda e: e.memset(ssA[:, sl:sl + 1], 0.0), writes=[f"ssA{sl}"])
                        S.op("act", lambda e: e.activation(junk[:], xt[:], AF.Square, accum_out=ssA[:, sl:sl + 1]),
                             reads=[xk], writes=["junk", f"ssA{sl}"])
                        rstd_from_ss(rsA[:, sl:sl + 1], ssA[:, sl:sl + 1], 1024.0, f"rsA{sl}", f"ssA{sl}")
                        S.op("dve", lambda e: e.scalar_tensor_tensor(hb[:], xt[:], rsA[:, sl:sl + 1], gbc[:], ALU.mult, ALU.mult),
                             reads=[xk, f"rsA{sl}", "gbc"], writes=[hk])
                        transpose_to_hT(hb, hk, t, t % 2)

                    for G in range(4):
                        gs = slice(G * 512, (G + 1) * 512)
                        hTk = ("hT", G)
                        for lc in range(3):
                            bn = 2 + lc
                            for c in range(8):
                                S.op("pe", lambda e: e.matmul(bank(bn), wlat[:, c, lc * 128:(lc + 1) * 128], hT[:, c, gs],
                                                              start=(c == 0), stop=(c == 7)),
                                     reads=["wlat", hTk], writes=[bk(bn)])
                            S.op("act", lambda e: e.copy(latf[:, lc, :], bank(bn)), reads=[bk(bn)], writes=[("latf", lc)])
                            S.op("dve", lambda e: e.tensor_tensor(latsq[:, lc, :], latf[:, lc, :], latf[:, lc, :], ALU.mult),
                                 reads=[("latf", lc)], writes=[("latsq", lc)])
                        S.op("pe", lambda e: e.matmul(bank(5), ones_bf[:], latsq[:, 0, :], start=True, stop=False),
                             reads=["ones_bf", ("latsq", 0)], writes=[bk(5)])
                        S.op("pe", lambda e: e.matmul(bank(5), ones_bf[:], latsq[:, 1, :], start=False, stop=True),
                             reads=["ones_bf", ("latsq", 1)], writes=[bk(5)])
                        S.op("pe", lambda e: e.matmul(bank(6), ones_bf[:], latsq[:, 2, :], start=True, stop=True),
                             reads=["ones_bf", ("latsq", 2)], writes=[bk(6)])
                        for (ri, bnk_, nn) in ((0, 5, 256.0), (1, 6, 128.0)):
                            S.op("act", lambda e: e.activation(rbc[:, ri, :], bank(bnk_), AF.Ln, bias=epsc[:], scale=1.0 / nn),
                                 reads=[bk(bnk_), "epsc"], writes=[("rbc", ri)])
                            S.op("act", lambda e: e.activation(rbc[:, ri, :], rbc[:, ri, :], AF.Exp, scale=-0.5),
                                 reads=[("rbc", ri)], writes=[("rbc", ri)])
                        for lc in range(2):
                            S.op("dve", lambda e: e.tensor_tensor(cqTn[:, lc, gs], latf[:, lc, :], rbc[:, 0, :], ALU.mult),
                                 reads=[("latf", lc), ("rbc", 0)], writes=["cqTn"])
                        S.op("dve", lambda e: e.tensor_tensor(ckvTn[:, gs], latf[:, 2, :], rbc[:, 1, :], ALU.mult),
                             reads=[("latf", 2), ("rbc", 1)], writes=["ckvTn"])
                        P = slice(64, 96)
                        S.dma("sp", lambda e: e.dma_start(out=posi[P, :], in_=positions[b:b + 1, gs].partition_broadcast(32)),
                              writes=["posi"])
                        S.op("dve", lambda e: e.tensor_copy(ang[P, :], posi[P, :]), reads=["posi"], writes=["ang"])
                        S.op("dve", lambda e: e.tensor_scalar(ang[P, :], ang[P, :], invf_sb[P, 0:1], None, ALU.mult),
                             reads=["ang", "invf"], writes=["ang"])
                        for (dst, dk, add) in ((sinT, "sinT", 0.0), (cosT, "cosT", float(np.pi / 2))):
                            S.op("dve", lambda e: e.tensor_scalar(ru[P, :], ang[P, :], add, None, ALU.add),
                                 reads=["ang"], writes=["ru"])
                            S.op("dve", lambda e: e.tensor_scalar(rtmp[P, :], ru[P, :], 1.0 / TWO_PI, None, ALU.mult),
                                 reads=["ru"], writes=["rtmp"])
                            S.op("dve", lambda e: e.tensor_copy(posi[P, :], rtmp[P, :]), reads=["rtmp"], writes=["posi"])
                            S.op("dve", lambda e: e.tensor_copy(rtmp[P, :], posi[P, :]), reads=["posi"], writes=["rtmp"])
                            S.op("dve", lambda e: e.scalar_tensor_tensor(rtmp[P, :], rtmp[P, :], -TWO_PI, ru[P, :], ALU.mult, ALU.add),
                                 reads=["rtmp", "ru"], writes=["rtmp"])
                            S.op("dve", lambda e: e.tensor_scalar(rtmp[P, :], rtmp[P, :], float(np.pi), float(-np.pi), ALU.min, ALU.max),
                                 reads=["rtmp"], writes=["rtmp"])
                            S.op("act", lambda e: e.activation(dst[P, gs], rtmp[P, :], AF.Sin), reads=["rtmp"], writes=[(dk, G)])
                        for (wt, wk, bn) in ((wkr, "wkr", 7), (wkr2, "wkr2", 5)):
                            for c in range(8):
                                S.op("pe", lambda e: e.matmul(bank(bn)[0:96, :], wt[:, c, :], hT[:, c, gs], start=(c == 0), stop=(c == 7)),
                                     reads=[wk, hTk], writes=[bk(bn)])
                        S.op("dve", lambda e: e.tensor_tensor(ta[P, :], bank(7)[P, :], cosT[P, gs], ALU.mult),
                             reads=[bk(7), ("cosT", G)], writes=["ta"])
                        S.op("dve", lambda e: e.tensor_tensor(tb[P, :], bank(5)[P, :], sinT[P, gs], ALU.mult),
                             reads=[bk(5), ("sinT", G)], writes=["tb"])
                        S.op("dve", lambda e: e.tensor_tensor(krope[P, gs], ta[P, :], tb[P, :], ALU.add),
                             reads=["ta", "tb"], writes=["krope"])
                    S.barrier()

                if debug and b == 0:
                    S.dma("sp", lambda e: e.dma_start(out=dbg_hT[:, :, :], in_=hT[:]), reads=[("hT", g) for g in range(4)], writes=["dbg_hT"])
                    S.barrier()
                def run_passes(proj_chunks, loop_iters, npass):
                    for ch in proj_chunks(0):
                        ch()
                    for j in range(npass):
                        nxt = S.record(proj_chunks(j + 1)) if j + 1 < npass else []
                        iters = loop_iters(j)
                        per = -(-len(nxt) // max(1, len(iters) - 6))
                        for idx, it in enumerate(iters):
                            it()
                            if idx >= 2:
                                for _ in range(per):
                                    if nxt:
                                        nxt.pop(0)()
                        while nxt:
                            nxt.pop(0)()

                with ExitStack() as sp_:
                    wsb2 = [sb(sp_, f"wsb{i}", [128, 8, 3, 128], BF16) for i in range(2)]
                    qs02 = [[sb(sp_, f"qs0_{i}_{h}", [128, 2048], BF16) for h in range(2)] for i in range(2)]
                    ksT2 = [sb(sp_, f"ksT{i}", [128, 2048], BF16) for i in range(2)]
                    vs2 = [sb(sp_, f"vs{i}", [128, 16, 128], BF16) for i in range(2)]
                    E1 = [sb(sp_, f"E1_{i}", [128, 512]) for i in range(4)]
                    Lb = [sb(sp_, f"Lb_{i}", [128, 512], BF16) for i in range(3)]
                    Xe = [sb(sp_, f"Xe_{i}", [128, 512]) for i in range(2)]
                    At = [sb(sp_, f"At_{i}", [128, 512], BF16) for i in range(2)]
                    S32 = [sb(sp_, f"S32_{i}", [128, 512]) for i in range(2)]
                    Sb = [sb(sp_, f"Sb_{i}", [128, 512], BF16) for i in range(2)]
                    wi_c = w_in.rearrange("(c p) n -> p c n", p=128)

                    def sb_proj_chunks(j):
                        pj = j % 2
                        wsb, qs0, ksT, vs = wsb2[pj], qs02[pj], ksT2[pj], vs2[pj]
                        chunks = []

                        def c_load():
                            for w in range(3):
                                S.dma("pool", lambda e: e.dma_start(out=wsb[:, :, w, :],
                                                                    in_=wi_c[:, :, w * 512 + j * 128:w * 512 + (j + 1) * 128]),
                                      writes=[("wsb", pj, w)])
                            S.op("dve", lambda e: e.memset(qs0[0][64:128, :], 0.0), writes=[("qs0", pj, 0, G) for G in range(4)])
                            S.op("dve", lambda e: e.memset(qs0[1][0:64, :], 0.0), writes=[("qs0", pj, 1, G) for G in range(4)])
                        chunks.append(c_load)
                        for G in range(4):
                            gs = slice(G * 512, (G + 1) * 512)

                            def c_q(G=G, gs=gs):
                                for c in range(8):
                                    S.op("pe", lambda e: e.matmul(bank(6), wsb[:, c, 0, :], hT[:, c, gs], start=(c == 0), stop=(c == 7)),
                                         reads=[("wsb", pj, 0), ("hT", G)], writes=[bk(6)])
                                S.op("dve", lambda e: e.tensor_scalar(qs0[0][0:64, gs], bank(6)[0:64, :], 0.125, None, ALU.mult),
                                     reads=[bk(6)], writes=[("qs0", pj, 0, G)])
                                S.op("dve", lambda e: e.tensor_scalar(qs0[1][64:128, gs], bank(6)[64:128, :], 0.125, None, ALU.mult),
                                     reads=[bk(6)], writes=[("qs0", pj, 1, G)])

                            def c_k(G=G, gs=gs):
                                for c in range(8):
                                    S.op("pe", lambda e: e.matmul(bank(7), wsb[:, c, 1, :], hT[:, c, gs], start=(c == 0), stop=(c == 7)),
                                         reads=[("wsb", pj, 1), ("hT", G)], writes=[bk(7)])
                                S.op("dve", lambda e: e.tensor_copy(ksT[:, gs], bank(7)), reads=[bk(7)], writes=[("ksT", pj, G)])

                            def c_v(G=G):
                                bn = 6 + (G % 2)
                                for tl in range(4):
                                    t = G * 4 + tl
                                    for c in range(8):
                                        S.op("pe", lambda e: e.matmul(bank(bn)[:, tl * 128:(tl + 1) * 128], hT[:, c, t * 128:(t + 1) * 128],
                                                                      wsb[:, c, 2, :], start=(c == 0), stop=(c == 7)),
                                             reads=[("wsb", pj, 2), ("hT", G)], writes=[bk(bn)])
                                S.op("dve", lambda e: e.tensor_copy(vs[:, G * 4:(G + 1) * 4, :], bank(bn).rearrange("p (t n) -> p t n", t=4)),
                                     reads=[bk(bn)], writes=[("vs", pj, G)])
                            chunks += [c_q, c_k, c_v]
                        return chunks

                    def sb_loop_iters(j):
                        pj = j % 2
                        qs0, ksT, vs = qs02[pj], ksT2[pj], vs2[pj]
                        units = [(hl, G, i) for G in range(4) for i in range(4 * G + 3, -1, -1) for hl in range(2)]
                        nU = len(units)

                        def geom(u):
                            hl, G, i = units[u]
                            q0 = max(i, 4 * G) * 128
                            off = q0 - G * 512
                            return dict(hl=hl, G=G, i=i, off=off, cs=slice(off, 512), qsl=slice(q0, (G + 1) * 512),
                                        ksl=slice(i * 128, (i + 1) * 128), diag=(i >= 4 * G), pr=slice(hl * 64, (hl + 1) * 64),
                                        first=(i == 4 * G + 3), last=(i == 0), gs=slice(G * 512, (G + 1) * 512))

                        def st0(u):
                            g = geom(u)
                            zb = u % 2
                            S.op("pe", lambda e: e.matmul(bank(zb)[:, g["cs"]], ksT[:, g["ksl"]], qs0[g["hl"]][:, g["qsl"]], start=True, stop=True),
                                 reads=[("ksT", pj, g["i"] // 4), ("qs0", pj, g["hl"], g["G"])], writes=[bk(zb)])

                        def st1a(u):
                            g = geom(u)
                            zb, cs, off = u % 2, g["cs"], g["off"]
                            e1, e1k = E1[u % 4], f"E1_{u % 4}"
                            S.op("act", lambda e: e.activation(e1[:, cs], bank(zb)[:, cs], AF.Exp), reads=[bk(zb)], writes=[e1k])
                            if g["diag"]:
                                S.op("dve", lambda e: e.tensor_tensor(e1[:, off:off + 128], e1[:, off:off + 128], mstrict, ALU.mult),
                                     reads=[e1k, "cst"], writes=[e1k])

                        def st1b(u):
                            g = geom(u)
                            cs = g["cs"]
                            e1, lb = E1[u % 4], Lb[u % 3]
                            e1k, lbk = f"E1_{u % 4}", f"Lb_{u % 3}"
                            S.op("act", lambda e: e.activation(lb[:, cs], e1[:, cs], AF.Ln, bias=onec[:], scale=1.0),
                                 reads=[e1k, "onec"], writes=[lbk])

                        def st2a(u):
                            g = geom(u)
                            cs, hl = g["cs"], g["hl"]
                            cbk = 2 + u % 2
                            lb, xe = Lb[u % 3], Xe[u % 2]
                            lbk, xek = f"Lb_{u % 3}", f"Xe_{u % 2}"
                            S.op("pe", lambda e: e.matmul(bank(cbk)[:, cs], tri, lb[:, cs], start=True, stop=g["first"]),
                                 reads=["cst", lbk], writes=[bk(cbk)])
                            if not g["first"]:
                                S.op("pe", lambda e: e.matmul(bank(cbk)[:, cs], ones_bf[:], Sb[hl][:, cs], start=False, stop=True),
                                     reads=["ones_bf", f"Sb_{hl}"], writes=[bk(cbk)])
                            S.op("act", lambda e: e.activation(xe[:, cs], bank(cbk)[:, cs], AF.Exp, scale=-1.0), reads=[bk(cbk)], writes=[xek])

                        def st2b(u):
                            g = geom(u)
                            cs, hl = g["cs"], g["hl"]
                            e1, lb, xe, at = E1[u % 4], Lb[u % 3], Xe[u % 2], At[u % 2]
                            e1k, lbk, xek, atk = f"E1_{u % 4}", f"Lb_{u % 3}", f"Xe_{u % 2}", f"At_{u % 2}"
                            if not g["last"]:
                                if g["first"]:
                                    S.op("dve", lambda e: e.memset(S32[hl][:], 0.0), writes=[f"S32_{hl}"])
                                S.op("dve", lambda e: e.tensor_tensor(S32[hl][:, cs], S32[hl][:, cs], lb[:, cs], ALU.add),
                                     reads=[f"S32_{hl}", lbk], writes=[f"S32_{hl}"])
                            S.op("dve", lambda e: e.tensor_tensor(at[:, cs], xe[:, cs], e1[:, cs], ALU.mult), reads=[xek, e1k], writes=[atk])
                            if not g["last"]:
                                S.op("dve", lambda e: e.tensor_copy(Sb[hl][:], S32[hl][:]), reads=[f"S32_{hl}"], writes=[f"Sb_{hl}"])

                        def st3(u):
                            g = geom(u)
                            cs = g["cs"]
                            ob = 4 + g["hl"]
                            at, atk = At[u % 2], f"At_{u % 2}"
                            S.op("pe", lambda e: e.matmul(bank(ob)[0:64, cs], vs[:, g["i"], g["pr"]], at[:, cs], start=g["first"], stop=g["last"],
                                                          skip_group_check=True),
                                 reads=[("vs", pj, g["i"] // 4), atk], writes=[bk(ob)])
                            if g["last"]:
                                S.op("act", lambda e: e.copy(oT[g["pr"], j, g["gs"]], bank(ob)[0:64, :]), reads=[bk(ob)], writes=[("oT", j)])

                        def mk(k):
                            def it():
                                if 0 <= k < nU:
                                    st1b(k)
                                if 0 <= k + 1 < nU:
                                    st1a(k + 1)
                                if 0 <= k - 1 < nU:
                                    st2a(k - 1)
                                if 0 <= k + 2 < nU:
                                    st0(k + 2)
                                if 0 <= k - 1 < nU:
                                    st2b(k - 1)
                                if 0 <= k - 2 < nU:
                                    st3(k - 2)
                            return it
                        return [mk(k) for k in range(-2, nU + 2)]

                    run_passes(sb_proj_chunks, sb_loop_iters, 4)
                    S.barrier()

                with ExitStack() as sm:
                    qmT2 = [sb(sm, f"qmT{i}", [128, 2, 2048], BF16) for i in range(2)]
                    kmT2 = [sb(sm, f"kmT{i}", [128, 2, 2048], BF16) for i in range(2)]
                    vm2 = [sb(sm, f"vm{i}", [128, 16, 2, 65], BF16) for i in range(2)]
                    Et = [sb(sm, f"Et_{i}", [128, 512], BF16) for i in range(3)]
                    dr = sb(sm, "dr", [128, 512])
                    rb = sb(sm, "rb", [128, 512])
                    ta = sb(sm, "ta", [128, 512])
                    tb = sb(sm, "tb", [128, 512])
                    P = slice(64, 96)
                    scale = float(96 ** -0.5)

                    def mla_proj_chunks(m):
                        pm = m % 2
                        qmT, kmT, vm = qmT2[pm], kmT2[pm], vm2[pm]
                        chunks = []

                        def c_init():
                            S.op("dve", lambda e: e.memset(vm[:], 1.0), writes=[("vm", pm, G) for G in range(4)])
                            S.op("dve", lambda e: e.memset(qmT[96:128], 0.0), writes=[("qmT", pm, G) for G in range(4)])
                            S.op("dve", lambda e: e.memset(kmT[96:128], 0.0), writes=[("kmT", pm, G) for G in range(4)])
                        chunks.append(c_init)
                        for G in range(4):
                            gs = slice(G * 512, (G + 1) * 512)
                            for hl in range(2):
                                def c_qk(G=G, gs=gs, hl=hl):
                                    h = 2 * m + hl
                                    for (wt, wk, bn) in ((w_uq_sb, "w_uq", 6), (w2_sb, "w2", 7)):
                                        for c in range(2):
                                            S.op("pe", lambda e: e.matmul(bank(bn)[0:96, :], wt[:, c, h, :], cqTn[:, c, gs], start=(c == 0), stop=(c == 1)),
                                                 reads=[wk, "cqTn"], writes=[bk(bn)])
                                    S.op("dve", lambda e: e.tensor_copy(qmT[0:64, hl, gs], bank(6)[0:64, :]), reads=[bk(6)], writes=[("qmT", pm, G)])
                                    S.op("dve", lambda e: e.tensor_tensor(ta[P, :], bank(6)[P, :], cosT[P, gs], ALU.mult),
                                         reads=[bk(6), ("cosT", G)], writes=["ta"])
                                    S.op("dve", lambda e: e.tensor_tensor(tb[P, :], bank(7)[P, :], sinT[P, gs], ALU.mult),
                                         reads=[bk(7), ("sinT", G)], writes=["tb"])
                                    S.op("dve", lambda e: e.tensor_tensor(qmT[P, hl, gs], ta[P, :], tb[P, :], ALU.add),
                                         reads=["ta", "tb"], writes=[("qmT", pm, G)])
                                    S.op("pe", lambda e: e.matmul(bank(6)[0:64, :], w_ukv_sb[:, h * 128:h * 128 + 64], ckvTn[:, gs], start=True, stop=True),
                                         reads=["w_ukv", "ckvTn"], writes=[bk(6)])
                                    S.op("dve", lambda e: e.tensor_copy(kmT[0:64, hl, gs], bank(6)[0:64, :]), reads=[bk(6)], writes=[("kmT", pm, G)])
                                    S.op("dve", lambda e: e.tensor_copy(kmT[P, hl, gs], krope[P, gs]), reads=["krope"], writes=[("kmT", pm, G)])
                                chunks.append(c_qk)

                            def c_v(G=G):
                                bn = 7
                                for tl in range(4):
                                    t = G * 4 + tl
                                    S.op("pe", lambda e: e.matmul(bank(bn)[:, tl * 128:(tl + 1) * 128], ckvTn[:, t * 128:(t + 1) * 128],
                                                                  wv_sb[:, 2 * m:2 * m + 2, :], start=True, stop=True),
                                         reads=["wv", "ckvTn"], writes=[bk(bn)])
                                S.op("dve", lambda e: e.tensor_copy(vm[:, G * 4:(G + 1) * 4, :, 0:64],
                                                                    bank(bn).rearrange("p (t h d) -> p t h d", t=4, h=2)),
                                     reads=[bk(bn)], writes=[("vm", pm, G)])
                            chunks.append(c_v)
                        return chunks

                    def mla_loop_iters(m):
                        pm = m % 2
                        qmT, kmT, vm = qmT2[pm], kmT2[pm], vm2[pm]
                        units = [(hl, G, i) for G in range(4) for i in range(0, 4 * G + 4) for hl in range(2)]
                        nU = len(units)

                        def geom(u):
                            hl, G, i = units[u]
                            q0 = max(i, 4 * G) * 128
                            off = q0 - G * 512
                            return dict(hl=hl, G=G, i=i, off=off, cs=slice(off, 512), qsl=slice(q0, (G + 1) * 512),
                                        ksl=slice(i * 128, (i + 1) * 128), diag=(i >= 4 * G), pr=slice(hl * 64, (hl + 1) * 64),
                                        first=(i == 0), last=(i == 4 * G + 3), gs=slice(G * 512, (G + 1) * 512))

                        def st0(u):
                            g = geom(u)
                            zb = u % 3
                            S.op("pe", lambda e: e.matmul(bank(zb)[:, g["cs"]], kmT[:, g["hl"], g["ksl"]], qmT[:, g["hl"], g["qsl"]],
                                                          start=True, stop=True),
                                 reads=[("kmT", pm, g["i"] // 4), ("qmT", pm, g["G"])], writes=[bk(zb)])

                        def st1(u):
                            g = geom(u)
                            zb, cs, off = u % 3, g["cs"], g["off"]
                            et, etk = Et[u % 3], f"Et_{u % 3}"
                            S.op("act", lambda e: e.activation(et[:, cs], bank(zb)[:, cs], AF.Exp, scale=scale), reads=[bk(zb)], writes=[etk])
                            if g["diag"]:
                                S.op("dve", lambda e: e.tensor_tensor(et[:, off:off + 128], et[:, off:off + 128], mcausal, ALU.mult),
                                     reads=[etk, "cst"], writes=[etk])

                        def st2(u):
                            g = geom(u)
                            cs = g["cs"]
                            ob = 4 + g["hl"]
                            et, etk = Et[u % 3], f"Et_{u % 3}"
                            S.op("pe", lambda e: e.matmul(bank(ob)[0:65, cs], vm[:, g["i"], g["hl"], :], et[:, cs], start=g["first"], stop=g["last"],
                                                          skip_group_check=True),
                                 reads=[("vm", pm, g["i"] // 4), etk], writes=[bk(ob)])
                            if g["last"]:
                                S.op("act", lambda e: e.activation(dr[64:65, :], bank(ob)[64:65, :], AF.Ln), reads=[bk(ob)], writes=["dr"])

                                def fin(ob=ob, pr=g["pr"], gs=g["gs"]):
                                    S.op("act", lambda e: e.activation(dr[64:65, :], dr[64:65, :], AF.Exp, scale=-1.0), reads=["dr"], writes=["dr"])
                                    S.op("pe", lambda e: e.matmul(bank(3)[0:64, :], ones_f[64:65, 0:64], dr[64:65, :], start=True, stop=True),
                                         reads=["ones_f", "dr"], writes=[bk(3)])
                                    S.op("act", lambda e: e.copy(rb[0:64, :], bank(3)[0:64, :]), reads=[bk(3)], writes=["rb"])
                                    S.op("dve", lambda e: e.tensor_tensor(oT[pr, 4 + m, gs], bank(ob)[0:64, :], rb[0:64, :], ALU.mult),
                                         reads=[bk(ob), "rb"], writes=[("oT", 4 + m)])
                                deferred.append(fin)

                        deferred = []

                        def mk(k):
                            def it():
                                if 0 <= k + 1 < nU:
                                    st0(k + 1)
                                if 0 <= k < nU:
                                    st1(k)
                                pend = list(deferred)
                                del deferred[:]
                                for f in pend:
                                    f()
                                if 0 <= k - 1 < nU:
                                    st2(k - 1)
                                if k >= nU:
                                    for f in list(deferred):
                                        f()
                                    del deferred[:]
                            return it
                        return [mk(k) for k in range(-1, nU + 1)]

                    run_passes(mla_proj_chunks, mla_loop_iters, 4)
                    S.barrier()

                if debug and b == 0:
                    S.dma("sp", lambda e: e.dma_start(out=dbg_oT[:, :, :], in_=oT[:]), reads=[("oT", jj) for jj in range(8)], writes=["dbg_oT"])
                    S.dma("sp", lambda e: e.dma_start(out=dbg_lat[:, 0:2, :], in_=cqTn[:]), reads=["cqTn"], writes=["dbg_lat0"])
                    S.dma("sp", lambda e: e.dma_start(out=dbg_lat[:, 2, :], in_=ckvTn[:]), reads=["ckvTn"], writes=["dbg_lat1"])
                    S.dma("sp", lambda e: e.dma_start(out=dbg_lat[64:96, 3, :], in_=krope[64:96, :]), reads=["krope"], writes=["dbg_lat2"])
                    S.barrier()
                with ExitStack() as sd:
                    xts = [sb(sd, f"xd{i}", [128, 1024]) for i in range(2)]
                    x1t = [sb(sd, f"x1t{i}", [128, 1024]) for i in range(2)]
                    h2b = [sb(sd, f"h2b{i}", [128, 1024], BF16) for i in range(2)]
                    junk = sb(sd, "junkd", [128, 1024], BF16)
                    osq2 = [sb(sd, f"osq{i}", [128, 8, 128], BF16) for i in range(2)]
                    rsDall = sb(sd, "rsDall", [128, 32])
                    ssD = sb(sd, "ssD", [128, 2])
                    rsD = sb(sd, "rsD", [128, 2])
                    ss2 = sb(sd, "ss2", [128, 1])
                    rs2 = sb(sd, "rs2", [128, 1])
                    lgall = sb(sd, "lgall", [128, 16, 36])
                    lgraw = sb(sd, "lgraw", [128, 16, 36])
                    ss2all = sb(sd, "ss2all", [128, 16])
                    gmax = sb(sd, "gmax", [128, 16, 1])
                    ohg = sb(sd, "ohg", [128, 16, 4])
                    gsh = sb(sd, "gsh", [128, 16, 4])
                    gex = sb(sd, "gex", [128, 16, 4])
                    gsum = sb(sd, "gsum", [128, 16, 1])
                    gval = sb(sd, "gval", [128, 16, 1])
                    tmp4 = sb(sd, "tmp4", [128, 16, 4, 8])
                    loc = sb(sd, "loc", [128, 16, 8])
                    loc2 = sb(sd, "loc2", [128, 16, 8])
                    l1 = sb(sd, "l1", [128, 16, 1])
                    l2 = sb(sd, "l2", [128, 16, 1])
                    m1 = sb(sd, "m1", [128, 16, 8])
                    m2 = sb(sd, "m2", [128, 16, 8])
                    dd = sb(sd, "dd", [128, 16, 1])
                    s1 = sb(sd, "s1", [128, 16, 1])
                    w1 = sb(sd, "w1", [128, 16, 1])
                    w2 = sb(sd, "w2", [128, 16, 1])
                    wl = sb(sd, "wl", [128, 16, 8])
                    wl2 = sb(sd, "wl2", [128, 16, 8])
                    gbc = sb(sd, "gbc", [128, 1024])
                    S.dma("sp", lambda e: e.dma_start(out=gbc[:], in_=ffn_norm[0:1, :].partition_broadcast(128)), writes=["gbc"])
                    def d_partA(t):
                        ts_ = slice(t * 128, (t + 1) * 128)
                        xt, xk = xts[t % 2], f"xd{t % 2}"
                        x1, x1k = x1t[t % 2], f"x1t{t % 2}"
                        hb, hk = h2b[t % 2], f"h2b{t % 2}"
                        S.dma("sp", lambda e: e.dma_start(out=xt[:], in_=x[b, ts_, :]), writes=[xk])
                        for grp in range(2):
                            for hh in range(2):
                                bn = grp * 2 + hh
                                for jl in range(4):
                                    jj = grp * 4 + jl
                                    S.op("pe", lambda e: e.matmul(bank(bn), oT[:, jj, ts_], w_out_sb[:, jj, hh * 512:(hh + 1) * 512],
                                                                  start=(jl == 0), stop=(jl == 3)),
                                         reads=[("oT", jj), "w_out"], writes=[bk(bn)])
                        S.op("dve", lambda e: e.scalar_tensor_tensor(x1[:], PS[0][:], rsDall[:, 2 * t:2 * t + 1], xt[:], ALU.mult, ALU.add),
                             reads=[bk(0), bk(1), "rsDall", xk], writes=[x1k])
                        S.op("dve", lambda e: e.scalar_tensor_tensor(x1[:], PS[1][:], rsDall[:, 2 * t + 1:2 * t + 2], x1[:], ALU.mult, ALU.add),
                             reads=[bk(2), bk(3), "rsDall", x1k], writes=[x1k])
                        S.dma("sp", lambda e: e.dma_start(out=x1s[b, ts_, :], in_=x1[:]), reads=[x1k], writes=[("x1s", b, t)])
                        S.op("act", lambda e: e.activation(junk[:], x1[:], AF.Square, accum_out=ss2all[:, t:t + 1]),
                             reads=[x1k, "ss2init"], writes=["junkd", ("ss2all", t)])
                        S.op("dve", lambda e: e.tensor_tensor(hb[:], x1[:], gbc[:], ALU.mult), reads=[x1k, "gbc"], writes=[hk])

                    def d_partB(t):
                        ts_ = slice(t * 128, (t + 1) * 128)
                        x1, x1k = x1t[t % 2], f"x1t{t % 2}"
                        hb, hk = h2b[t % 2], f"h2b{t % 2}"
                        transpose_to_hT(hb, hk, t, 5)
                        for c in range(8):
                            S.op("pe", lambda e: e.matmul(bank(6)[:, 0:36], hT[:, c, ts_], wr_sb[:, c, :], start=(c == 0), stop=(c == 7)),
                                 reads=[("hT", t // 4), ("hTt", t), "wr"], writes=[bk(6)])
                        S.op("dve", lambda e: e.tensor_copy(lgraw[:, t, :], bank(6)[:, 0:36]), reads=[bk(6)], writes=["lgraw"])

                    S.op("dve", lambda e: e.memset(ss2all[:], 0.0), writes=["ss2init"] + [("ss2all", t) for t in range(16)])
                    for t in range(16):
                        ts_ = slice(t * 128, (t + 1) * 128)
                        oq, oqk = osq2[t % 2], f"osq{t % 2}"
                        S.op("dve", lambda e: e.tensor_tensor(oq[:], oT[:, :, ts_], oT[:, :, ts_], ALU.mult),
                             reads=[("oT", jj) for jj in range(8)], writes=[oqk])
                        for grp in range(2):
                            for jl in range(4):
                                S.op("pe", lambda e: e.matmul(bank(4)[:, 2 * t + grp:2 * t + grp + 1], oq[:, grp * 4 + jl, :], ones_bf[:, 0:1],
                                                              start=(t == 0 and jl == 0 and grp == 0), stop=(t == 15 and grp == 1 and jl == 3),
                                                              skip_group_check=True),
                                     reads=[oqk, "ones_bf"], writes=[bk(4)])
                    rstd_from_ss(rsDall[:], bank(4)[:, 0:32], 512.0, "rsDall", bk(4))
                    for t in range(17):
                        if t < 16:
                            d_partA(t)
                        if t >= 1:
                            d_partB(t - 1)
                    S.op("act", lambda e: e.activation(rs2all[:], ss2all[:], AF.Sqrt, bias=epsc[:], scale=1.0 / 1024.0),
                         reads=[("ss2all", t) for t in range(16)] + ["epsc"], writes=["rs2all"])
                    S.op("dve", lambda e: e.reciprocal(rs2all[:], rs2all[:]), reads=["rs2all"], writes=["rs2all"])
                    rs2b = rs2all[:].rearrange("p (t o) -> p t o", o=1)
                    S.op("dve", lambda e: e.tensor_tensor(lgall[:], lgraw[:], rs2b.to_broadcast([128, 16, 36]), ALU.mult),
                         reads=["lgraw", "rs2all"], writes=["lgall"])
                    S.op("dve", lambda e: e.tensor_tensor(lgall[:], lgall[:], br_bc[:].rearrange("p (o n) -> p o n", o=1).to_broadcast([128, 16, 36]), ALU.add),
                         reads=["lgall", "br"], writes=["lgall"])
                    T4 = [128, 16, 4]
                    T8 = [128, 16, 8]
                    T48 = [128, 16, 4, 8]
                    glg = lgall[:, :, 0:4]
                    elg = lgall[:, :, 4:36].rearrange("p t (g k) -> p t g k", g=4)
                    ohg4 = ohg[:].rearrange("p t (g o) -> p t g o", o=1)
                    V = lambda fn, r, w: S.op("dve", fn, reads=r, writes=w)
                    V(lambda e: e.reduce_max(gmax[:], glg, axis=AX.X), ["lgall"], ["gmax"])
                    V(lambda e: e.tensor_tensor(ohg[:], glg, gmax[:].to_broadcast(T4), ALU.is_equal), ["lgall", "gmax"], ["ohg"])
                    V(lambda e: e.tensor_tensor(gsh[:], glg, gmax[:].to_broadcast(T4), ALU.subtract), ["lgall", "gmax"], ["gsh"])
                    S.op("act", lambda e: e.activation(gex[:], gsh[:], AF.Exp), reads=["gsh"], writes=["gex"])
                    V(lambda e: e.reduce_sum(gsum[:], gex[:], axis=AX.X), ["gex"], ["gsum"])
                    V(lambda e: e.reciprocal(gval[:], gsum[:]), ["gsum"], ["gval"])
                    V(lambda e: e.tensor_tensor(tmp4[:], elg, ohg4.to_broadcast(T48), ALU.mult), ["lgall", "ohg"], ["tmp4"])
                    V(lambda e: e.reduce_sum(loc[:].rearrange("p t (k o) -> p t k o", o=1), tmp4[:].rearrange("p t g k -> p t k g"), axis=AX.X),
                      ["tmp4"], ["loc"])
                    V(lambda e: e.reduce_max(l1[:], loc[:], axis=AX.X), ["loc"], ["l1"])
                    V(lambda e: e.tensor_tensor(m1[:], loc[:], l1[:].to_broadcast(T8), ALU.is_equal), ["loc", "l1"], ["m1"])
                    V(lambda e: e.scalar_tensor_tensor(loc2[:], m1[:], -1e30, loc[:], ALU.mult, ALU.add), ["m1", "loc"], ["loc2"])
                    V(lambda e: e.reduce_max(l2[:], loc2[:], axis=AX.X), ["loc2"], ["l2"])
                    V(lambda e: e.tensor_tensor(m2[:], loc2[:], l2[:].to_broadcast(T8), ALU.is_equal), ["loc2", "l2"], ["m2"])
                    V(lambda e: e.tensor_tensor(dd[:], l2[:], l1[:], ALU.subtract), ["l1", "l2"], ["dd"])
                    S.op("act", lambda e: e.activation(s1[:], dd[:], AF.Exp), reads=["dd"], writes=["s1"])
                    V(lambda e: e.tensor_scalar(s1[:], s1[:], 1.0, None, ALU.add), ["s1"], ["s1"])
                    V(lambda e: e.reciprocal(s1[:], s1[:]), ["s1"], ["s1"])
                    V(lambda e: e.tensor_tensor(w1[:], gval[:], s1[:], ALU.mult), ["gval", "s1"], ["w1"])
                    V(lambda e: e.tensor_tensor(w2[:], gval[:], w1[:], ALU.subtract), ["gval", "w1"], ["w2"])
                    V(lambda e: e.tensor_tensor(wl[:], m1[:], w1[:].to_broadcast(T8), ALU.mult), ["m1", "w1"], ["wl"])
                    V(lambda e: e.tensor_tensor(wl2[:], m2[:], w2[:].to_broadcast(T8), ALU.mult), ["m2", "w2"], ["wl2"])
                    V(lambda e: e.tensor_tensor(wl[:], wl[:], wl2[:], ALU.add), ["wl", "wl2"], ["wl"])
                    V(lambda e: e.tensor_tensor(Wfull[:].rearrange("p t (g k) -> p t g k", g=4), ohg4.to_broadcast(T48),
                                                wl[:].rearrange("p t (o k) -> p t o k", o=1).to_broadcast(T48), ALU.mult),
                      ["ohg", "wl"], [("Wfull", t) for t in range(16)])
                    V(lambda e: e.tensor_tensor(Wfull[:], Wfull[:], rs2b.to_broadcast([128, 16, 32]), ALU.mult),
                      [("Wfull", t) for t in range(16)] + ["rs2all"], [("Wfull", t) for t in range(16)])
                    S.barrier()

            if "moe" in phases:
              with ExitStack() as s2:
                acc = sb(s2, "acc", [128, 16, 1024])
                NW = 3
                wgu = [sb(s2, f"wgu{i}", [128, 8, 512], BF16) for i in range(NW)]
                wd = [sb(s2, f"wd{i}", [128, 2, 1024], BF16) for i in range(NW)]
                sg = [sb(s2, f"sg{i}", [128, 256]) for i in range(2)]
                hid = [sb(s2, f"hid{i}", [128, 256], BF16) for i in range(2)]
                hidT = [sb(s2, f"hidT{i}", [128, 256], BF16) for i in range(2)]
                junk = sb(s2, "junke", [128, 1024], BF16)
                ss3 = sb(s2, "ss3", [128, 2])
                rs3 = sb(s2, "rs3", [128, 2])
                yt = [sb(s2, f"yt{i}", [128, 1024]) for i in range(2)]
                gbc = sb(s2, "gbc", [128, 1024])
                S.dma("sp", lambda e: e.dma_start(out=gbc[:], in_=final_norm[0:1, :].partition_broadcast(128)), writes=["gbc"])
                for t in range(16):
                    S.dma("sp", lambda e: e.dma_start(out=acc[:, t, :], in_=x1s[b, t * 128:(t + 1) * 128, :]),
                          reads=[("x1s", b, t)], writes=[("acc", t)])

                def load_expert(ex):
                    sl = ex % NW
                    S.dma("pool", lambda e: e.dma_start(out=wgu[sl][:, :, 0:256], in_=w_gate[ex].rearrange("(c p) n -> p c n", p=128)),
                          writes=[f"wgu{sl}"])
                    S.dma("pool", lambda e: e.dma_start(out=wgu[sl][:, :, 256:512], in_=w_up[ex].rearrange("(c p) n -> p c n", p=128)),
                          writes=[f"wgu{sl}"])
                    S.dma("pool", lambda e: e.dma_start(out=wd[sl][:], in_=w_down[ex].rearrange("(c p) n -> p c n", p=128)),
                          writes=[f"wd{sl}"])

                for ex0 in range(min(NW, n_experts)):
                    load_expert(ex0)
                steps = [(ex, t) for ex in range(n_experts) for t in range(16)]
                nS = len(steps)

                def m_gu(k):
                    ex, t = steps[k]
                    sl, p2, ts_ = ex % NW, k % 2, slice(t * 128, (t + 1) * 128)
                    gb = p2
                    for c in range(8):
                        S.op("pe", lambda e: e.matmul(bank(gb), hT[:, c, ts_], wgu[sl][:, c, :], start=(c == 0), stop=(c == 7)),
                             reads=[("hT", t // 4), ("hTt", t), f"wgu{sl}"], writes=[bk(gb)])
                    S.op("act", lambda e: e.activation(sg[p2][:], bank(gb)[:, 0:256], AF.Silu, scale=rs2all[:, t:t + 1]), reads=[bk(gb), "rs2all"], writes=[f"sg{p2}"])
                    S.op("dve", lambda e: e.scalar_tensor_tensor(hid[p2][:], bank(gb)[:, 256:512], Wfull[:, t, ex:ex + 1], sg[p2][:],
                                                                 ALU.mult, ALU.mult),
                         reads=[bk(gb), ("Wfull", t), f"sg{p2}"], writes=[f"hid{p2}"])

                def m_tr(k):
                    p2 = k % 2
                    tbk = 2 + p2
                    tb_ = bank_bf(tbk)
                    for c in range(2):
                        S.op("pe", lambda e: e.transpose(tb_[:, c * 128:(c + 1) * 128], hid[p2][:, c * 128:(c + 1) * 128], ident),
                             reads=[f"hid{p2}", "cst"], writes=[bk(tbk)])
                    S.op("act", lambda e: e.copy(hidT[p2][:], tb_[:, 0:256]), reads=[bk(tbk)], writes=[f"hidT{p2}"])

                def m_dn(k):
                    ex, t = steps[k]
                    sl, p2 = ex % NW, k % 2
                    dps = 2 + p2
                    for hh in range(2):
                        for c in range(2):
                            S.op("pe", lambda e: e.matmul(PS[dps][:, hh * 512:(hh + 1) * 512], hidT[p2][:, c * 128:(c + 1) * 128],
                                                          wd[sl][:, c, hh * 512:(hh + 1) * 512], start=(c == 0), stop=(c == 1)),
                                 reads=[f"hidT{p2}", f"wd{sl}"], writes=[bk(2 * dps + hh)])
                    S.op("dve", lambda e: e.tensor_tensor(acc[:, t, :], acc[:, t, :], PS[dps][:], ALU.add),
                         reads=[("acc", t), bk(2 * dps), bk(2 * dps + 1)], writes=[("acc", t)])
                    if t == 15 and ex + NW < n_experts:
                        load_expert(ex + NW)

                for k in range(nS + 2):
                    if k < nS:
                        m_gu(k)
                    if 0 <= k - 1 < nS:
                        m_tr(k - 1)
                    if 0 <= k - 2 < nS:
                        m_dn(k - 2)
                for t in range(16):
                    sl = t % 2
                    S.op("dve", lambda e: e.memset(ss3[:, sl:sl + 1], 0.0), writes=[f"ss3{sl}"])
                    S.op("act", lambda e: e.activation(junk[:], acc[:, t, :], AF.Square, accum_out=ss3[:, sl:sl + 1]),
                         reads=[("acc", t)], writes=["junke", f"ss3{sl}"])
                    rstd_from_ss(rs3[:, sl:sl + 1], ss3[:, sl:sl + 1], 1024.0, f"rs3{sl}", f"ss3{sl}")
                    S.op("dve", lambda e: e.scalar_tensor_tensor(yt[sl][:], acc[:, t, :], rs3[:, sl:sl + 1], gbc[:], ALU.mult, ALU.mult),
                         reads=[("acc", t), f"rs3{sl}", "gbc"], writes=[f"yt{sl}"])
                    S.dma("sp", lambda e: e.dma_start(out=y[b, t * 128:(t + 1) * 128, :], in_=yt[sl][:]), reads=[f"yt{sl}"], writes=[("y", b, t)])
                S.barrier()
        S.final_wait("sp")
        print("instructions emitted:", S.nins, {e: S.cnt[e] for e in ENGS}, S.ndma)
    return nc


def _consts():
    k = np.arange(128)[:, None]
    q = np.arange(128)[None, :]
    ident = (k == q)
    tri = (k >= q)
    strict = (k < q)
    causal = (k <= q)
    c = np.concatenate([ident, tri, strict, causal, strict], axis=1).astype(np.float32)
    half = 16
    inv_freq = (np.float32(10000.0) ** (-np.arange(half, dtype=np.float32) / np.float32(half))).astype(np.float32)
    invf = np.zeros((128, 1), np.float32)
    for p in range(64, 96):
        invf[p, 0] = inv_freq[(p - 64) % 16]
    return c.astype(ml_dtypes.bfloat16), invf


def make_in_maps(inputs, n_cores, nseq):
    f = lambda a: np.ascontiguousarray(np.asarray(a))
    cst, invf = _consts()
    shared = {
        "attn_norm": f(inputs["attn_norm"]).reshape(1, 1024),
        "w_in": f(inputs["w_in"]).reshape(1024, 1952),
        "q_norm": f(inputs["q_norm"]).reshape(256, 1),
        "w_uq": f(inputs["w_uq"]).reshape(256, 768),
        "kv_norm": f(inputs["kv_norm"]).reshape(128, 1),
        "w_ukv": f(inputs["w_ukv"]).reshape(128, 1024),
        "out_norm": np.concatenate([f(inputs["sb_out_norm"]).reshape(-1), f(inputs["mla_out_norm"]).reshape(-1)]).reshape(1024, 1),
        "w_out": f(inputs["w_out"]).reshape(1024, 1024),
        "ffn_norm": f(inputs["ffn_norm"]).reshape(1, 1024),
        "w_group_router": f(inputs["w_group_router"]).reshape(1024, 4),
        "b_group_router": f(inputs["b_group_router"]).reshape(1, 4),
        "w_expert_router": f(inputs["w_expert_router"]).reshape(1024, 32),
        "b_expert_router": f(inputs["b_expert_router"]).reshape(1, 32),
        "w_gate": f(inputs["w_gate"]).reshape(32, 1024, 256),
        "w_up": f(inputs["w_up"]).reshape(32, 1024, 256),
        "w_down": f(inputs["w_down"]).reshape(32, 256, 1024),
        "final_norm": f(inputs["final_norm"]).reshape(1, 1024),
        "consts": cst,
        "invf": invf,
    }
    xs = f(inputs["x"])
    ps = f(inputs["positions"]).astype(np.int32)
    maps = []
    for c in range(n_cores):
        m = dict(shared)
        m["x"] = xs[c * nseq:(c + 1) * nseq]
        m["positions"] = ps[c * nseq:(c + 1) * nseq]
        maps.append(m)
    return maps


def kernel(**inputs):
    n_cores = 8
    nseq = 4
    nc = build(NSEQ=nseq)
    maps = make_in_maps(inputs, n_cores, nseq)
    res = run_bass_kernel_spmd(nc, maps, core_ids=list(range(n_cores)))
    out = np.concatenate([np.asarray(r["y"]) for r in res.results], axis=0)
    return out.astype(np.float32)
```

```python
import numpy as np
import ml_dtypes
from contextlib import ExitStack
import concourse.bass as bass
import concourse.mybir as mybir
from concourse.bass_utils import run_bass_kernel_spmd

F32 = mybir.dt.float32
BF16 = mybir.dt.bfloat16
I32 = mybir.dt.int32
AF = mybir.ActivationFunctionType
ALU = mybir.AluOpType
AX = mybir.AxisListType

ENGS = ["pe", "act", "dve", "pool", "sp"]
DMA_ENGS = ["sp", "pool"]
ND = 8
EPS = 1e-6
TWO_PI = float(2 * np.pi)


class _Proxy:
    def __init__(self):
        self.call = None

    def __getattr__(self, name):
        def f(*a, **k):
            self.call = (name, a, k)
            return self
        return f


class Sched:
    def __init__(self, nc, es):
        self.nc = nc
        self.eng = {"pe": nc.tensor, "act": nc.scalar, "dve": nc.vector, "pool": nc.gpsimd, "sp": nc.sync}
        self.sem = {e: es.enter_context(nc.semaphore("c_" + e)) for e in ENGS}
        self.dsem = {e: [es.enter_context(nc.semaphore(f"d_{e}_{i}")) for i in range(ND)] for e in DMA_ENGS}
        self.cnt = {e: 0 for e in ENGS}
        self.ndma = {e: 0 for e in DMA_ENGS}
        self.dlast = {e: {} for e in DMA_ENGS}
        self.lastw = {}
        self.readers = {}
        self.seen = {e: {} for e in ENGS}
        self.nins = 0
        self.rec = None

    def record(self, chunk_fns):
        self.rec = []
        for f in chunk_fns:
            f()
        out, self.rec = self.rec, None
        return out

    def _semobj(self, key):
        return self.sem[key[1]] if key[0] == "c" else self.dsem[key[1]][key[2]]

    def _collect(self, reads, writes):
        toks = {}

        def add(k, v):
            if toks.get(k, 0) < v:
                toks[k] = v
        for r in reads:
            t = self.lastw.get(r)
            if t is not None:
                add(*t)
        for w in writes:
            t = self.lastw.get(w)
            if t is not None:
                add(*t)
            for k, v in self.readers.get(w, {}).items():
                add(k, v)
        return toks

    def _wait(self, eng, toks):
        seen = self.seen[eng]
        for k, v in toks.items():
            if k[0] == "c" and k[1] == eng and eng == "pe":
                continue
            if seen.get(k, 0) >= v:
                continue
            self.eng[eng].wait_ge(self._semobj(k), v)
            seen[k] = v
            self.nins += 1

    def _record(self, tok, reads, writes):
        for w in writes:
            self.lastw[w] = tok
            self.readers[w] = {}
        for r in reads:
            d = self.readers.setdefault(r, {})
            if d.get(tok[0], 0) < tok[1]:
                d[tok[0]] = tok[1]

    def op(self, eng, fn, reads=(), writes=()):
        if self.rec is not None:
            p = _Proxy()
            fn(p)
            name, a, k = p.call
            self.rec.append(lambda: self.op(eng, lambda e: getattr(e, name)(*a, **k), reads, writes))
            return
        toks = self._collect(reads, writes)
        self._wait(eng, toks)
        ins = fn(self.eng[eng])
        self.cnt[eng] += 1
        ins.then_inc(self.sem[eng], 1)
        self.nins += 1
        tok = (("c", eng), self.cnt[eng])
        self._record(tok, reads, writes)

    def dma(self, eng, fn, reads=(), writes=()):
        if self.rec is not None:
            p = _Proxy()
            fn(p)
            name, a, k = p.call
            self.rec.append(lambda: self.dma(eng, lambda e: getattr(e, name)(*a, **k), reads, writes))
            return
        n = self.ndma[eng]
        self.ndma[eng] += 1
        slot = n % ND
        val = 16 * (n // ND + 1)
        toks = self._collect(reads, writes)
        key = ("d", eng, slot)
        if val > 16:
            toks[key] = max(toks.get(key, 0), val - 16)
        self._wait(eng, toks)
        ins = fn(self.eng[eng])
        ins.then_inc(self.dsem[eng][slot], 16)
        self.nins += 1
        self.dlast[eng][slot] = val
        self._record((key, val), reads, writes)

    def barrier(self):
        toks = {}
        for e in ENGS:
            if self.cnt[e] > 0:
                toks[("c", e)] = self.cnt[e]
        for q in DMA_ENGS:
            for slot, val in self.dlast[q].items():
                toks[("d", q, slot)] = val
        for e in ENGS:
            t = {k: v for k, v in toks.items() if not (k[0] == "c" and k[1] == e)}
            self._wait(e, t)

    def final_wait(self, eng="sp"):
        toks = {}
        for q in DMA_ENGS:
            for slot, val in self.dlast[q].items():
                toks[("d", q, slot)] = val
        for e in ENGS:
            if self.cnt[e] > 0 and e != eng:
                toks[("c", e)] = self.cnt[e]
        self._wait(eng, toks)


def build(NSEQ=4, debug=False, n_experts=32, phases=("attn", "moe"), nd_sb=0, nd_mla=0):
    nc = bass.Bass("TRN2", target_bir_lowering=False)

    def din(name, shape, dtype=F32):
        return nc.dram_tensor(name, shape, dtype, kind="ExternalInput").ap()

    x = din("x", [NSEQ, 2048, 1024])
    positions = din("positions", [NSEQ, 2048], I32)
    attn_norm = din("attn_norm", [1, 1024])
    w_in = din("w_in", [1024, 1952])
    q_norm = din("q_norm", [256, 1])
    w_uq = din("w_uq", [256, 768])
    kv_norm = din("kv_norm", [128, 1])
    w_ukv = din("w_ukv", [128, 1024])
    out_norm = din("out_norm", [1024, 1])
    w_out = din("w_out", [1024, 1024])
    ffn_norm = din("ffn_norm", [1, 1024])
    w_gr = din("w_group_router", [1024, 4])
    b_gr = din("b_group_router", [1, 4])
    w_er = din("w_expert_router", [1024, 32])
    b_er = din("b_expert_router", [1, 32])
    w_gate = din("w_gate", [32, 1024, 256])
    w_up = din("w_up", [32, 1024, 256])
    w_down = din("w_down", [32, 256, 1024])
    final_norm = din("final_norm", [1, 1024])
    consts = din("consts", [128, 640], BF16)
    invf = din("invf", [128, 1])
    y = nc.dram_tensor("y", [NSEQ, 2048, 1024], F32, kind="ExternalOutput").ap()
    x1s = nc.dram_tensor("x1s", [NSEQ, 2048, 1024], F32,
                         kind="ExternalOutput" if debug else "Internal").ap()

    dbg_oT = nc.dram_tensor("dbg_oT", [128, 8, 2048], BF16, kind="ExternalOutput").ap() if debug else None
    dbg_hT = nc.dram_tensor("dbg_hT", [128, 8, 2048], BF16, kind="ExternalOutput").ap() if debug else None
    dbg_lat = nc.dram_tensor("dbg_lat", [128, 4, 2048], BF16, kind="ExternalOutput").ap() if debug else None

    with ExitStack() as es:
        S = Sched(nc, es)

        uid = [0]

        def sb(stack, name, shape, dtype=F32):
            uid[0] += 1
            return stack.enter_context(nc.sbuf_tensor(f"{name}_{uid[0]}", shape, dtype))

        PS = [es.enter_context(nc.psum_tensor(f"ps{i}", [128, 1024], F32)) for i in range(4)]
        PSB = [p[:].bitcast(BF16) for p in PS]

        def bank(k):
            return PS[k // 2][:, (k % 2) * 512:(k % 2) * 512 + 512]

        def bank_bf(k):
            return PSB[k // 2][:, (k % 2) * 1024:(k % 2) * 1024 + 1024]

        def bk(k):
            return f"B{k}"

        cst = sb(es, "cst", [128, 640], BF16)
        ident = cst[:, 0:128]
        tri = cst[:, 128:256]
        mstrict = cst[:, 256:384]
        mcausal = cst[:, 384:512]
        slow = cst[:, 512:640]
        ones_bf = sb(es, "ones_bf", [128, 128], BF16)
        ones_f = sb(es, "ones_f", [128, 64], F32)
        epsc = sb(es, "epsc", [128, 1])
        onec = sb(es, "onec", [128, 1])
        invf_sb = sb(es, "invf_sb", [128, 1])
        w_uq_sb = sb(es, "w_uq_sb", [128, 2, 8, 96], BF16)
        w2_sb = sb(es, "w2_sb", [128, 2, 8, 96], BF16)
        w_ukv_sb = sb(es, "w_ukv_sb", [128, 1024], BF16)
        wv_sb = sb(es, "wv_sb", [128, 8, 64], BF16)
        wkr = sb(es, "wkr", [128, 8, 96], BF16)
        wkr2 = sb(es, "wkr2", [128, 8, 96], BF16)
        w_out_sb = sb(es, "w_out_sb", [128, 8, 1024], BF16)
        wr_sb = sb(es, "wr_sb", [128, 8, 36], BF16)
        br_bc = sb(es, "br_bc", [128, 36])
        hT = sb(es, "hT", [128, 8, 2048], BF16)
        Wfull = sb(es, "Wfull", [128, 16, 32])
        rs2all = sb(es, "rs2all", [128, 16])

        S.dma("sp", lambda e: e.dma_start(out=cst[:], in_=consts[:, :]), writes=["cst"])
        S.dma("sp", lambda e: e.dma_start(out=invf_sb[:], in_=invf[:, :]), writes=["invf"])
        S.op("pool", lambda e: e.memset(ones_bf[:], 1.0), writes=["ones_bf"])
        S.op("pool", lambda e: e.memset(ones_f[:], 1.0), writes=["ones_f"])
        S.op("pool", lambda e: e.memset(epsc[:], EPS), writes=["epsc"])
        S.op("pool", lambda e: e.memset(onec[:], 1.0), writes=["onec"])
        S.op("pool", lambda e: e.memset(w2_sb[:], 0.0), writes=["w2"])
        S.op("pool", lambda e: e.memset(wkr[:], 0.0), writes=["wkr"])
        S.op("pool", lambda e: e.memset(wkr2[:], 0.0), writes=["wkr2"])
        with ExitStack() as ss:
            stg = sb(ss, "stg", [128, 2048])
            qn = sb(ss, "qn", [128, 2])
            kvn = sb(ss, "kvn", [128, 1])
            og = sb(ss, "og", [128, 8])
            S.dma("sp", lambda e: e.dma_start(out=stg[:, 0:1536].rearrange("p (c n) -> p c n", c=2),
                                              in_=w_uq.rearrange("(c p) n -> p c n", p=128)), writes=["stg"])
            for c in range(2):
                S.dma("sp", lambda e: e.dma_start(out=qn[:, c:c + 1], in_=q_norm[c * 128:(c + 1) * 128, :]), writes=["qn"])
            stv = stg[:, 0:1536].rearrange("p (c h d) -> p c h d", c=2, h=8)
            for c in range(2):
                S.op("dve", lambda e: e.tensor_scalar(w_uq_sb[:, c], stv[:, c], qn[:, c:c + 1], None, ALU.mult),
                     reads=["stg", "qn"], writes=["w_uq"])
                S.op("dve", lambda e: e.tensor_scalar(w2_sb[:, c, :, 64:80], stv[:, c, :, 80:96], qn[:, c:c + 1], -1.0,
                                                      ALU.mult, ALU.mult), reads=["stg", "qn"], writes=["w2"])
                S.op("dve", lambda e: e.tensor_scalar(w2_sb[:, c, :, 80:96], stv[:, c, :, 64:80], qn[:, c:c + 1], None,
                                                      ALU.mult), reads=["stg", "qn"], writes=["w2"])
            S.dma("sp", lambda e: e.dma_start(out=stg[:, 0:1024], in_=w_ukv[:, :]), writes=["stg"])
            S.dma("sp", lambda e: e.dma_start(out=kvn[:], in_=kv_norm[:, :]), writes=["kvn"])
            S.op("dve", lambda e: e.tensor_scalar(w_ukv_sb[:], stg[:, 0:1024], kvn[:, 0:1], None, ALU.mult),
                 reads=["stg", "kvn"], writes=["w_ukv"])
            S.op("dve", lambda e: e.tensor_scalar(wv_sb[:], stg[:, 0:1024].rearrange("p (h d) -> p h d", h=8)[:, :, 64:128],
                                                  kvn[:, 0:1], None, ALU.mult), reads=["stg", "kvn"], writes=["wv"])
            wi_c = w_in.rearrange("(c p) n -> p c n", p=128)
            S.dma("pool", lambda e: e.dma_start(out=wkr[:, :, 64:96], in_=wi_c[:, :, 1920:1952]), writes=["wkr"])
            S.dma("pool", lambda e: e.dma_start(out=wkr2[:, :, 80:96], in_=wi_c[:, :, 1920:1936]), writes=["wkr2"])
            S.dma("pool", lambda e: e.dma_start(out=wkr2[:, :, 64:80], in_=wi_c[:, :, 1936:1952]), writes=["wkr2"])
            S.op("pool", lambda e: e.tensor_scalar(wkr2[:, :, 64:80], wkr2[:, :, 64:80], -1.0, None, ALU.mult),
                 reads=["wkr2"], writes=["wkr2"])
            for j in range(8):
                S.dma("sp", lambda e: e.dma_start(out=og[:, j:j + 1], in_=out_norm[j * 128:(j + 1) * 128, :]), writes=["og"])
            wo_c = w_out.rearrange("(c p) n -> p c n", p=128)
            for jj in range(4):
                S.dma("sp", lambda e: e.dma_start(out=stg[:].rearrange("p (c n) -> p c n", c=2),
                                                  in_=wo_c[:, 2 * jj:2 * jj + 2, :]), writes=["stg"])
                for jl in range(2):
                    j = 2 * jj + jl
                    S.op("dve", lambda e: e.tensor_scalar(w_out_sb[:, j, :], stg[:, jl * 1024:(jl + 1) * 1024],
                                                          og[:, j:j + 1], None, ALU.mult),
                         reads=["stg", "og"], writes=["w_out"])
            S.dma("pool", lambda e: e.dma_start(out=wr_sb[:, :, 0:4], in_=w_gr.rearrange("(c p) n -> p c n", p=128)), writes=["wr"])
            S.dma("pool", lambda e: e.dma_start(out=wr_sb[:, :, 4:36], in_=w_er.rearrange("(c p) n -> p c n", p=128)), writes=["wr"])
            S.dma("sp", lambda e: e.dma_start(out=br_bc[:, 0:4], in_=b_gr[0:1, :].partition_broadcast(128)), writes=["br"])
            S.dma("sp", lambda e: e.dma_start(out=br_bc[:, 4:36], in_=b_er[0:1, :].partition_broadcast(128)), writes=["br"])
            S.barrier()

        def rstd_from_ss(rs, ss_ap, n, rkey, sskey):
            S.op("act", lambda e: e.activation(rs, ss_ap, AF.Sqrt, bias=epsc[:], scale=1.0 / n),
                 reads=[sskey, "epsc"], writes=[rkey])
            S.op("dve", lambda e: e.reciprocal(rs, rs), reads=[rkey], writes=[rkey])

        def transpose_to_hT(src_bf, srckey, t, bnk):
            pb = bank_bf(bnk)
            for c in range(8):
                S.op("pe", lambda e: e.transpose(pb[:, c * 128:(c + 1) * 128], src_bf[:, c * 128:(c + 1) * 128], ident),
                     reads=[srckey, "cst", ("hTt", t)] if c == 0 else [srckey, "cst"], writes=[bk(bnk)])
            S.op("act", lambda e: e.copy(hT[:, :, t * 128:(t + 1) * 128], pb.rearrange("p (c n) -> p c n", c=8)),
                 reads=[bk(bnk)], writes=[("hT", t // 4), ("hTt", t)])

        for b in range(NSEQ):
            if "attn" in phases:
              with ExitStack() as s1:
                cqTn = sb(s1, "cqTn", [128, 2, 2048], BF16)
                ckvTn = sb(s1, "ckvTn", [128, 2048], BF16)
                krope = sb(s1, "krope", [128, 2048], BF16)
                oT = sb(s1, "oT", [128, 8, 2048], BF16)
                sinT = sb(s1, "sinT", [128, 2048])
                cosT = sb(s1, "cosT", [128, 2048])

                with ExitStack() as sa:
                    xts = [sb(sa, f"xt{i}", [128, 1024]) for i in range(2)]
                    hbs = [sb(sa, f"hb{i}", [128, 1024], BF16) for i in range(2)]
                    junk = sb(sa, "junk", [128, 1024], BF16)
                    ssA = sb(sa, "ssA", [128, 2])
                    rsA = sb(sa, "rsA", [128, 2])
                    wlat = sb(sa, "wlat", [128, 8, 384], BF16)
                    latf = sb(sa, "latf", [128, 3, 512])
                    latsq = sb(sa, "latsq", [128, 3, 512], BF16)
                    rbc = sb(sa, "rbc", [128, 2, 512])
                    posi = sb(sa, "posi", [128, 512], I32)
                    ang = sb(sa, "ang", [128, 512])
                    rtmp = sb(sa, "rtmp", [128, 512])
                    ru = sb(sa, "ru", [128, 512])
                    ta = sb(sa, "ta", [128, 512])
                    tb = sb(sa, "tb", [128, 512])

                    gbc = sb(sa, "gbc", [128, 1024])
                    S.dma("sp", lambda e: e.dma_start(out=gbc[:], in_=attn_norm[0:1, :].partition_broadcast(128)), writes=["gbc"])
                    S.dma("pool", lambda e: e.dma_start(out=wlat[:], in_=w_in.rearrange("(c p) n -> p c n", p=128)[:, :, 1536:1920]),
                          writes=["wlat"])
                    for t in range(16):
                        xt = xts[t % 2]
                        hb = hbs[t % 2]
                        xk, hk = f"xt{t % 2}", f"hb{t % 2}"
                        sl = t % 2
                        S.dma("sp", lambda e: e.dma_start(out=xt[:], in_=x[b, t * 128:(t + 1) * 128, :]), writes=[xk])
                        S.op("dve", lambda e: e.memset(ssA[:, sl:sl + 1], 0.0), writes=[f"ssA{sl}"])
                        S.op("act", lambda e: e.activation(junk[:], xt[:], AF.Square, accum_out=ssA[:, sl:sl + 1]),
                             reads=[xk], writes=["junk", f"ssA{sl}"])
                        rstd_from_ss(rsA[:, sl:sl + 1], ssA[:, sl:sl + 1], 1024.0, f"rsA{sl}", f"ssA{sl}")
                        S.op("dve", lambda e: e.scalar_tensor_tensor(hb[:], xt[:], rsA[:, sl:sl + 1], gbc[:], ALU.mult, ALU.mult),
                             reads=[xk, f"rsA{sl}", "gbc"], writes=[hk])
                        transpose_to_hT(hb, hk, t, t % 2)

                    for G in range(4):
                        gs = slice(G * 512, (G + 1) * 512)
                        hTk = ("hT", G)
                        for lc in range(3):
                            bn = 2 + lc
                            for c in range(8):
                                S.op("pe", lambda e: e.matmul(bank(bn), wlat[:, c, lc * 128:(lc + 1) * 128], hT[:, c, gs],
                                                              start=(c == 0), stop=(c == 7)),
                                     reads=["wlat", hTk], writes=[bk(bn)])
                            S.op("act", lambda e: e.copy(latf[:, lc, :], bank(bn)), reads=[bk(bn)], writes=[("latf", lc)])
                            S.op("dve", lambda e: e.tensor_tensor(latsq[:, lc, :], latf[:, lc, :], latf[:, lc, :], ALU.mult),
                                 reads=[("latf", lc)], writes=[("latsq", lc)])
                        S.op("pe", lambda e: e.matmul(bank(5), ones_bf[:], latsq[:, 0, :], start=True, stop=False),
                             reads=["ones_bf", ("latsq", 0)], writes=[bk(5)])
                        S.op("pe", lambda e: e.matmul(bank(5), ones_bf[:], latsq[:, 1, :], start=False, stop=True),
                             reads=["ones_bf", ("latsq", 1)], writes=[bk(5)])
                        S.op("pe", lambda e: e.matmul(bank(6), ones_bf[:], latsq[:, 2, :], start=True, stop=True),
                             reads=["ones_bf", ("latsq", 2)], writes=[bk(6)])
                        for (ri, bnk_, nn) in ((0, 5, 256.0), (1, 6, 128.0)):
                            S.op("act", lambda e: e.activation(rbc[:, ri, :], bank(bnk_), AF.Ln, bias=epsc[:], scale=1.0 / nn),
                                 reads=[bk(bnk_), "epsc"], writes=[("rbc", ri)])
                            S.op("act", lambda e: e.activation(rbc[:, ri, :], rbc[:, ri, :], AF.Exp, scale=-0.5),
                                 reads=[("rbc", ri)], writes=[("rbc", ri)])
                        for lc in range(2):
                            S.op("dve", lambda e: e.tensor_tensor(cqTn[:, lc, gs], latf[:, lc, :], rbc[:, 0, :], ALU.mult),
                                 reads=[("latf", lc), ("rbc", 0)], writes=["cqTn"])
                        S.op("dve", lambda e: e.tensor_tensor(ckvTn[:, gs], latf[:, 2, :], rbc[:, 1, :], ALU.mult),
                             reads=[("latf", 2), ("rbc", 1)], writes=["ckvTn"])
                        P = slice(64, 96)
                        S.dma("sp", lambda e: e.dma_start(out=posi[P, :], in_=positions[b:b + 1, gs].partition_broadcast(32)),
                              writes=["posi"])
                        S.op("dve", lambda e: e.tensor_copy(ang[P, :], posi[P, :]), reads=["posi"], writes=["ang"])
                        S.op("dve", lambda e: e.tensor_scalar(ang[P, :], ang[P, :], invf_sb[P, 0:1], None, ALU.mult),
                             reads=["ang", "invf"], writes=["ang"])
                        for (dst, dk, add) in ((sinT, "sinT", 0.0), (cosT, "cosT", float(np.pi / 2))):
                            S.op("dve", lambda e: e.tensor_scalar(ru[P, :], ang[P, :], add, None, ALU.add),
                                 reads=["ang"], writes=["ru"])
                            S.op("dve", lambda e: e.tensor_scalar(rtmp[P, :], ru[P, :], 1.0 / TWO_PI, None, ALU.mult),
                                 reads=["ru"], writes=["rtmp"])
                            S.op("dve", lambda e: e.tensor_copy(posi[P, :], rtmp[P, :]), reads=["rtmp"], writes=["posi"])
                            S.op("dve", lambda e: e.tensor_copy(rtmp[P, :], posi[P, :]), reads=["posi"], writes=["rtmp"])
                            S.op("dve", lambda e: e.scalar_tensor_tensor(rtmp[P, :], rtmp[P, :], -TWO_PI, ru[P, :], ALU.mult, ALU.add),
                                 reads=["rtmp", "ru"], writes=["rtmp"])
                            S.op("dve", lambda e: e.tensor_scalar(rtmp[P, :], rtmp[P, :], float(np.pi), float(-np.pi), ALU.min, ALU.max),
                                 reads=["rtmp"], writes=["rtmp"])
                            S.op("act", lambda e: e.activation(dst[P, gs], rtmp[P, :], AF.Sin), reads=["rtmp"], writes=[(dk, G)])
                        for (wt, wk, bn) in ((wkr, "wkr", 7), (wkr2, "wkr2", 5)):
                            for c in range(8):
                                S.op("pe", lambda e: e.matmul(bank(bn)[0:96, :], wt[:, c, :], hT[:, c, gs], start=(c == 0), stop=(c == 7)),
                                     reads=[wk, hTk], writes=[bk(bn)])
                        S.op("dve", lambda e: e.tensor_tensor(ta[P, :], bank(7)[P, :], cosT[P, gs], ALU.mult),
                             reads=[bk(7), ("cosT", G)], writes=["ta"])
                        S.op("dve", lambda e: e.tensor_tensor(tb[P, :], bank(5)[P, :], sinT[P, gs], ALU.mult),
                             reads=[bk(5), ("sinT", G)], writes=["tb"])
                        S.op("dve", lambda e: e.tensor_tensor(krope[P, gs], ta[P, :], tb[P, :], ALU.add),
                             reads=["ta", "tb"], writes=["krope"])
                    S.barrier()

                if debug and b == 0:
                    S.dma("sp", lambda e: e.dma_start(out=dbg_hT[:, :, :], in_=hT[:]), reads=[("hT", g) for g in range(4)], writes=["dbg_hT"])
                    S.barrier()
                def run_passes(proj_chunks, loop_iters, npass):
                    for ch in proj_chunks(0):
                        ch()
                    for j in range(npass):
                        nxt = S.record(proj_chunks(j + 1)) if j + 1 < npass else []
                        iters = loop_iters(j)
                        per = -(-len(nxt) // max(1, len(iters) - 6))
                        for idx, it in enumerate(iters):
                            it()
                            if idx >= 2:
                                for _ in range(per):
                                    if nxt:
                                        nxt.pop(0)()
                        while nxt:
                            nxt.pop(0)()

                with ExitStack() as sp_:
                    wsb2 = [sb(sp_, f"wsb{i}", [128, 8, 3, 128], BF16) for i in range(2)]
                    qs02 = [[sb(sp_, f"qs0_{i}_{h}", [128, 2048], BF16) for h in range(2)] for i in range(2)]
                    ksT2 = [sb(sp_, f"ksT{i}", [128, 2048], BF16) for i in range(2)]
                    vs2 = [sb(sp_, f"vs{i}", [128, 16, 128], BF16) for i in range(2)]
                    E1 = [sb(sp_, f"E1_{i}", [128, 512]) for i in range(4)]
                    Lb = [sb(sp_, f"Lb_{i}", [128, 512], BF16) for i in range(3)]
                    Xe = [sb(sp_, f"Xe_{i}", [128, 512]) for i in range(2)]
                    At = [sb(sp_, f"At_{i}", [128, 512], BF16) for i in range(2)]
                    S32 = [sb(sp_, f"S32_{i}", [128, 512]) for i in range(2)]
                    Sb = [sb(sp_, f"Sb_{i}", [128, 512], BF16) for i in range(2)]
                    wi_c = w_in.rearrange("(c p) n -> p c n", p=128)

                    def sb_proj_chunks(j):
                        pj = j % 2
                        wsb, qs0, ksT, vs = wsb2[pj], qs02[pj], ksT2[pj], vs2[pj]
                        chunks = []

                        def c_load():
                            for w in range(3):
                                S.dma("pool", lambda e: e.dma_start(out=wsb[:, :, w, :],
                                                                    in_=wi_c[:, :, w * 512 + j * 128:w * 512 + (j + 1) * 128]),
                                      writes=[("wsb", pj, w)])
                            S.op("dve", lambda e: e.memset(qs0[0][64:128, :], 0.0), writes=[("qs0", pj, 0, G) for G in range(4)])
                            S.op("dve", lambda e: e.memset(qs0[1][0:64, :], 0.0), writes=[("qs0", pj, 1, G) for G in range(4)])
                        chunks.append(c_load)
                        for G in range(4):
                            gs = slice(G * 512, (G + 1) * 512)

                            def c_q(G=G, gs=gs):
                                for c in range(8):
                                    S.op("pe", lambda e: e.matmul(bank(6), wsb[:, c, 0, :], hT[:, c, gs], start=(c == 0), stop=(c == 7)),
                                         reads=[("wsb", pj, 0), ("hT", G)], writes=[bk(6)])
                                S.op("dve", lambda e: e.tensor_scalar(qs0[0][0:64, gs], bank(6)[0:64, :], 0.125, None, ALU.mult),
                                     reads=[bk(6)], writes=[("qs0", pj, 0, G)])
                                S.op("dve", lambda e: e.tensor_scalar(qs0[1][64:128, gs], bank(6)[64:128, :], 0.125, None, ALU.mult),
                                     reads=[bk(6)], writes=[("qs0", pj, 1, G)])

                            def c_k(G=G, gs=gs):
                                for c in range(8):
                                    S.op("pe", lambda e: e.matmul(bank(7), wsb[:, c, 1, :], hT[:, c, gs], start=(c == 0), stop=(c == 7)),
                                         reads=[("wsb", pj, 1), ("hT", G)], writes=[bk(7)])
                                S.op("dve", lambda e: e.tensor_copy(ksT[:, gs], bank(7)), reads=[bk(7)], writes=[("ksT", pj, G)])

                            def c_v(G=G):
                                bn = 6 + (G % 2)
                                for tl in range(4):
                                    t = G * 4 + tl
                                    for c in range(8):
                                        S.op("pe", lambda e: e.matmul(bank(bn)[:, tl * 128:(tl + 1) * 128], hT[:, c, t * 128:(t + 1) * 128],
                                                                      wsb[:, c, 2, :], start=(c == 0), stop=(c == 7)),
                                             reads=[("wsb", pj, 2), ("hT", G)], writes=[bk(bn)])
                                S.op("dve", lambda e: e.tensor_copy(vs[:, G * 4:(G + 1) * 4, :], bank(bn).rearrange("p (t n) -> p t n", t=4)),
                                     reads=[bk(bn)], writes=[("vs", pj, G)])
                            chunks += [c_q, c_k, c_v]
                        return chunks

                    def sb_loop_iters(j):
                        pj = j % 2
                        qs0, ksT, vs = qs02[pj], ksT2[pj], vs2[pj]
                        units = [(hl, G, i) for G in range(4) for i in range(4 * G + 3, -1, -1) for hl in range(2)]
                        nU = len(units)

                        def geom(u):
                            hl, G, i = units[u]
                            q0 = max(i, 4 * G) * 128
                            off = q0 - G * 512
                            return dict(hl=hl, G=G, i=i, off=off, cs=slice(off, 512), qsl=slice(q0, (G + 1) * 512),
                                        ksl=slice(i * 128, (i + 1) * 128), diag=(i >= 4 * G), pr=slice(hl * 64, (hl + 1) * 64),
                                        first=(i == 4 * G + 3), last=(i == 0), gs=slice(G * 512, (G + 1) * 512))

                        def st0(u):
                            g = geom(u)
                            zb = u % 2
                            S.op("pe", lambda e: e.matmul(bank(zb)[:, g["cs"]], ksT[:, g["ksl"]], qs0[g["hl"]][:, g["qsl"]], start=True, stop=True),
                                 reads=[("ksT", pj, g["i"] // 4), ("qs0", pj, g["hl"], g["G"])], writes=[bk(zb)])

                        def st1a(u):
                            g = geom(u)
                            zb, cs, off = u % 2, g["cs"], g["off"]
                            e1, e1k = E1[u % 4], f"E1_{u % 4}"
                            S.op("act", lambda e: e.activation(e1[:, cs], bank(zb)[:, cs], AF.Exp), reads=[bk(zb)], writes=[e1k])
                            if g["diag"]:
                                S.op("dve", lambda e: e.tensor_tensor(e1[:, off:off + 128], e1[:, off:off + 128], mstrict, ALU.mult),
                                     reads=[e1k, "cst"], writes=[e1k])

                        def st1b(u):
                            g = geom(u)
                            cs = g["cs"]
                            e1, lb = E1[u % 4], Lb[u % 3]
                            e1k, lbk = f"E1_{u % 4}", f"Lb_{u % 3}"
                            S.op("act", lambda e: e.activation(lb[:, cs], e1[:, cs], AF.Ln, bias=onec[:], scale=1.0),
                                 reads=[e1k, "onec"], writes=[lbk])

                        def st2a(u):
                            g = geom(u)
                            cs, hl = g["cs"], g["hl"]
                            cbk = 2 + u % 2
                            lb, xe = Lb[u % 3], Xe[u % 2]
                            lbk, xek = f"Lb_{u % 3}", f"Xe_{u % 2}"
                            S.op("pe", lambda e: e.matmul(bank(cbk)[:, cs], tri, lb[:, cs], start=True, stop=g["first"]),
                                 reads=["cst", lbk], writes=[bk(cbk)])
                            if not g["first"]:
                                S.op("pe", lambda e: e.matmul(bank(cbk)[:, cs], ones_bf[:], Sb[hl][:, cs], start=False, stop=True),
                                     reads=["ones_bf", f"Sb_{hl}"], writes=[bk(cbk)])
                            S.op("act", lambda e: e.activation(xe[:, cs], bank(cbk)[:, cs], AF.Exp, scale=-1.0), reads=[bk(cbk)], writes=[xek])

                        def st2b(u):
                            g = geom(u)
                            cs, hl = g["cs"], g["hl"]
                            e1, lb, xe, at = E1[u % 4], Lb[u % 3], Xe[u % 2], At[u % 2]
                            e1k, lbk, xek, atk = f"E1_{u % 4}", f"Lb_{u % 3}", f"Xe_{u % 2}", f"At_{u % 2}"
                            if not g["last"]:
                                if g["first"]:
                                    S.op("dve", lambda e: e.memset(S32[hl][:], 0.0), writes=[f"S32_{hl}"])
                                S.op("dve", lambda e: e.tensor_tensor(S32[hl][:, cs], S32[hl][:, cs], lb[:, cs], ALU.add),
                                     reads=[f"S32_{hl}", lbk], writes=[f"S32_{hl}"])
                            S.op("dve", lambda e: e.tensor_tensor(at[:, cs], xe[:, cs], e1[:, cs], ALU.mult), reads=[xek, e1k], writes=[atk])
                            if not g["last"]:
                                S.op("dve", lambda e: e.tensor_copy(Sb[hl][:], S32[hl][:]), reads=[f"S32_{hl}"], writes=[f"Sb_{hl}"])

                        def st3(u):
                            g = geom(u)
                            cs = g["cs"]
                            ob = 4 + g["hl"]
                            at, atk = At[u % 2], f"At_{u % 2}"
                            S.op("pe", lambda e: e.matmul(bank(ob)[0:64, cs], vs[:, g["i"], g["pr"]], at[:, cs], start=g["first"], stop=g["last"],
                                                          skip_group_check=True),
                                 reads=[("vs", pj, g["i"] // 4), atk], writes=[bk(ob)])
                            if g["last"]:
                                S.op("act", lambda e: e.copy(oT[g["pr"], j, g["gs"]], bank(ob)[0:64, :]), reads=[bk(ob)], writes=[("oT", j)])

                        def mk(k):
                            def it():
                                if 0 <= k < nU:
                                    st1b(k)
                                if 0 <= k + 1 < nU:
                                    st1a(k + 1)
                                if 0 <= k - 1 < nU:
                                    st2a(k - 1)
                                if 0 <= k + 2 < nU:
                                    st0(k + 2)
                                if 0 <= k - 1 < nU:
                                    st2b(k - 1)
                                if 0 <= k - 2 < nU:
                                    st3(k - 2)
                            return it
                        return [mk(k) for k in range(-2, nU + 2)]

                    run_passes(sb_proj_chunks, sb_loop_iters, 4)
                    S.barrier()

                with ExitStack() as sm:
                    qmT2 = [sb(sm, f"qmT{i}", [128, 2, 2048], BF16) for i in range(2)]
                    kmT2 = [sb(sm, f"kmT{i}", [128, 2, 2048], BF16) for i in range(2)]
                    vm2 = [sb(sm, f"vm{i}", [128, 16, 2, 65], BF16) for i in range(2)]
                    Et = [sb(sm, f"Et_{i}", [128, 512], BF16) for i in range(3)]
                    dr = sb(sm, "dr", [128, 512])
                    rb = sb(sm, "rb", [128, 512])
                    ta = sb(sm, "ta", [128, 512])
                    tb = sb(sm, "tb", [128, 512])
                    P = slice(64, 96)
                    scale = float(96 ** -0.5)

                    def mla_proj_chunks(m):
                        pm = m % 2
                        qmT, kmT, vm = qmT2[pm], kmT2[pm], vm2[pm]
                        chunks = []

                        def c_init():
                            S.op("dve", lambda e: e.memset(vm[:], 1.0), writes=[("vm", pm, G) for G in range(4)])
                            S.op("dve", lambda e: e.memset(qmT[96:128], 0.0), writes=[("qmT", pm, G) for G in range(4)])
                            S.op("dve", lambda e: e.memset(kmT[96:128], 0.0), writes=[("kmT", pm, G) for G in range(4)])
                        chunks.append(c_init)
                        for G in range(4):
                            gs = slice(G * 512, (G + 1) * 512)
                            for hl in range(2):
                                def c_qk(G=G, gs=gs, hl=hl):
                                    h = 2 * m + hl
                                    for (wt, wk, bn) in ((w_uq_sb, "w_uq", 6), (w2_sb, "w2", 7)):
                                        for c in range(2):
                                            S.op("pe", lambda e: e.matmul(bank(bn)[0:96, :], wt[:, c, h, :], cqTn[:, c, gs], start=(c == 0), stop=(c == 1)),
                                                 reads=[wk, "cqTn"], writes=[bk(bn)])
                                    S.op("dve", lambda e: e.tensor_copy(qmT[0:64, hl, gs], bank(6)[0:64, :]), reads=[bk(6)], writes=[("qmT", pm, G)])
                                    S.op("dve", lambda e: e.tensor_tensor(ta[P, :], bank(6)[P, :], cosT[P, gs], ALU.mult),
                                         reads=[bk(6), ("cosT", G)], writes=["ta"])
                                    S.op("dve", lambda e: e.tensor_tensor(tb[P, :], bank(7)[P, :], sinT[P, gs], ALU.mult),
                                         reads=[bk(7), ("sinT", G)], writes=["tb"])
                                    S.op("dve", lambda e: e.tensor_tensor(qmT[P, hl, gs], ta[P, :], tb[P, :], ALU.add),
                                         reads=["ta", "tb"], writes=[("qmT", pm, G)])
                                    S.op("pe", lambda e: e.matmul(bank(6)[0:64, :], w_ukv_sb[:, h * 128:h * 128 + 64], ckvTn[:, gs], start=True, stop=True),
                                         reads=["w_ukv", "ckvTn"], writes=[bk(6)])
                                    S.op("dve", lambda e: e.tensor_copy(kmT[0:64, hl, gs], bank(6)[0:64, :]), reads=[bk(6)], writes=[("kmT", pm, G)])
                                    S.op("dve", lambda e: e.tensor_copy(kmT[P, hl, gs], krope[P, gs]), reads=["krope"], writes=[("kmT", pm, G)])
                                chunks.append(c_qk)

                            def c_v(G=G):
                                bn = 7
                                for tl in range(4):
                                    t = G * 4 + tl
                                    S.op("pe", lambda e: e.matmul(bank(bn)[:, tl * 128:(tl + 1) * 128], ckvTn[:, t * 128:(t + 1) * 128],
                                                                  wv_sb[:, 2 * m:2 * m + 2, :], start=True, stop=True),
                                         reads=["wv", "ckvTn"], writes=[bk(bn)])
                                S.op("dve", lambda e: e.tensor_copy(vm[:, G * 4:(G + 1) * 4, :, 0:64],
                                                                    bank(bn).rearrange("p (t h d) -> p t h d", t=4, h=2)),
                                     reads=[bk(bn)], writes=[("vm", pm, G)])
                            chunks.append(c_v)
                        return chunks

                    def mla_loop_iters(m):
                        pm = m % 2
                        qmT, kmT, vm = qmT2[pm], kmT2[pm], vm2[pm]
                        units = [(hl, G, i) for G in range(4) for i in range(0, 4 * G + 4) for hl in range(2)]
                        nU = len(units)

                        def geom(u):
                            hl, G, i = units[u]
                            q0 = max(i, 4 * G) * 128
                            off = q0 - G * 512
                            return dict(hl=hl, G=G, i=i, off=off, cs=slice(off, 512), qsl=slice(q0, (G + 1) * 512),
                                        ksl=slice(i * 128, (i + 1) * 128), diag=(i >= 4 * G), pr=slice(hl * 64, (hl + 1) * 64),
                                        first=(i == 0), last=(i == 4 * G + 3), gs=slice(G * 512, (G + 1) * 512))

                        def st0(u):
                            g = geom(u)
                            zb = u % 3
                            S.op("pe", lambda e: e.matmul(bank(zb)[:, g["cs"]], kmT[:, g["hl"], g["ksl"]], qmT[:, g["hl"], g["qsl"]],
                                                          start=True, stop=True),
                                 reads=[("kmT", pm, g["i"] // 4), ("qmT", pm, g["G"])], writes=[bk(zb)])

                        def st1(u):
                            g = geom(u)
                            zb, cs, off = u % 3, g["cs"], g["off"]
                            et, etk = Et[u % 3], f"Et_{u % 3}"
                            S.op("act", lambda e: e.activation(et[:, cs], bank(zb)[:, cs], AF.Exp, scale=scale), reads=[bk(zb)], writes=[etk])
                            if g["diag"]:
                                S.op("dve", lambda e: e.tensor_tensor(et[:, off:off + 128], et[:, off:off + 128], mcausal, ALU.mult),
                                     reads=[etk, "cst"], writes=[etk])

                        def st2(u):
                            g = geom(u)
                            cs = g["cs"]
                            ob = 4 + g["hl"]
                            et, etk = Et[u % 3], f"Et_{u % 3}"
                            S.op("pe", lambda e: e.matmul(bank(ob)[0:65, cs], vm[:, g["i"], g["hl"], :], et[:, cs], start=g["first"], stop=g["last"],
                                                          skip_group_check=True),
                                 reads=[("vm", pm, g["i"] // 4), etk], writes=[bk(ob)])
                            if g["last"]:
                                S.op("act", lambda e: e.activation(dr[64:65, :], bank(ob)[64:65, :], AF.Ln), reads=[bk(ob)], writes=["dr"])

                                def fin(ob=ob, pr=g["pr"], gs=g["gs"]):
                                    S.op("act", lambda e: e.activation(dr[64:65, :], dr[64:65, :], AF.Exp, scale=-1.0), reads=["dr"], writes=["dr"])
                                    S.op("pe", lambda e: e.matmul(bank(3)[0:64, :], ones_f[64:65, 0:64], dr[64:65, :], start=True, stop=True),
                                         reads=["ones_f", "dr"], writes=[bk(3)])
                                    S.op("act", lambda e: e.copy(rb[0:64, :], bank(3)[0:64, :]), reads=[bk(3)], writes=["rb"])
                                    S.op("dve", lambda e: e.tensor_tensor(oT[pr, 4 + m, gs], bank(ob)[0:64, :], rb[0:64, :], ALU.mult),
                                         reads=[bk(ob), "rb"], writes=[("oT", 4 + m)])
                                deferred.append(fin)

                        deferred = []

                        def mk(k):
                            def it():
                                if 0 <= k + 1 < nU:
                                    st0(k + 1)
                                if 0 <= k < nU:
                                    st1(k)
                                pend = list(deferred)
                                del deferred[:]
                                for f in pend:
                                    f()
                                if 0 <= k - 1 < nU:
                                    st2(k - 1)
                                if k >= nU:
                                    for f in list(deferred):
                                        f()
                                    del deferred[:]
                            return it
                        return [mk(k) for k in range(-1, nU + 1)]

                    run_passes(mla_proj_chunks, mla_loop_iters, 4)
                    S.barrier()

                if debug and b == 0:
                    S.dma("sp", lambda e: e.dma_start(out=dbg_oT[:, :, :], in_=oT[:]), reads=[("oT", jj) for jj in range(8)], writes=["dbg_oT"])
                    S.dma("sp", lambda e: e.dma_start(out=dbg_lat[:, 0:2, :], in_=cqTn[:]), reads=["cqTn"], writes=["dbg_lat0"])
                    S.dma("sp", lambda e: e.dma_start(out=dbg_lat[:, 2, :], in_=ckvTn[:]), reads=["ckvTn"], writes=["dbg_lat1"])
                    S.dma("sp", lambda e: e.dma_start(out=dbg_lat[64:96, 3, :], in_=krope[64:96, :]), reads=["krope"], writes=["dbg_lat2"])
                    S.barrier()
                with ExitStack() as sd:
                    xts = [sb(sd, f"xd{i}", [128, 1024]) for i in range(2)]
                    x1t = [sb(sd, f"x1t{i}", [128, 1024]) for i in range(2)]
                    h2b = [sb(sd, f"h2b{i}", [128, 1024], BF16) for i in range(2)]
                    junk = sb(sd, "junkd", [128, 1024], BF16)
                    osq2 = [sb(sd, f"osq{i}", [128, 8, 128], BF16) for i in range(2)]
                    rsDall = sb(sd, "rsDall", [128, 32])
                    ssD = sb(sd, "ssD", [128, 2])
                    rsD = sb(sd, "rsD", [128, 2])
                    ss2 = sb(sd, "ss2", [128, 1])
                    rs2 = sb(sd, "rs2", [128, 1])
                    lgall = sb(sd, "lgall", [128, 16, 36])
                    lgraw = sb(sd, "lgraw", [128, 16, 36])
                    ss2all = sb(sd, "ss2all", [128, 16])
                    gmax = sb(sd, "gmax", [128, 16, 1])
                    ohg = sb(sd, "ohg", [128, 16, 4])
                    gsh = sb(sd, "gsh", [128, 16, 4])
                    gex = sb(sd, "gex", [128, 16, 4])
                    gsum = sb(sd, "gsum", [128, 16, 1])
                    gval = sb(sd, "gval", [128, 16, 1])
                    tmp4 = sb(sd, "tmp4", [128, 16, 4, 8])
                    loc = sb(sd, "loc", [128, 16, 8])
                    loc2 = sb(sd, "loc2", [128, 16, 8])
                    l1 = sb(sd, "l1", [128, 16, 1])
                    l2 = sb(sd, "l2", [128, 16, 1])
                    m1 = sb(sd, "m1", [128, 16, 8])
                    m2 = sb(sd, "m2", [128, 16, 8])
                    dd = sb(sd, "dd", [128, 16, 1])
                    s1 = sb(sd, "s1", [128, 16, 1])
                    w1 = sb(sd, "w1", [128, 16, 1])
                    w2 = sb(sd, "w2", [128, 16, 1])
                    wl = sb(sd, "wl", [128, 16, 8])
                    wl2 = sb(sd, "wl2", [128, 16, 8])
                    gbc = sb(sd, "gbc", [128, 1024])
                    S.dma("sp", lambda e: e.dma_start(out=gbc[:], in_=ffn_norm[0:1, :].partition_broadcast(128)), writes=["gbc"])
                    def d_partA(t):
                        ts_ = slice(t * 128, (t + 1) * 128)
                        xt, xk = xts[t % 2], f"xd{t % 2}"
                        x1, x1k = x1t[t % 2], f"x1t{t % 2}"
                        hb, hk = h2b[t % 2], f"h2b{t % 2}"
                        S.dma("sp", lambda e: e.dma_start(out=xt[:], in_=x[b, ts_, :]), writes=[xk])
                        for grp in range(2):
                            for hh in range(2):
                                bn = grp * 2 + hh
                                for jl in range(4):
                                    jj = grp * 4 + jl
                                    S.op("pe", lambda e: e.matmul(bank(bn), oT[:, jj, ts_], w_out_sb[:, jj, hh * 512:(hh + 1) * 512],
                                                                  start=(jl == 0), stop=(jl == 3)),
                                         reads=[("oT", jj), "w_out"], writes=[bk(bn)])
                        S.op("dve", lambda e: e.scalar_tensor_tensor(x1[:], PS[0][:], rsDall[:, 2 * t:2 * t + 1], xt[:], ALU.mult, ALU.add),
                             reads=[bk(0), bk(1), "rsDall", xk], writes=[x1k])
                        S.op("dve", lambda e: e.scalar_tensor_tensor(x1[:], PS[1][:], rsDall[:, 2 * t + 1:2 * t + 2], x1[:], ALU.mult, ALU.add),
                             reads=[bk(2), bk(3), "rsDall", x1k], writes=[x1k])
                        S.dma("sp", lambda e: e.dma_start(out=x1s[b, ts_, :], in_=x1[:]), reads=[x1k], writes=[("x1s", b, t)])
                        S.op("act", lambda e: e.activation(junk[:], x1[:], AF.Square, accum_out=ss2all[:, t:t + 1]),
                             reads=[x1k, "ss2init"], writes=["junkd", ("ss2all", t)])
                        S.op("dve", lambda e: e.tensor_tensor(hb[:], x1[:], gbc[:], ALU.mult), reads=[x1k, "gbc"], writes=[hk])

                    def d_partB(t):
                        ts_ = slice(t * 128, (t + 1) * 128)
                        x1, x1k = x1t[t % 2], f"x1t{t % 2}"
                        hb, hk = h2b[t % 2], f"h2b{t % 2}"
                        transpose_to_hT(hb, hk, t, 5)
                        for c in range(8):
                            S.op("pe", lambda e: e.matmul(bank(6)[:, 0:36], hT[:, c, ts_], wr_sb[:, c, :], start=(c == 0), stop=(c == 7)),
                                 reads=[("hT", t // 4), ("hTt", t), "wr"], writes=[bk(6)])
                        S.op("dve", lambda e: e.tensor_copy(lgraw[:, t, :], bank(6)[:, 0:36]), reads=[bk(6)], writes=["lgraw"])

                    S.op("dve", lambda e: e.memset(ss2all[:], 0.0), writes=["ss2init"] + [("ss2all", t) for t in range(16)])
                    for t in range(16):
                        ts_ = slice(t * 128, (t + 1) * 128)
                        oq, oqk = osq2[t % 2], f"osq{t % 2}"
                        S.op("dve", lambda e: e.tensor_tensor(oq[:], oT[:, :, ts_], oT[:, :, ts_], ALU.mult),
                             reads=[("oT", jj) for jj in range(8)], writes=[oqk])
                        for grp in range(2):
                            for jl in range(4):
                                S.op("pe", lambda e: e.matmul(bank(4)[:, 2 * t + grp:2 * t + grp + 1], oq[:, grp * 4 + jl, :], ones_bf[:, 0:1],
                                                              start=(t == 0 and jl == 0 and grp == 0), stop=(t == 15 and grp == 1 and jl == 3),
                                                              skip_group_check=True),
                                     reads=[oqk, "ones_bf"], writes=[bk(4)])
                    rstd_from_ss(rsDall[:], bank(4)[:, 0:32], 512.0, "rsDall", bk(4))
                    for t in range(17):
                        if t < 16:
                            d_partA(t)
                        if t >= 1:
                            d_partB(t - 1)
                    S.op("act", lambda e: e.activation(rs2all[:], ss2all[:], AF.Sqrt, bias=epsc[:], scale=1.0 / 1024.0),
                         reads=[("ss2all", t) for t in range(16)] + ["epsc"], writes=["rs2all"])
                    S.op("dve", lambda e: e.reciprocal(rs2all[:], rs2all[:]), reads=["rs2all"], writes=["rs2all"])
                    rs2b = rs2all[:].rearrange("p (t o) -> p t o", o=1)
                    S.op("dve", lambda e: e.tensor_tensor(lgall[:], lgraw[:], rs2b.to_broadcast([128, 16, 36]), ALU.mult),
                         reads=["lgraw", "rs2all"], writes=["lgall"])
                    S.op("dve", lambda e: e.tensor_tensor(lgall[:], lgall[:], br_bc[:].rearrange("p (o n) -> p o n", o=1).to_broadcast([128, 16, 36]), ALU.add),
                         reads=["lgall", "br"], writes=["lgall"])
                    T4 = [128, 16, 4]
                    T8 = [128, 16, 8]
                    T48 = [128, 16, 4, 8]
                    glg = lgall[:, :, 0:4]
                    elg = lgall[:, :, 4:36].rearrange("p t (g k) -> p t g k", g=4)
                    ohg4 = ohg[:].rearrange("p t (g o) -> p t g o", o=1)
                    V = lambda fn, r, w: S.op("dve", fn, reads=r, writes=w)
                    V(lambda e: e.reduce_max(gmax[:], glg, axis=AX.X), ["lgall"], ["gmax"])
                    V(lambda e: e.tensor_tensor(ohg[:], glg, gmax[:].to_broadcast(T4), ALU.is_equal), ["lgall", "gmax"], ["ohg"])
                    V(lambda e: e.tensor_tensor(gsh[:], glg, gmax[:].to_broadcast(T4), ALU.subtract), ["lgall", "gmax"], ["gsh"])
                    S.op("act", lambda e: e.activation(gex[:], gsh[:], AF.Exp), reads=["gsh"], writes=["gex"])
                    V(lambda e: e.reduce_sum(gsum[:], gex[:], axis=AX.X), ["gex"], ["gsum"])
                    V(lambda e: e.reciprocal(gval[:], gsum[:]), ["gsum"], ["gval"])
                    V(lambda e: e.tensor_tensor(tmp4[:], elg, ohg4.to_broadcast(T48), ALU.mult), ["lgall", "ohg"], ["tmp4"])
                    V(lambda e: e.reduce_sum(loc[:].rearrange("p t (k o) -> p t k o", o=1), tmp4[:].rearrange("p t g k -> p t k g"), axis=AX.X),
                      ["tmp4"], ["loc"])
                    V(lambda e: e.reduce_max(l1[:], loc[:], axis=AX.X), ["loc"], ["l1"])
                    V(lambda e: e.tensor_tensor(m1[:], loc[:], l1[:].to_broadcast(T8), ALU.is_equal), ["loc", "l1"], ["m1"])
                    V(lambda e: e.scalar_tensor_tensor(loc2[:], m1[:], -1e30, loc[:], ALU.mult, ALU.add), ["m1", "loc"], ["loc2"])
                    V(lambda e: e.reduce_max(l2[:], loc2[:], axis=AX.X), ["loc2"], ["l2"])
                    V(lambda e: e.tensor_tensor(m2[:], loc2[:], l2[:].to_broadcast(T8), ALU.is_equal), ["loc2", "l2"], ["m2"])
                    V(lambda e: e.tensor_tensor(dd[:], l2[:], l1[:], ALU.subtract), ["l1", "l2"], ["dd"])
                    S.op("act", lambda e: e.activation(s1[:], dd[:], AF.Exp), reads=["dd"], writes=["s1"])
                    V(lambda e: e.tensor_scalar(s1[:], s1[:], 1.0, None, ALU.add), ["s1"], ["s1"])
                    V(lambda e: e.reciprocal(s1[:], s1[:]), ["s1"], ["s1"])
                    V(lambda e: e.tensor_tensor(w1[:], gval[:], s1[:], ALU.mult), ["gval", "s1"], ["w1"])
                    V(lambda e: e.tensor_tensor(w2[:], gval[:], w1[:], ALU.subtract), ["gval", "w1"], ["w2"])
                    V(lambda e: e.tensor_tensor(wl[:], m1[:], w1[:].to_broadcast(T8), ALU.mult), ["m1", "w1"], ["wl"])
                    V(lambda e: e.tensor_tensor(wl2[:], m2[:], w2[:].to_broadcast(T8), ALU.mult), ["m2", "w2"], ["wl2"])
                    V(lambda e: e.tensor_tensor(wl[:], wl[:], wl2[:], ALU.add), ["wl", "wl2"], ["wl"])
                    V(lambda e: e.tensor_tensor(Wfull[:].rearrange("p t (g k) -> p t g k", g=4), ohg4.to_broadcast(T48),
                                                wl[:].rearrange("p t (o k) -> p t o k", o=1).to_broadcast(T48), ALU.mult),
                      ["ohg", "wl"], [("Wfull", t) for t in range(16)])
                    V(lambda e: e.tensor_tensor(Wfull[:], Wfull[:], rs2b.to_broadcast([128, 16, 32]), ALU.mult),
                      [("Wfull", t) for t in range(16)] + ["rs2all"], [("Wfull", t) for t in range(16)])
                    S.barrier()

            if "moe" in phases:
              with ExitStack() as s2:
                acc = sb(s2, "acc", [128, 16, 1024])
                NW = 3
                wgu = [sb(s2, f"wgu{i}", [128, 8, 512], BF16) for i in range(NW)]
                wd = [sb(s2, f"wd{i}", [128, 2, 1024], BF16) for i in range(NW)]
                sg = [sb(s2, f"sg{i}", [128, 256]) for i in range(2)]
                hid = [sb(s2, f"hid{i}", [128, 256], BF16) for i in range(2)]
                hidT = [sb(s2, f"hidT{i}", [128, 256], BF16) for i in range(2)]
                junk = sb(s2, "junke", [128, 1024], BF16)
                ss3 = sb(s2, "ss3", [128, 2])
                rs3 = sb(s2, "rs3", [128, 2])
                yt = [sb(s2, f"yt{i}", [128, 1024]) for i in range(2)]
                gbc = sb(s2, "gbc", [128, 1024])
                S.dma("sp", lambda e: e.dma_start(out=gbc[:], in_=final_norm[0:1, :].partition_broadcast(128)), writes=["gbc"])
                for t in range(16):
                    S.dma("sp", lambda e: e.dma_start(out=acc[:, t, :], in_=x1s[b, t * 128:(t + 1) * 128, :]),
                          reads=[("x1s", b, t)], writes=[("acc", t)])

                def load_expert(ex):
                    sl = ex % NW
                    S.dma("pool", lambda e: e.dma_start(out=wgu[sl][:, :, 0:256], in_=w_gate[ex].rearrange("(c p) n -> p c n", p=128)),
                          writes=[f"wgu{sl}"])
                    S.dma("pool", lambda e: e.dma_start(out=wgu[sl][:, :, 256:512], in_=w_up[ex].rearrange("(c p) n -> p c n", p=128)),
                          writes=[f"wgu{sl}"])
                    S.dma("pool", lambda e: e.dma_start(out=wd[sl][:], in_=w_down[ex].rearrange("(c p) n -> p c n", p=128)),
                          writes=[f"wd{sl}"])

                for ex0 in range(min(NW, n_experts)):
                    load_expert(ex0)
                steps = [(ex, t) for ex in range(n_experts) for t in range(16)]
                nS = len(steps)

                def m_gu(k):
                    ex, t = steps[k]
                    sl, p2, ts_ = ex % NW, k % 2, slice(t * 128, (t + 1) * 128)
                    gb = p2
                    for c in range(8):
                        S.op("pe", lambda e: e.matmul(bank(gb), hT[:, c, ts_], wgu[sl][:, c, :], start=(c == 0), stop=(c == 7)),
                             reads=[("hT", t // 4), ("hTt", t), f"wgu{sl}"], writes=[bk(gb)])
                    S.op("act", lambda e: e.activation(sg[p2][:], bank(gb)[:, 0:256], AF.Silu, scale=rs2all[:, t:t + 1]), reads=[bk(gb), "rs2all"], writes=[f"sg{p2}"])
                    S.op("dve", lambda e: e.scalar_tensor_tensor(hid[p2][:], bank(gb)[:, 256:512], Wfull[:, t, ex:ex + 1], sg[p2][:],
                                                                 ALU.mult, ALU.mult),
                         reads=[bk(gb), ("Wfull", t), f"sg{p2}"], writes=[f"hid{p2}"])

                def m_tr(k):
                    p2 = k % 2
                    tbk = 2 + p2
                    tb_ = bank_bf(tbk)
                    for c in range(2):
                        S.op("pe", lambda e: e.transpose(tb_[:, c * 128:(c + 1) * 128], hid[p2][:, c * 128:(c + 1) * 128], ident),
                             reads=[f"hid{p2}", "cst"], writes=[bk(tbk)])
                    S.op("act", lambda e: e.copy(hidT[p2][:], tb_[:, 0:256]), reads=[bk(tbk)], writes=[f"hidT{p2}"])

                def m_dn(k):
                    ex, t = steps[k]
                    sl, p2 = ex % NW, k % 2
                    dps = 2 + p2
                    for hh in range(2):
                        for c in range(2):
                            S.op("pe", lambda e: e.matmul(PS[dps][:, hh * 512:(hh + 1) * 512], hidT[p2][:, c * 128:(c + 1) * 128],
                                                          wd[sl][:, c, hh * 512:(hh + 1) * 512], start=(c == 0), stop=(c == 1)),
                                 reads=[f"hidT{p2}", f"wd{sl}"], writes=[bk(2 * dps + hh)])
                    S.op("dve", lambda e: e.tensor_tensor(acc[:, t, :], acc[:, t, :], PS[dps][:], ALU.add),
                         reads=[("acc", t), bk(2 * dps), bk(2 * dps + 1)], writes=[("acc", t)])
                    if t == 15 and ex + NW < n_experts:
                        load_expert(ex + NW)
                    if ex == n_experts - 1:
                        m_final(t)

                def m_final(t):
                    sl = t % 2
                    S.op("dve", lambda e: e.memset(ss3[:, sl:sl + 1], 0.0), writes=[f"ss3{sl}"])
                    S.op("act", lambda e: e.activation(junk[:], acc[:, t, :], AF.Square, accum_out=ss3[:, sl:sl + 1]),
                         reads=[("acc", t)], writes=["junke", f"ss3{sl}"])
                    rstd_from_ss(rs3[:, sl:sl + 1], ss3[:, sl:sl + 1], 1024.0, f"rs3{sl}", f"ss3{sl}")
                    S.op("dve", lambda e: e.scalar_tensor_tensor(yt[sl][:], acc[:, t, :], rs3[:, sl:sl + 1], gbc[:], ALU.mult, ALU.mult),
                         reads=[("acc", t), f"rs3{sl}", "gbc"], writes=[f"yt{sl}"])
                    S.dma("sp", lambda e: e.dma_start(out=y[b, t * 128:(t + 1) * 128, :], in_=yt[sl][:]), reads=[f"yt{sl}"], writes=[("y", b, t)])

                for k in range(nS + 2):
                    if k < nS:
                        m_gu(k)
                    if 0 <= k - 1 < nS:
                        m_tr(k - 1)
                    if 0 <= k - 2 < nS:
                        m_dn(k - 2)
                S.barrier()
        S.final_wait("sp")
        print("instructions emitted:", S.nins, {e: S.cnt[e] for e in ENGS}, S.ndma)
    return nc


def _consts():
    k = np.arange(128)[:, None]
    q = np.arange(128)[None, :]
    ident = (k == q)
    tri = (k >= q)
    strict = (k < q)
    causal = (k <= q)
    c = np.concatenate([ident, tri, strict, causal, strict], axis=1).astype(np.float32)
    half = 16
    inv_freq = (np.float32(10000.0) ** (-np.arange(half, dtype=np.float32) / np.float32(half))).astype(np.float32)
    invf = np.zeros((128, 1), np.float32)
    for p in range(64, 96):
        invf[p, 0] = inv_freq[(p - 64) % 16]
    return c.astype(ml_dtypes.bfloat16), invf


def make_in_maps(inputs, n_cores, nseq):
    f = lambda a: np.ascontiguousarray(np.asarray(a))
    cst, invf = _consts()
    shared = {
        "attn_norm": f(inputs["attn_norm"]).reshape(1, 1024),
        "w_in": f(inputs["w_in"]).reshape(1024, 1952),
        "q_norm": f(inputs["q_norm"]).reshape(256, 1),
        "w_uq": f(inputs["w_uq"]).reshape(256, 768),
        "kv_norm": f(inputs["kv_norm"]).reshape(128, 1),
        "w_ukv": f(inputs["w_ukv"]).reshape(128, 1024),
        "out_norm": np.concatenate([f(inputs["sb_out_norm"]).reshape(-1), f(inputs["mla_out_norm"]).reshape(-1)]).reshape(1024, 1),
        "w_out": f(inputs["w_out"]).reshape(1024, 1024),
        "ffn_norm": f(inputs["ffn_norm"]).reshape(1, 1024),
        "w_group_router": f(inputs["w_group_router"]).reshape(1024, 4),
        "b_group_router": f(inputs["b_group_router"]).reshape(1, 4),
        "w_expert_router": f(inputs["w_expert_router"]).reshape(1024, 32),
        "b_expert_router": f(inputs["b_expert_router"]).reshape(1, 32),
        "w_gate": f(inputs["w_gate"]).reshape(32, 1024, 256),
        "w_up": f(inputs["w_up"]).reshape(32, 1024, 256),
        "w_down": f(inputs["w_down"]).reshape(32, 256, 1024),
        "final_norm": f(inputs["final_norm"]).reshape(1, 1024),
        "consts": cst,
        "invf": invf,
    }
    xs = f(inputs["x"])
    ps = f(inputs["positions"]).astype(np.int32)
    maps = []
    for c in range(n_cores):
        m = dict(shared)
        m["x"] = xs[c * nseq:(c + 1) * nseq]
        m["positions"] = ps[c * nseq:(c + 1) * nseq]
        maps.append(m)
    return maps


def kernel(**inputs):
    n_cores = 8
    nseq = 4
    nc = build(NSEQ=nseq)
    maps = make_in_maps(inputs, n_cores, nseq)
    res = run_bass_kernel_spmd(nc, maps, core_ids=list(range(n_cores)))
    out = np.concatenate([np.asarray(r["y"]) for r in res.results], axis=0)
    return out.astype(np.float32)
```

```python
import numpy as np
import ml_dtypes
from contextlib import ExitStack
import concourse.bass as bass
import concourse.mybir as mybir
from concourse.bass_utils import run_bass_kernel_spmd

F32 = mybir.dt.float32
BF16 = mybir.dt.bfloat16
I32 = mybir.dt.int32
AF = mybir.ActivationFunctionType
ALU = mybir.AluOpType
AX = mybir.AxisListType

ENGS = ["pe", "act", "dve", "pool", "sp"]
DMA_ENGS = ["sp", "pool"]
ND = 8
EPS = 1e-6
TWO_PI = float(2 * np.pi)


class _Proxy:
    def __init__(self):
        self.call = None

    def __getattr__(self, name):
        def f(*a, **k):
            self.call = (name, a, k)
            return self
        return f


class Sched:
    def __init__(self, nc, es):
        self.nc = nc
        self.eng = {"pe": nc.tensor, "act": nc.scalar, "dve": nc.vector, "pool": nc.gpsimd, "sp": nc.sync}
        self.sem = {e: es.enter_context(nc.semaphore("c_" + e)) for e in ENGS}
        self.dsem = {e: [es.enter_context(nc.semaphore(f"d_{e}_{i}")) for i in range(ND)] for e in DMA_ENGS}
        self.cnt = {e: 0 for e in ENGS}
        self.ndma = {e: 0 for e in DMA_ENGS}
        self.dlast = {e: {} for e in DMA_ENGS}
        self.lastw = {}
        self.readers = {}
        self.seen = {e: {} for e in ENGS}
        self.nins = 0
        self.rec = None

    def record(self, chunk_fns):
        self.rec = []
        for f in chunk_fns:
            f()
        out, self.rec = self.rec, None
        return out

    def _semobj(self, key):
        return self.sem[key[1]] if key[0] == "c" else self.dsem[key[1]][key[2]]

    def _collect(self, reads, writes):
        toks = {}

        def add(k, v):
            if toks.get(k, 0) < v:
                toks[k] = v
        for r in reads:
            t = self.lastw.get(r)
            if t is not None:
                add(*t)
        for w in writes:
            t = self.lastw.get(w)
            if t is not None:
                add(*t)
            for k, v in self.readers.get(w, {}).items():
                add(k, v)
        return toks

    def _wait(self, eng, toks):
        seen = self.seen[eng]
        for k, v in toks.items():
            if k[0] == "c" and k[1] == eng and eng == "pe":
                continue
            if seen.get(k, 0) >= v:
                continue
            self.eng[eng].wait_ge(self._semobj(k), v)
            seen[k] = v
            self.nins += 1

    def _record(self, tok, reads, writes):
        for w in writes:
            self.lastw[w] = tok
            self.readers[w] = {}
        for r in reads:
            d = self.readers.setdefault(r, {})
            if d.get(tok[0], 0) < tok[1]:
                d[tok[0]] = tok[1]

    def op(self, eng, fn, reads=(), writes=()):
        if self.rec is not None:
            p = _Proxy()
            fn(p)
            name, a, k = p.call
            self.rec.append(lambda: self.op(eng, lambda e: getattr(e, name)(*a, **k), reads, writes))
            return
        toks = self._collect(reads, writes)
        self._wait(eng, toks)
        ins = fn(self.eng[eng])
        self.cnt[eng] += 1
        ins.then_inc(self.sem[eng], 1)
        self.nins += 1
        tok = (("c", eng), self.cnt[eng])
        self._record(tok, reads, writes)

    def dma(self, eng, fn, reads=(), writes=()):
        if self.rec is not None:
            p = _Proxy()
            fn(p)
            name, a, k = p.call
            self.rec.append(lambda: self.dma(eng, lambda e: getattr(e, name)(*a, **k), reads, writes))
            return
        n = self.ndma[eng]
        self.ndma[eng] += 1
        slot = n % ND
        val = 16 * (n // ND + 1)
        toks = self._collect(reads, writes)
        key = ("d", eng, slot)
        if val > 16:
            toks[key] = max(toks.get(key, 0), val - 16)
        self._wait(eng, toks)
        ins = fn(self.eng[eng])
        ins.then_inc(self.dsem[eng][slot], 16)
        self.nins += 1
        self.dlast[eng][slot] = val
        self._record((key, val), reads, writes)

    def barrier(self):
        toks = {}
        for e in ENGS:
            if self.cnt[e] > 0:
                toks[("c", e)] = self.cnt[e]
        for q in DMA_ENGS:
            for slot, val in self.dlast[q].items():
                toks[("d", q, slot)] = val
        for e in ENGS:
            t = {k: v for k, v in toks.items() if not (k[0] == "c" and k[1] == e)}
            self._wait(e, t)

    def final_wait(self, eng="sp"):
        toks = {}
        for q in DMA_ENGS:
            for slot, val in self.dlast[q].items():
                toks[("d", q, slot)] = val
        for e in ENGS:
            if self.cnt[e] > 0 and e != eng:
                toks[("c", e)] = self.cnt[e]
        self._wait(eng, toks)


def build(NSEQ=4, debug=False, n_experts=32, phases=("attn", "moe"), nd_sb=0, nd_mla=0):
    nc = bass.Bass("TRN2", target_bir_lowering=False)

    def din(name, shape, dtype=F32):
        return nc.dram_tensor(name, shape, dtype, kind="ExternalInput").ap()

    x = din("x", [NSEQ, 2048, 1024])
    positions = din("positions", [NSEQ, 2048], I32)
    attn_norm = din("attn_norm", [1, 1024])
    w_in = din("w_in", [1024, 1952])
    q_norm = din("q_norm", [256, 1])
    w_uq = din("w_uq", [256, 768])
    kv_norm = din("kv_norm", [128, 1])
    w_ukv = din("w_ukv", [128, 1024])
    out_norm = din("out_norm", [1024, 1])
    w_out = din("w_out", [1024, 1024])
    ffn_norm = din("ffn_norm", [1, 1024])
    w_gr = din("w_group_router", [1024, 4])
    b_gr = din("b_group_router", [1, 4])
    w_er = din("w_expert_router", [1024, 32])
    b_er = din("b_expert_router", [1, 32])
    w_gate = din("w_gate", [32, 1024, 256])
    w_up = din("w_up", [32, 1024, 256])
    w_down = din("w_down", [32, 256, 1024])
    final_norm = din("final_norm", [1, 1024])
    consts = din("consts", [128, 640], BF16)
    invf = din("invf", [128, 1])
    y = nc.dram_tensor("y", [NSEQ, 2048, 1024], F32, kind="ExternalOutput").ap()
    x1s = nc.dram_tensor("x1s", [NSEQ, 2048, 1024], F32,
                         kind="ExternalOutput" if debug else "Internal").ap()

    dbg_oT = nc.dram_tensor("dbg_oT", [128, 8, 2048], BF16, kind="ExternalOutput").ap() if debug else None
    dbg_hT = nc.dram_tensor("dbg_hT", [128, 8, 2048], BF16, kind="ExternalOutput").ap() if debug else None
    dbg_lat = nc.dram_tensor("dbg_lat", [128, 4, 2048], BF16, kind="ExternalOutput").ap() if debug else None

    with ExitStack() as es:
        S = Sched(nc, es)

        uid = [0]

        def sb(stack, name, shape, dtype=F32):
            uid[0] += 1
            return stack.enter_context(nc.sbuf_tensor(f"{name}_{uid[0]}", shape, dtype))

        PS = [es.enter_context(nc.psum_tensor(f"ps{i}", [128, 1024], F32)) for i in range(4)]
        PSB = [p[:].bitcast(BF16) for p in PS]

        def bank(k):
            return PS[k // 2][:, (k % 2) * 512:(k % 2) * 512 + 512]

        def bank_bf(k):
            return PSB[k // 2][:, (k % 2) * 1024:(k % 2) * 1024 + 1024]

        def bk(k):
            return f"B{k}"

        cst = sb(es, "cst", [128, 640], BF16)
        ident = cst[:, 0:128]
        tri = cst[:, 128:256]
        mstrict = cst[:, 256:384]
        mcausal = cst[:, 384:512]
        slow = cst[:, 512:640]
        ones_bf = sb(es, "ones_bf", [128, 128], BF16)
        ones_f = sb(es, "ones_f", [128, 64], F32)
        epsc = sb(es, "epsc", [128, 1])
        onec = sb(es, "onec", [128, 1])
        invf_sb = sb(es, "invf_sb", [128, 1])
        w_uq_sb = sb(es, "w_uq_sb", [128, 2, 8, 96], BF16)
        w2_sb = sb(es, "w2_sb", [128, 2, 8, 96], BF16)
        w_ukv_sb = sb(es, "w_ukv_sb", [128, 1024], BF16)
        wv_sb = sb(es, "wv_sb", [128, 8, 64], BF16)
        wkr = sb(es, "wkr", [128, 8, 96], BF16)
        wkr2 = sb(es, "wkr2", [128, 8, 96], BF16)
        w_out_sb = sb(es, "w_out_sb", [128, 8, 1024], BF16)
        wr_sb = sb(es, "wr_sb", [128, 8, 36], BF16)
        br_bc = sb(es, "br_bc", [128, 36])
        hT = sb(es, "hT", [128, 8, 2048], BF16)
        Wfull = sb(es, "Wfull", [128, 16, 32])
        rs2all = sb(es, "rs2all", [128, 16])

        S.dma("sp", lambda e: e.dma_start(out=cst[:], in_=consts[:, :]), writes=["cst"])
        S.dma("sp", lambda e: e.dma_start(out=invf_sb[:], in_=invf[:, :]), writes=["invf"])
        S.op("pool", lambda e: e.memset(ones_bf[:], 1.0), writes=["ones_bf"])
        S.op("pool", lambda e: e.memset(ones_f[:], 1.0), writes=["ones_f"])
        S.op("pool", lambda e: e.memset(epsc[:], EPS), writes=["epsc"])
        S.op("pool", lambda e: e.memset(onec[:], 1.0), writes=["onec"])
        S.op("pool", lambda e: e.memset(w2_sb[:], 0.0), writes=["w2"])
        S.op("pool", lambda e: e.memset(wkr[:], 0.0), writes=["wkr"])
        S.op("pool", lambda e: e.memset(wkr2[:], 0.0), writes=["wkr2"])
        with ExitStack() as ss:
            stg = sb(ss, "stg", [128, 2048])
            qn = sb(ss, "qn", [128, 2])
            kvn = sb(ss, "kvn", [128, 1])
            og = sb(ss, "og", [128, 8])
            S.dma("sp", lambda e: e.dma_start(out=stg[:, 0:1536].rearrange("p (c n) -> p c n", c=2),
                                              in_=w_uq.rearrange("(c p) n -> p c n", p=128)), writes=["stg"])
            for c in range(2):
                S.dma("sp", lambda e: e.dma_start(out=qn[:, c:c + 1], in_=q_norm[c * 128:(c + 1) * 128, :]), writes=["qn"])
            stv = stg[:, 0:1536].rearrange("p (c h d) -> p c h d", c=2, h=8)
            for c in range(2):
                S.op("dve", lambda e: e.tensor_scalar(w_uq_sb[:, c], stv[:, c], qn[:, c:c + 1], None, ALU.mult),
                     reads=["stg", "qn"], writes=["w_uq"])
                S.op("dve", lambda e: e.tensor_scalar(w2_sb[:, c, :, 64:80], stv[:, c, :, 80:96], qn[:, c:c + 1], -1.0,
                                                      ALU.mult, ALU.mult), reads=["stg", "qn"], writes=["w2"])
                S.op("dve", lambda e: e.tensor_scalar(w2_sb[:, c, :, 80:96], stv[:, c, :, 64:80], qn[:, c:c + 1], None,
                                                      ALU.mult), reads=["stg", "qn"], writes=["w2"])
            S.dma("sp", lambda e: e.dma_start(out=stg[:, 0:1024], in_=w_ukv[:, :]), writes=["stg"])
            S.dma("sp", lambda e: e.dma_start(out=kvn[:], in_=kv_norm[:, :]), writes=["kvn"])
            S.op("dve", lambda e: e.tensor_scalar(w_ukv_sb[:], stg[:, 0:1024], kvn[:, 0:1], None, ALU.mult),
                 reads=["stg", "kvn"], writes=["w_ukv"])
            S.op("dve", lambda e: e.tensor_scalar(wv_sb[:], stg[:, 0:1024].rearrange("p (h d) -> p h d", h=8)[:, :, 64:128],
                                                  kvn[:, 0:1], None, ALU.mult), reads=["stg", "kvn"], writes=["wv"])
            wi_c = w_in.rearrange("(c p) n -> p c n", p=128)
            S.dma("pool", lambda e: e.dma_start(out=wkr[:, :, 64:96], in_=wi_c[:, :, 1920:1952]), writes=["wkr"])
            S.dma("pool", lambda e: e.dma_start(out=wkr2[:, :, 80:96], in_=wi_c[:, :, 1920:1936]), writes=["wkr2"])
            S.dma("pool", lambda e: e.dma_start(out=wkr2[:, :, 64:80], in_=wi_c[:, :, 1936:1952]), writes=["wkr2"])
            S.op("pool", lambda e: e.tensor_scalar(wkr2[:, :, 64:80], wkr2[:, :, 64:80], -1.0, None, ALU.mult),
                 reads=["wkr2"], writes=["wkr2"])
            for j in range(8):
                S.dma("sp", lambda e: e.dma_start(out=og[:, j:j + 1], in_=out_norm[j * 128:(j + 1) * 128, :]), writes=["og"])
            wo_c = w_out.rearrange("(c p) n -> p c n", p=128)
            for jj in range(4):
                S.dma("sp", lambda e: e.dma_start(out=stg[:].rearrange("p (c n) -> p c n", c=2),
                                                  in_=wo_c[:, 2 * jj:2 * jj + 2, :]), writes=["stg"])
                for jl in range(2):
                    j = 2 * jj + jl
                    S.op("dve", lambda e: e.tensor_scalar(w_out_sb[:, j, :], stg[:, jl * 1024:(jl + 1) * 1024],
                                                          og[:, j:j + 1], None, ALU.mult),
                         reads=["stg", "og"], writes=["w_out"])
            S.dma("pool", lambda e: e.dma_start(out=wr_sb[:, :, 0:4], in_=w_gr.rearrange("(c p) n -> p c n", p=128)), writes=["wr"])
            S.dma("pool", lambda e: e.dma_start(out=wr_sb[:, :, 4:36], in_=w_er.rearrange("(c p) n -> p c n", p=128)), writes=["wr"])
            S.dma("sp", lambda e: e.dma_start(out=br_bc[:, 0:4], in_=b_gr[0:1, :].partition_broadcast(128)), writes=["br"])
            S.dma("sp", lambda e: e.dma_start(out=br_bc[:, 4:36], in_=b_er[0:1, :].partition_broadcast(128)), writes=["br"])
            S.barrier()

        def rstd_from_ss(rs, ss_ap, n, rkey, sskey):
            S.op("act", lambda e: e.activation(rs, ss_ap, AF.Sqrt, bias=epsc[:], scale=1.0 / n),
                 reads=[sskey, "epsc"], writes=[rkey])
            S.op("dve", lambda e: e.reciprocal(rs, rs), reads=[rkey], writes=[rkey])

        def transpose_to_hT(src_bf, srckey, t, bnk):
            pb = bank_bf(bnk)
            for c in range(8):
                S.op("pe", lambda e: e.transpose(pb[:, c * 128:(c + 1) * 128], src_bf[:, c * 128:(c + 1) * 128], ident),
                     reads=[srckey, "cst", ("hTt", t)] if c == 0 else [srckey, "cst"], writes=[bk(bnk)])
            S.op("act", lambda e: e.copy(hT[:, :, t * 128:(t + 1) * 128], pb.rearrange("p (c n) -> p c n", c=8)),
                 reads=[bk(bnk)], writes=[("hT", t // 4), ("hTt", t)])

        for b in range(NSEQ):
            if "attn" in phases:
              with ExitStack() as s1:
                cqTn = sb(s1, "cqTn", [128, 2, 2048], BF16)
                ckvTn = sb(s1, "ckvTn", [128, 2048], BF16)
                krope = sb(s1, "krope", [128, 2048], BF16)
                oT = sb(s1, "oT", [128, 8, 2048], BF16)
                sinT = sb(s1, "sinT", [128, 2048])
                cosT = sb(s1, "cosT", [128, 2048])

                with ExitStack() as sa:
                    xts = [sb(sa, f"xt{i}", [128, 1024]) for i in range(2)]
                    hbs = [sb(sa, f"hb{i}", [128, 1024], BF16) for i in range(2)]
                    junk = sb(sa, "junk", [128, 1024], BF16)
                    ssA = sb(sa, "ssA", [128, 2])
                    rsA = sb(sa, "rsA", [128, 2])
                    wlat = sb(sa, "wlat", [128, 8, 384], BF16)
                    latf = sb(sa, "latf", [128, 3, 512])
                    latsq = sb(sa, "latsq", [128, 3, 512], BF16)
                    rbc = sb(sa, "rbc", [128, 2, 512])
                    posi = sb(sa, "posi", [128, 512], I32)
                    ang = sb(sa, "ang", [128, 512])
                    rtmp = sb(sa, "rtmp", [128, 512])
                    ru = sb(sa, "ru", [128, 512])
                    ta = sb(sa, "ta", [128, 512])
                    tb = sb(sa, "tb", [128, 512])

                    gbc = sb(sa, "gbc", [128, 1024])
                    S.dma("sp", lambda e: e.dma_start(out=gbc[:], in_=attn_norm[0:1, :].partition_broadcast(128)), writes=["gbc"])
                    S.dma("pool", lambda e: e.dma_start(out=wlat[:], in_=w_in.rearrange("(c p) n -> p c n", p=128)[:, :, 1536:1920]),
                          writes=["wlat"])
                    for t in range(16):
                        xt = xts[t % 2]
                        hb = hbs[t % 2]
                        xk, hk = f"xt{t % 2}", f"hb{t % 2}"
                        sl = t % 2
                        S.dma("sp", lambda e: e.dma_start(out=xt[:], in_=x[b, t * 128:(t + 1) * 128, :]), writes=[xk])
                        S.op("dve", lambda e: e.memset(ssA[:, sl:sl + 1], 0.0), writes=[f"ssA{sl}"])
                        S.op("act", lambda e: e.activation(junk[:], xt[:], AF.Square, accum_out=ssA[:, sl:sl + 1]),
                             reads=[xk], writes=["junk", f"ssA{sl}"])
                        rstd_from_ss(rsA[:, sl:sl + 1], ssA[:, sl:sl + 1], 1024.0, f"rsA{sl}", f"ssA{sl}")
                        S.op("dve", lambda e: e.scalar_tensor_tensor(hb[:], xt[:], rsA[:, sl:sl + 1], gbc[:], ALU.mult, ALU.mult),
                             reads=[xk, f"rsA{sl}", "gbc"], writes=[hk])
                        transpose_to_hT(hb, hk, t, t % 2)

                    for G in range(4):
                        gs = slice(G * 512, (G + 1) * 512)
                        hTk = ("hT", G)
                        for lc in range(3):
                            bn = 2 + lc
                            for c in range(8):
                                S.op("pe", lambda e: e.matmul(bank(bn), wlat[:, c, lc * 128:(lc + 1) * 128], hT[:, c, gs],
                                                              start=(c == 0), stop=(c == 7)),
                                     reads=["wlat", hTk], writes=[bk(bn)])
                            S.op("act", lambda e: e.copy(latf[:, lc, :], bank(bn)), reads=[bk(bn)], writes=[("latf", lc)])
                            S.op("dve", lambda e: e.tensor_tensor(latsq[:, lc, :], latf[:, lc, :], latf[:, lc, :], ALU.mult),
                                 reads=[("latf", lc)], writes=[("latsq", lc)])
                        S.op("pe", lambda e: e.matmul(bank(5), ones_bf[:], latsq[:, 0, :], start=True, stop=False),
                             reads=["ones_bf", ("latsq", 0)], writes=[bk(5)])
                        S.op("pe", lambda e: e.matmul(bank(5), ones_bf[:], latsq[:, 1, :], start=False, stop=True),
                             reads=["ones_bf", ("latsq", 1)], writes=[bk(5)])
                        S.op("pe", lambda e: e.matmul(bank(6), ones_bf[:], latsq[:, 2, :], start=True, stop=True),
                             reads=["ones_bf", ("latsq", 2)], writes=[bk(6)])
                        for (ri, bnk_, nn) in ((0, 5, 256.0), (1, 6, 128.0)):
                            S.op("act", lambda e: e.activation(rbc[:, ri, :], bank(bnk_), AF.Ln, bias=epsc[:], scale=1.0 / nn),
                                 reads=[bk(bnk_), "epsc"], writes=[("rbc", ri)])
                            S.op("act", lambda e: e.activation(rbc[:, ri, :], rbc[:, ri, :], AF.Exp, scale=-0.5),
                                 reads=[("rbc", ri)], writes=[("rbc", ri)])
                        for lc in range(2):
                            S.op("dve", lambda e: e.tensor_tensor(cqTn[:, lc, gs], latf[:, lc, :], rbc[:, 0, :], ALU.mult),
                                 reads=[("latf", lc), ("rbc", 0)], writes=["cqTn"])
                        S.op("dve", lambda e: e.tensor_tensor(ckvTn[:, gs], latf[:, 2, :], rbc[:, 1, :], ALU.mult),
                             reads=[("latf", 2), ("rbc", 1)], writes=["ckvTn"])
                        P = slice(64, 96)
                        S.dma("sp", lambda e: e.dma_start(out=posi[P, :], in_=positions[b:b + 1, gs].partition_broadcast(32)),
                              writes=["posi"])
                        S.op("dve", lambda e: e.tensor_copy(ang[P, :], posi[P, :]), reads=["posi"], writes=["ang"])
                        S.op("dve", lambda e: e.tensor_scalar(ang[P, :], ang[P, :], invf_sb[P, 0:1], None, ALU.mult),
                             reads=["ang", "invf"], writes=["ang"])
                        for (dst, dk, add) in ((sinT, "sinT", 0.0), (cosT, "cosT", float(np.pi / 2))):
                            S.op("dve", lambda e: e.tensor_scalar(ru[P, :], ang[P, :], add, None, ALU.add),
                                 reads=["ang"], writes=["ru"])
                            S.op("dve", lambda e: e.tensor_scalar(rtmp[P, :], ru[P, :], 1.0 / TWO_PI, None, ALU.mult),
                                 reads=["ru"], writes=["rtmp"])
                            S.op("dve", lambda e: e.tensor_copy(posi[P, :], rtmp[P, :]), reads=["rtmp"], writes=["posi"])
                            S.op("dve", lambda e: e.tensor_copy(rtmp[P, :], posi[P, :]), reads=["posi"], writes=["rtmp"])
                            S.op("dve", lambda e: e.scalar_tensor_tensor(rtmp[P, :], rtmp[P, :], -TWO_PI, ru[P, :], ALU.mult, ALU.add),
                                 reads=["rtmp", "ru"], writes=["rtmp"])
                            S.op("dve", lambda e: e.tensor_scalar(rtmp[P, :], rtmp[P, :], float(np.pi), float(-np.pi), ALU.min, ALU.max),
                                 reads=["rtmp"], writes=["rtmp"])
                            S.op("act", lambda e: e.activation(dst[P, gs], rtmp[P, :], AF.Sin), reads=["rtmp"], writes=[(dk, G)])
                        for (wt, wk, bn) in ((wkr, "wkr", 7), (wkr2, "wkr2", 5)):
                            for c in range(8):
                                S.op("pe", lambda e: e.matmul(bank(bn)[0:96, :], wt[:, c, :], hT[:, c, gs], start=(c == 0), stop=(c == 7)),
                                     reads=[wk, hTk], writes=[bk(bn)])
                        S.op("dve", lambda e: e.tensor_tensor(ta[P, :], bank(7)[P, :], cosT[P, gs], ALU.mult),
                             reads=[bk(7), ("cosT", G)], writes=["ta"])
                        S.op("dve", lambda e: e.tensor_tensor(tb[P, :], bank(5)[P, :], sinT[P, gs], ALU.mult),
                             reads=[bk(5), ("sinT", G)], writes=["tb"])
                        S.op("dve", lambda e: e.tensor_tensor(krope[P, gs], ta[P, :], tb[P, :], ALU.add),
                             reads=["ta", "tb"], writes=["krope"])
                    S.barrier()

                if debug and b == 0:
                    S.dma("sp", lambda e: e.dma_start(out=dbg_hT[:, :, :], in_=hT[:]), reads=[("hT", g) for g in range(4)], writes=["dbg_hT"])
                    S.barrier()
                def run_passes(proj_chunks, loop_iters, npass):
                    for ch in proj_chunks(0):
                        ch()
                    for j in range(npass):
                        nxt = S.record(proj_chunks(j + 1)) if j + 1 < npass else []
                        iters = loop_iters(j)
                        per = -(-len(nxt) // max(1, len(iters) - 6))
                        for idx, it in enumerate(iters):
                            it()
                            if idx >= 2:
                                for _ in range(per):
                                    if nxt:
                                        nxt.pop(0)()
                        while nxt:
                            nxt.pop(0)()

                with ExitStack() as sp_:
                    wsb2 = [sb(sp_, f"wsb{i}", [128, 8, 3, 128], BF16) for i in range(2)]
                    qs02 = [[sb(sp_, f"qs0_{i}_{h}", [128, 2048], BF16) for h in range(2)] for i in range(2)]
                    ksT2 = [sb(sp_, f"ksT{i}", [128, 2048], BF16) for i in range(2)]
                    vs2 = [sb(sp_, f"vs{i}", [128, 16, 128], BF16) for i in range(2)]
                    E1 = [sb(sp_, f"E1_{i}", [128, 512]) for i in range(4)]
                    Lb = [sb(sp_, f"Lb_{i}", [128, 512], BF16) for i in range(3)]
                    Xe = [sb(sp_, f"Xe_{i}", [128, 512]) for i in range(2)]
                    At = [sb(sp_, f"At_{i}", [128, 512], BF16) for i in range(2)]
                    S32 = [sb(sp_, f"S32_{i}", [128, 512]) for i in range(2)]
                    Sb = [sb(sp_, f"Sb_{i}", [128, 512], BF16) for i in range(2)]
                    wi_c = w_in.rearrange("(c p) n -> p c n", p=128)

                    def sb_proj_chunks(j):
                        pj = j % 2
                        wsb, qs0, ksT, vs = wsb2[pj], qs02[pj], ksT2[pj], vs2[pj]
                        chunks = []

                        def c_load():
                            for w in range(3):
                                S.dma("pool", lambda e: e.dma_start(out=wsb[:, :, w, :],
                                                                    in_=wi_c[:, :, w * 512 + j * 128:w * 512 + (j + 1) * 128]),
                                      writes=[("wsb", pj, w)])
                            S.op("dve", lambda e: e.memset(qs0[0][64:128, :], 0.0), writes=[("qs0", pj, 0, G) for G in range(4)])
                            S.op("dve", lambda e: e.memset(qs0[1][0:64, :], 0.0), writes=[("qs0", pj, 1, G) for G in range(4)])
                        chunks.append(c_load)
                        for G in range(4):
                            gs = slice(G * 512, (G + 1) * 512)

                            def c_q(G=G, gs=gs):
                                for c in range(8):
                                    S.op("pe", lambda e: e.matmul(bank(6), wsb[:, c, 0, :], hT[:, c, gs], start=(c == 0), stop=(c == 7)),
                                         reads=[("wsb", pj, 0), ("hT", G)], writes=[bk(6)])
                                S.op("dve", lambda e: e.tensor_scalar(qs0[0][0:64, gs], bank(6)[0:64, :], 0.125, None, ALU.mult),
                                     reads=[bk(6)], writes=[("qs0", pj, 0, G)])
                                S.op("dve", lambda e: e.tensor_scalar(qs0[1][64:128, gs], bank(6)[64:128, :], 0.125, None, ALU.mult),
                                     reads=[bk(6)], writes=[("qs0", pj, 1, G)])

                            def c_k(G=G, gs=gs):
                                for c in range(8):
                                    S.op("pe", lambda e: e.matmul(bank(7), wsb[:, c, 1, :], hT[:, c, gs], start=(c == 0), stop=(c == 7)),
                                         reads=[("wsb", pj, 1), ("hT", G)], writes=[bk(7)])
                                S.op("dve", lambda e: e.tensor_copy(ksT[:, gs], bank(7)), reads=[bk(7)], writes=[("ksT", pj, G)])

                            def c_v(G=G):
                                bn = 6 + (G % 2)
                                for tl in range(4):
                                    t = G * 4 + tl
                                    for c in range(8):
                                        S.op("pe", lambda e: e.matmul(bank(bn)[:, tl * 128:(tl + 1) * 128], hT[:, c, t * 128:(t + 1) * 128],
                                                                      wsb[:, c, 2, :], start=(c == 0), stop=(c == 7)),
                                             reads=[("wsb", pj, 2), ("hT", G)], writes=[bk(bn)])
                                S.op("dve", lambda e: e.tensor_copy(vs[:, G * 4:(G + 1) * 4, :], bank(bn).rearrange("p (t n) -> p t n", t=4)),
                                     reads=[bk(bn)], writes=[("vs", pj, G)])
                            chunks += [c_q, c_k, c_v]
                        return chunks

                    def sb_loop_iters(j):
                        pj = j % 2
                        qs0, ksT, vs = qs02[pj], ksT2[pj], vs2[pj]
                        units = [(hl, G, i) for G in range(4) for i in range(4 * G + 3, -1, -1) for hl in range(2)]
                        nU = len(units)

                        def geom(u):
                            hl, G, i = units[u]
                            q0 = max(i, 4 * G) * 128
                            off = q0 - G * 512
                            return dict(hl=hl, G=G, i=i, off=off, cs=slice(off, 512), qsl=slice(q0, (G + 1) * 512),
                                        ksl=slice(i * 128, (i + 1) * 128), diag=(i >= 4 * G), pr=slice(hl * 64, (hl + 1) * 64),
                                        first=(i == 4 * G + 3), last=(i == 0), gs=slice(G * 512, (G + 1) * 512))

                        def st0(u):
                            g = geom(u)
                            zb = u % 2
                            S.op("pe", lambda e: e.matmul(bank(zb)[:, g["cs"]], ksT[:, g["ksl"]], qs0[g["hl"]][:, g["qsl"]], start=True, stop=True),
                                 reads=[("ksT", pj, g["i"] // 4), ("qs0", pj, g["hl"], g["G"])], writes=[bk(zb)])

                        def st1a(u):
                            g = geom(u)
                            zb, cs, off = u % 2, g["cs"], g["off"]
                            e1, e1k = E1[u % 4], f"E1_{u % 4}"
                            S.op("act", lambda e: e.activation(e1[:, cs], bank(zb)[:, cs], AF.Exp), reads=[bk(zb)], writes=[e1k])
                            if g["diag"]:
                                S.op("dve", lambda e: e.tensor_tensor(e1[:, off:off + 128], e1[:, off:off + 128], mstrict, ALU.mult),
                                     reads=[e1k, "cst"], writes=[e1k])

                        def st1b(u):
                            g = geom(u)
                            cs = g["cs"]
                            e1, lb = E1[u % 4], Lb[u % 3]
                            e1k, lbk = f"E1_{u % 4}", f"Lb_{u % 3}"
                            S.op("act", lambda e: e.activation(lb[:, cs], e1[:, cs], AF.Ln, bias=onec[:], scale=1.0),
                                 reads=[e1k, "onec"], writes=[lbk])

                        def st2a(u):
                            g = geom(u)
                            cs, hl = g["cs"], g["hl"]
                            cbk = 2 + u % 2
                            lb, xe = Lb[u % 3], Xe[u % 2]
                            lbk, xek = f"Lb_{u % 3}", f"Xe_{u % 2}"
                            S.op("pe", lambda e: e.matmul(bank(cbk)[:, cs], tri, lb[:, cs], start=True, stop=g["first"]),
                                 reads=["cst", lbk], writes=[bk(cbk)])
                            if not g["first"]:
                                S.op("pe", lambda e: e.matmul(bank(cbk)[:, cs], ones_bf[:], Sb[hl][:, cs], start=False, stop=True),
                                     reads=["ones_bf", f"Sb_{hl}"], writes=[bk(cbk)])
                            S.op("act", lambda e: e.activation(xe[:, cs], bank(cbk)[:, cs], AF.Exp, scale=-1.0), reads=[bk(cbk)], writes=[xek])

                        def st2b(u):
                            g = geom(u)
                            cs, hl = g["cs"], g["hl"]
                            e1, lb, xe, at = E1[u % 4], Lb[u % 3], Xe[u % 2], At[u % 2]
                            e1k, lbk, xek, atk = f"E1_{u % 4}", f"Lb_{u % 3}", f"Xe_{u % 2}", f"At_{u % 2}"
                            if not g["last"]:
                                if g["first"]:
                                    S.op("dve", lambda e: e.memset(S32[hl][:], 0.0), writes=[f"S32_{hl}"])
                                S.op("dve", lambda e: e.tensor_tensor(S32[hl][:, cs], S32[hl][:, cs], lb[:, cs], ALU.add),
                                     reads=[f"S32_{hl}", lbk], writes=[f"S32_{hl}"])
                            S.op("dve", lambda e: e.tensor_tensor(at[:, cs], xe[:, cs], e1[:, cs], ALU.mult), reads=[xek, e1k], writes=[atk])
                            if not g["last"]:
                                S.op("dve", lambda e: e.tensor_copy(Sb[hl][:], S32[hl][:]), reads=[f"S32_{hl}"], writes=[f"Sb_{hl}"])

                        def st3(u):
                            g = geom(u)
                            cs = g["cs"]
                            ob = 4 + g["hl"]
                            at, atk = At[u % 2], f"At_{u % 2}"
                            S.op("pe", lambda e: e.matmul(bank(ob)[0:64, cs], vs[:, g["i"], g["pr"]], at[:, cs], start=g["first"], stop=g["last"],
                                                          skip_group_check=True),
                                 reads=[("vs", pj, g["i"] // 4), atk], writes=[bk(ob)])
                            if g["last"]:
                                S.op("act", lambda e: e.copy(oT[g["pr"], j, g["gs"]], bank(ob)[0:64, :]), reads=[bk(ob)], writes=[("oT", j)])

                        def mk(k):
                            def it():
                                if 0 <= k < nU:
                                    st1b(k)
                                if 0 <= k + 1 < nU:
                                    st1a(k + 1)
                                if 0 <= k - 1 < nU:
                                    st2a(k - 1)
                                if 0 <= k + 2 < nU:
                                    st0(k + 2)
                                if 0 <= k - 1 < nU:
                                    st2b(k - 1)
                                if 0 <= k - 2 < nU:
                                    st3(k - 2)
                            return it
                        return [mk(k) for k in range(-2, nU + 2)]

                    run_passes(sb_proj_chunks, sb_loop_iters, 4)
                    S.barrier()

                with ExitStack() as sm:
                    qmT2 = [sb(sm, f"qmT{i}", [128, 2, 2048], BF16) for i in range(2)]
                    kmT2 = [sb(sm, f"kmT{i}", [128, 2, 2048], BF16) for i in range(2)]
                    vm2 = [sb(sm, f"vm{i}", [128, 16, 2, 65], BF16) for i in range(2)]
                    Et = [sb(sm, f"Et_{i}", [128, 512], BF16) for i in range(3)]
                    dr = sb(sm, "dr", [128, 512])
                    rb = sb(sm, "rb", [128, 512])
                    ta = sb(sm, "ta", [128, 512])
                    tb = sb(sm, "tb", [128, 512])
                    P = slice(64, 96)
                    scale = float(96 ** -0.5)

                    def mla_proj_chunks(m):
                        pm = m % 2
                        qmT, kmT, vm = qmT2[pm], kmT2[pm], vm2[pm]
                        chunks = []

                        def c_init():
                            S.op("dve", lambda e: e.memset(vm[:], 1.0), writes=[("vm", pm, G) for G in range(4)])
                            S.op("dve", lambda e: e.memset(qmT[96:128], 0.0), writes=[("qmT", pm, G) for G in range(4)])
                            S.op("dve", lambda e: e.memset(kmT[96:128], 0.0), writes=[("kmT", pm, G) for G in range(4)])
                        chunks.append(c_init)
                        for G in range(4):
                            gs = slice(G * 512, (G + 1) * 512)
                            for hl in range(2):
                                def c_qk(G=G, gs=gs, hl=hl):
                                    h = 2 * m + hl
                                    for (wt, wk, bn) in ((w_uq_sb, "w_uq", 6), (w2_sb, "w2", 7)):
                                        for c in range(2):
                                            S.op("pe", lambda e: e.matmul(bank(bn)[0:96, :], wt[:, c, h, :], cqTn[:, c, gs], start=(c == 0), stop=(c == 1)),
                                                 reads=[wk, "cqTn"], writes=[bk(bn)])
                                    S.op("dve", lambda e: e.tensor_copy(qmT[0:64, hl, gs], bank(6)[0:64, :]), reads=[bk(6)], writes=[("qmT", pm, G)])
                                    S.op("dve", lambda e: e.tensor_tensor(ta[P, :], bank(6)[P, :], cosT[P, gs], ALU.mult),
                                         reads=[bk(6), ("cosT", G)], writes=["ta"])
                                    S.op("dve", lambda e: e.tensor_tensor(tb[P, :], bank(7)[P, :], sinT[P, gs], ALU.mult),
                                         reads=[bk(7), ("sinT", G)], writes=["tb"])
                                    S.op("dve", lambda e: e.tensor_tensor(qmT[P, hl, gs], ta[P, :], tb[P, :], ALU.add),
                                         reads=["ta", "tb"], writes=[("qmT", pm, G)])
                                    S.op("pe", lambda e: e.matmul(bank(6)[0:64, :], w_ukv_sb[:, h * 128:h * 128 + 64], ckvTn[:, gs], start=True, stop=True),
                                         reads=["w_ukv", "ckvTn"], writes=[bk(6)])
                                    S.op("dve", lambda e: e.tensor_copy(kmT[0:64, hl, gs], bank(6)[0:64, :]), reads=[bk(6)], writes=[("kmT", pm, G)])
                                    S.op("dve", lambda e: e.tensor_copy(kmT[P, hl, gs], krope[P, gs]), reads=["krope"], writes=[("kmT", pm, G)])
                                chunks.append(c_qk)

                            def c_v(G=G):
                                bn = 7
                                for tl in range(4):
                                    t = G * 4 + tl
                                    S.op("pe", lambda e: e.matmul(bank(bn)[:, tl * 128:(tl + 1) * 128], ckvTn[:, t * 128:(t + 1) * 128],
                                                                  wv_sb[:, 2 * m:2 * m + 2, :], start=True, stop=True),
                                         reads=["wv", "ckvTn"], writes=[bk(bn)])
                                S.op("dve", lambda e: e.tensor_copy(vm[:, G * 4:(G + 1) * 4, :, 0:64],
                                                                    bank(bn).rearrange("p (t h d) -> p t h d", t=4, h=2)),
                                     reads=[bk(bn)], writes=[("vm", pm, G)])
                            chunks.append(c_v)
                        return chunks

                    def mla_loop_iters(m):
                        pm = m % 2
                        qmT, kmT, vm = qmT2[pm], kmT2[pm], vm2[pm]
                        units = [(hl, G, i) for G in range(4) for i in range(0, 4 * G + 4) for hl in range(2)]
                        nU = len(units)

                        def geom(u):
                            hl, G, i = units[u]
                            q0 = max(i, 4 * G) * 128
                            off = q0 - G * 512
                            return dict(hl=hl, G=G, i=i, off=off, cs=slice(off, 512), qsl=slice(q0, (G + 1) * 512),
                                        ksl=slice(i * 128, (i + 1) * 128), diag=(i >= 4 * G), pr=slice(hl * 64, (hl + 1) * 64),
                                        first=(i == 0), last=(i == 4 * G + 3), gs=slice(G * 512, (G + 1) * 512))

                        def st0(u):
                            g = geom(u)
                            zb = u % 3
                            S.op("pe", lambda e: e.matmul(bank(zb)[:, g["cs"]], kmT[:, g["hl"], g["ksl"]], qmT[:, g["hl"], g["qsl"]],
                                                          start=True, stop=True),
                                 reads=[("kmT", pm, g["i"] // 4), ("qmT", pm, g["G"])], writes=[bk(zb)])

                        def st1(u):
                            g = geom(u)
                            zb, cs, off = u % 3, g["cs"], g["off"]
                            et, etk = Et[u % 3], f"Et_{u % 3}"
                            S.op("act", lambda e: e.activation(et[:, cs], bank(zb)[:, cs], AF.Exp, scale=scale), reads=[bk(zb)], writes=[etk])
                            if g["diag"]:
                                S.op("dve", lambda e: e.tensor_tensor(et[:, off:off + 128], et[:, off:off + 128], mcausal, ALU.mult),
                                     reads=[etk, "cst"], writes=[etk])

                        def st2(u):
                            g = geom(u)
                            cs = g["cs"]
                            ob = 4 + g["hl"]
                            et, etk = Et[u % 3], f"Et_{u % 3}"
                            S.op("pe", lambda e: e.matmul(bank(ob)[0:65, cs], vm[:, g["i"], g["hl"], :], et[:, cs], start=g["first"], stop=g["last"],
                                                          skip_group_check=True),
                                 reads=[("vm", pm, g["i"] // 4), etk], writes=[bk(ob)])
                            if g["last"]:
                                S.op("act", lambda e: e.activation(dr[64:65, :], bank(ob)[64:65, :], AF.Ln), reads=[bk(ob)], writes=["dr"])

                                def fin(ob=ob, pr=g["pr"], gs=g["gs"]):
                                    S.op("pe", lambda e: e.matmul(bank(3)[0:64, :], ones_f[64:65, 0:64], dr[64:65, :], start=True, stop=True),
                                         reads=["ones_f", "dr"], writes=[bk(3)])
                                    S.op("act", lambda e: e.activation(rb[0:64, :], bank(3)[0:64, :], AF.Exp, scale=-1.0), reads=[bk(3)], writes=["rb"])
                                    S.op("dve", lambda e: e.tensor_tensor(oT[pr, 4 + m, gs], bank(ob)[0:64, :], rb[0:64, :], ALU.mult),
                                         reads=[bk(ob), "rb"], writes=[("oT", 4 + m)])
                                deferred.append(fin)

                        deferred = []

                        def mk(k):
                            def it():
                                if 0 <= k + 1 < nU:
                                    st0(k + 1)
                                if 0 <= k < nU:
                                    st1(k)
                                pend = list(deferred)
                                del deferred[:]
                                for f in pend:
                                    f()
                                if 0 <= k - 1 < nU:
                                    st2(k - 1)
                                if k >= nU:
                                    for f in list(deferred):
                                        f()
                                    del deferred[:]
                            return it
                        return [mk(k) for k in range(-1, nU + 1)]

                    run_passes(mla_proj_chunks, mla_loop_iters, 4)
                    S.barrier()

                if debug and b == 0:
                    S.dma("sp", lambda e: e.dma_start(out=dbg_oT[:, :, :], in_=oT[:]), reads=[("oT", jj) for jj in range(8)], writes=["dbg_oT"])
                    S.dma("sp", lambda e: e.dma_start(out=dbg_lat[:, 0:2, :], in_=cqTn[:]), reads=["cqTn"], writes=["dbg_lat0"])
                    S.dma("sp", lambda e: e.dma_start(out=dbg_lat[:, 2, :], in_=ckvTn[:]), reads=["ckvTn"], writes=["dbg_lat1"])
                    S.dma("sp", lambda e: e.dma_start(out=dbg_lat[64:96, 3, :], in_=krope[64:96, :]), reads=["krope"], writes=["dbg_lat2"])
                    S.barrier()
                with ExitStack() as sd:
                    xts = [sb(sd, f"xd{i}", [128, 1024]) for i in range(2)]
                    x1t = [sb(sd, f"x1t{i}", [128, 1024]) for i in range(2)]
                    h2b = [sb(sd, f"h2b{i}", [128, 1024], BF16) for i in range(2)]
                    junk = sb(sd, "junkd", [128, 1024], BF16)
                    osq2 = [sb(sd, f"osq{i}", [128, 8, 128], BF16) for i in range(2)]
                    rsDall = sb(sd, "rsDall", [128, 32])
                    ssD = sb(sd, "ssD", [128, 2])
                    rsD = sb(sd, "rsD", [128, 2])
                    ss2 = sb(sd, "ss2", [128, 1])
                    rs2 = sb(sd, "rs2", [128, 1])
                    lgall = sb(sd, "lgall", [128, 16, 36])
                    lgraw = sb(sd, "lgraw", [128, 16, 36])
                    ss2all = sb(sd, "ss2all", [128, 16])
                    gmax = sb(sd, "gmax", [128, 16, 1])
                    ohg = sb(sd, "ohg", [128, 16, 4])
                    gsh = sb(sd, "gsh", [128, 16, 4])
                    gex = sb(sd, "gex", [128, 16, 4])
                    gsum = sb(sd, "gsum", [128, 16, 1])
                    gval = sb(sd, "gval", [128, 16, 1])
                    tmp4 = sb(sd, "tmp4", [128, 16, 4, 8])
                    loc = sb(sd, "loc", [128, 16, 8])
                    loc2 = sb(sd, "loc2", [128, 16, 8])
                    l1 = sb(sd, "l1", [128, 16, 1])
                    l2 = sb(sd, "l2", [128, 16, 1])
                    m1 = sb(sd, "m1", [128, 16, 8])
                    m2 = sb(sd, "m2", [128, 16, 8])
                    dd = sb(sd, "dd", [128, 16, 1])
                    s1 = sb(sd, "s1", [128, 16, 1])
                    w1 = sb(sd, "w1", [128, 16, 1])
                    w2 = sb(sd, "w2", [128, 16, 1])
                    wl = sb(sd, "wl", [128, 16, 8])
                    wl2 = sb(sd, "wl2", [128, 16, 8])
                    gbc = sb(sd, "gbc", [128, 1024])
                    S.dma("sp", lambda e: e.dma_start(out=gbc[:], in_=ffn_norm[0:1, :].partition_broadcast(128)), writes=["gbc"])
                    def d_partA(t):
                        ts_ = slice(t * 128, (t + 1) * 128)
                        xt, xk = xts[t % 2], f"xd{t % 2}"
                        x1, x1k = x1t[t % 2], f"x1t{t % 2}"
                        hb, hk = h2b[t % 2], f"h2b{t % 2}"
                        S.dma("sp", lambda e: e.dma_start(out=xt[:], in_=x[b, ts_, :]), writes=[xk])
                        for grp in range(2):
                            for hh in range(2):
                                bn = grp * 2 + hh
                                for jl in range(4):
                                    jj = grp * 4 + jl
                                    S.op("pe", lambda e: e.matmul(bank(bn), oT[:, jj, ts_], w_out_sb[:, jj, hh * 512:(hh + 1) * 512],
                                                                  start=(jl == 0), stop=(jl == 3)),
                                         reads=[("oT", jj), "w_out"], writes=[bk(bn)])
                        S.op("dve", lambda e: e.scalar_tensor_tensor(x1[:], PS[0][:], rsDall[:, 2 * t:2 * t + 1], xt[:], ALU.mult, ALU.add),
                             reads=[bk(0), bk(1), "rsDall", xk], writes=[x1k])
                        S.op("dve", lambda e: e.scalar_tensor_tensor(x1[:], PS[1][:], rsDall[:, 2 * t + 1:2 * t + 2], x1[:], ALU.mult, ALU.add),
                             reads=[bk(2), bk(3), "rsDall", x1k], writes=[x1k])
                        S.dma("sp", lambda e: e.dma_start(out=x1s[b, ts_, :], in_=x1[:]), reads=[x1k], writes=[("x1s", b, t)])
                        S.op("act", lambda e: e.activation(junk[:], x1[:], AF.Square, accum_out=ss2all[:, t:t + 1]),
                             reads=[x1k, "ss2init"], writes=["junkd", ("ss2all", t)])
                        S.op("dve", lambda e: e.tensor_tensor(hb[:], x1[:], gbc[:], ALU.mult), reads=[x1k, "gbc"], writes=[hk])

                    def d_partB(t):
                        ts_ = slice(t * 128, (t + 1) * 128)
                        x1, x1k = x1t[t % 2], f"x1t{t % 2}"
                        hb, hk = h2b[t % 2], f"h2b{t % 2}"
                        transpose_to_hT(hb, hk, t, 5)
                        for c in range(8):
                            S.op("pe", lambda e: e.matmul(bank(6)[:, 0:36], hT[:, c, ts_], wr_sb[:, c, :], start=(c == 0), stop=(c == 7)),
                                 reads=[("hT", t // 4), ("hTt", t), "wr"], writes=[bk(6)])
                        S.op("dve", lambda e: e.tensor_copy(lgraw[:, t, :], bank(6)[:, 0:36]), reads=[bk(6)], writes=["lgraw"])

                    S.op("dve", lambda e: e.memset(ss2all[:], 0.0), writes=["ss2init"] + [("ss2all", t) for t in range(16)])
                    for t in range(16):
                        ts_ = slice(t * 128, (t + 1) * 128)
                        oq, oqk = osq2[t % 2], f"osq{t % 2}"
                        S.op("dve", lambda e: e.tensor_tensor(oq[:], oT[:, :, ts_], oT[:, :, ts_], ALU.mult),
                             reads=[("oT", jj) for jj in range(8)], writes=[oqk])
                        for grp in range(2):
                            for jl in range(4):
                                S.op("pe", lambda e: e.matmul(bank(4)[:, 2 * t + grp:2 * t + grp + 1], oq[:, grp * 4 + jl, :], ones_bf[:, 0:1],
                                                              start=(t == 0 and jl == 0 and grp == 0), stop=(t == 15 and grp == 1 and jl == 3),
                                                              skip_group_check=True),
                                     reads=[oqk, "ones_bf"], writes=[bk(4)])
                    rstd_from_ss(rsDall[:], bank(4)[:, 0:32], 512.0, "rsDall", bk(4))
                    for t in range(17):
                        if t < 16:
                            d_partA(t)
                        if t >= 1:
                            d_partB(t - 1)
                    S.op("act", lambda e: e.activation(rs2all[:], ss2all[:], AF.Sqrt, bias=epsc[:], scale=1.0 / 1024.0),
                         reads=[("ss2all", t) for t in range(16)] + ["epsc"], writes=["rs2all"])
                    S.op("dve", lambda e: e.reciprocal(rs2all[:], rs2all[:]), reads=["rs2all"], writes=["rs2all"])
                    rs2b = rs2all[:].rearrange("p (t o) -> p t o", o=1)
                    S.op("dve", lambda e: e.tensor_tensor(lgall[:], lgraw[:], rs2b.to_broadcast([128, 16, 36]), ALU.mult),
                         reads=["lgraw", "rs2all"], writes=["lgall"])
                    S.op("dve", lambda e: e.tensor_tensor(lgall[:], lgall[:], br_bc[:].rearrange("p (o n) -> p o n", o=1).to_broadcast([128, 16, 36]), ALU.add),
                         reads=["lgall", "br"], writes=["lgall"])
                    T4 = [128, 16, 4]
                    T8 = [128, 16, 8]
                    T48 = [128, 16, 4, 8]
                    glg = lgall[:, :, 0:4]
                    elg = lgall[:, :, 4:36].rearrange("p t (g k) -> p t g k", g=4)
                    ohg4 = ohg[:].rearrange("p t (g o) -> p t g o", o=1)
                    V = lambda fn, r, w: S.op("dve", fn, reads=r, writes=w)
                    V(lambda e: e.reduce_max(gmax[:], glg, axis=AX.X), ["lgall"], ["gmax"])
                    V(lambda e: e.tensor_tensor(ohg[:], glg, gmax[:].to_broadcast(T4), ALU.is_equal), ["lgall", "gmax"], ["ohg"])
                    V(lambda e: e.tensor_tensor(gsh[:], glg, gmax[:].to_broadcast(T4), ALU.subtract), ["lgall", "gmax"], ["gsh"])
                    S.op("act", lambda e: e.activation(gex[:], gsh[:], AF.Exp), reads=["gsh"], writes=["gex"])
                    V(lambda e: e.reduce_sum(gsum[:], gex[:], axis=AX.X), ["gex"], ["gsum"])
                    V(lambda e: e.reciprocal(gval[:], gsum[:]), ["gsum"], ["gval"])
                    V(lambda e: e.tensor_tensor(tmp4[:], elg, ohg4.to_broadcast(T48), ALU.mult), ["lgall", "ohg"], ["tmp4"])
                    V(lambda e: e.reduce_sum(loc[:].rearrange("p t (k o) -> p t k o", o=1), tmp4[:].rearrange("p t g k -> p t k g"), axis=AX.X),
                      ["tmp4"], ["loc"])
                    V(lambda e: e.reduce_max(l1[:], loc[:], axis=AX.X), ["loc"], ["l1"])
                    V(lambda e: e.tensor_tensor(m1[:], loc[:], l1[:].to_broadcast(T8), ALU.is_equal), ["loc", "l1"], ["m1"])
                    V(lambda e: e.scalar_tensor_tensor(loc2[:], m1[:], -1e30, loc[:], ALU.mult, ALU.add), ["m1", "loc"], ["loc2"])
                    V(lambda e: e.reduce_max(l2[:], loc2[:], axis=AX.X), ["loc2"], ["l2"])
                    V(lambda e: e.tensor_tensor(m2[:], loc2[:], l2[:].to_broadcast(T8), ALU.is_equal), ["loc2", "l2"], ["m2"])
                    V(lambda e: e.tensor_tensor(dd[:], l2[:], l1[:], ALU.subtract), ["l1", "l2"], ["dd"])
                    S.op("act", lambda e: e.activation(s1[:], dd[:], AF.Exp), reads=["dd"], writes=["s1"])
                    V(lambda e: e.tensor_scalar(s1[:], s1[:], 1.0, None, ALU.add), ["s1"], ["s1"])
                    V(lambda e: e.reciprocal(s1[:], s1[:]), ["s1"], ["s1"])
                    V(lambda e: e.tensor_tensor(w1[:], gval[:], s1[:], ALU.mult), ["gval", "s1"], ["w1"])
                    V(lambda e: e.tensor_tensor(w2[:], gval[:], w1[:], ALU.subtract), ["gval", "w1"], ["w2"])
                    V(lambda e: e.tensor_tensor(wl[:], m1[:], w1[:].to_broadcast(T8), ALU.mult), ["m1", "w1"], ["wl"])
                    V(lambda e: e.tensor_tensor(wl2[:], m2[:], w2[:].to_broadcast(T8), ALU.mult), ["m2", "w2"], ["wl2"])
                    V(lambda e: e.tensor_tensor(wl[:], wl[:], wl2[:], ALU.add), ["wl", "wl2"], ["wl"])
                    V(lambda e: e.tensor_tensor(Wfull[:].rearrange("p t (g k) -> p t g k", g=4), ohg4.to_broadcast(T48),
                                                wl[:].rearrange("p t (o k) -> p t o k", o=1).to_broadcast(T48), ALU.mult),
                      ["ohg", "wl"], [("Wfull", t) for t in range(16)])
                    V(lambda e: e.tensor_tensor(Wfull[:], Wfull[:], rs2b.to_broadcast([128, 16, 32]), ALU.mult),
                      [("Wfull", t) for t in range(16)] + ["rs2all"], [("Wfull", t) for t in range(16)])
                    S.barrier()

            if "moe" in phases:
              with ExitStack() as s2:
                acc = sb(s2, "acc", [128, 16, 1024])
                NW = 3
                wgu = [sb(s2, f"wgu{i}", [128, 8, 512], BF16) for i in range(NW)]
                wd = [sb(s2, f"wd{i}", [128, 2, 1024], BF16) for i in range(NW)]
                sg = [sb(s2, f"sg{i}", [128, 256]) for i in range(2)]
                hid = [sb(s2, f"hid{i}", [128, 256], BF16) for i in range(2)]
                hidT = [sb(s2, f"hidT{i}", [128, 256], BF16) for i in range(2)]
                junk = sb(s2, "junke", [128, 1024], BF16)
                ss3 = sb(s2, "ss3", [128, 2])
                rs3 = sb(s2, "rs3", [128, 2])
                yt = [sb(s2, f"yt{i}", [128, 1024]) for i in range(2)]
                gbc = sb(s2, "gbc", [128, 1024])
                S.dma("sp", lambda e: e.dma_start(out=gbc[:], in_=final_norm[0:1, :].partition_broadcast(128)), writes=["gbc"])
                for t in range(16):
                    S.dma("sp", lambda e: e.dma_start(out=acc[:, t, :], in_=x1s[b, t * 128:(t + 1) * 128, :]),
                          reads=[("x1s", b, t)], writes=[("acc", t)])

                def load_expert(ex):
                    sl = ex % NW
                    S.dma("pool", lambda e: e.dma_start(out=wgu[sl][:, :, 0:256], in_=w_gate[ex].rearrange("(c p) n -> p c n", p=128)),
                          writes=[f"wgu{sl}"])
                    S.dma("pool", lambda e: e.dma_start(out=wgu[sl][:, :, 256:512], in_=w_up[ex].rearrange("(c p) n -> p c n", p=128)),
                          writes=[f"wgu{sl}"])
                    S.dma("pool", lambda e: e.dma_start(out=wd[sl][:], in_=w_down[ex].rearrange("(c p) n -> p c n", p=128)),
                          writes=[f"wd{sl}"])

                for ex0 in range(min(NW, n_experts)):
                    load_expert(ex0)
                steps = [(ex, t) for ex in range(n_experts) for t in range(16)]
                nS = len(steps)

                def m_gu(k):
                    ex, t = steps[k]
                    sl, p2, ts_ = ex % NW, k % 2, slice(t * 128, (t + 1) * 128)
                    gb = p2
                    for c in range(8):
                        S.op("pe", lambda e: e.matmul(bank(gb), hT[:, c, ts_], wgu[sl][:, c, :], start=(c == 0), stop=(c == 7)),
                             reads=[("hT", t // 4), ("hTt", t), f"wgu{sl}"], writes=[bk(gb)])
                    S.op("act", lambda e: e.activation(sg[p2][:], bank(gb)[:, 0:256], AF.Silu, scale=rs2all[:, t:t + 1]), reads=[bk(gb), "rs2all"], writes=[f"sg{p2}"])
                    S.op("dve", lambda e: e.scalar_tensor_tensor(hid[p2][:], bank(gb)[:, 256:512], Wfull[:, t, ex:ex + 1], sg[p2][:],
                                                                 ALU.mult, ALU.mult),
                         reads=[bk(gb), ("Wfull", t), f"sg{p2}"], writes=[f"hid{p2}"])

                def m_tr(k):
                    p2 = k % 2
                    tbk = 2 + p2
                    tb_ = bank_bf(tbk)
                    for c in range(2):
                        S.op("pe", lambda e: e.transpose(tb_[:, c * 128:(c + 1) * 128], hid[p2][:, c * 128:(c + 1) * 128], ident),
                             reads=[f"hid{p2}", "cst"], writes=[bk(tbk)])
                    S.op("act", lambda e: e.copy(hidT[p2][:], tb_[:, 0:256]), reads=[bk(tbk)], writes=[f"hidT{p2}"])

                def m_dn(k):
                    ex, t = steps[k]
                    sl, p2 = ex % NW, k % 2
                    dps = 2 + p2
                    for hh in range(2):
                        for c in range(2):
                            S.op("pe", lambda e: e.matmul(PS[dps][:, hh * 512:(hh + 1) * 512], hidT[p2][:, c * 128:(c + 1) * 128],
                                                          wd[sl][:, c, hh * 512:(hh + 1) * 512], start=(c == 0), stop=(c == 1)),
                                 reads=[f"hidT{p2}", f"wd{sl}"], writes=[bk(2 * dps + hh)])
                    S.op("dve", lambda e: e.tensor_tensor(acc[:, t, :], acc[:, t, :], PS[dps][:], ALU.add),
                         reads=[("acc", t), bk(2 * dps), bk(2 * dps + 1)], writes=[("acc", t)])
                    if t == 15 and ex + NW < n_experts:
                        load_expert(ex + NW)

                for k in range(nS + 2):
                    if k < nS:
                        m_gu(k)
                    if 0 <= k - 1 < nS:
                        m_tr(k - 1)
                    if 0 <= k - 2 < nS:
                        m_dn(k - 2)
                for t in range(16):
                    sl = t % 2
                    S.op("dve", lambda e: e.memset(ss3[:, sl:sl + 1], 0.0), writes=[f"ss3{sl}"])
                    S.op("act", lambda e: e.activation(junk[:], acc[:, t, :], AF.Square, accum_out=ss3[:, sl:sl + 1]),
                         reads=[("acc", t)], writes=["junke", f"ss3{sl}"])
                    rstd_from_ss(rs3[:, sl:sl + 1], ss3[:, sl:sl + 1], 1024.0, f"rs3{sl}", f"ss3{sl}")
                    S.op("dve", lambda e: e.scalar_tensor_tensor(yt[sl][:], acc[:, t, :], rs3[:, sl:sl + 1], gbc[:], ALU.mult, ALU.mult),
                         reads=[("acc", t), f"rs3{sl}", "gbc"], writes=[f"yt{sl}"])
                    S.dma("sp", lambda e: e.dma_start(out=y[b, t * 128:(t + 1) * 128, :], in_=yt[sl][:]), reads=[f"yt{sl}"], writes=[("y", b, t)])
                S.barrier()
        S.final_wait("sp")
        print("instructions emitted:", S.nins, {e: S.cnt[e] for e in ENGS}, S.ndma)
    return nc


def _consts():
    k = np.arange(128)[:, None]
    q = np.arange(128)[None, :]
    ident = (k == q)
    tri = (k >= q)
    strict = (k < q)
    causal = (k <= q)
    c = np.concatenate([ident, tri, strict, causal, strict], axis=1).astype(np.float32)
    half = 16
    inv_freq = (np.float32(10000.0) ** (-np.arange(half, dtype=np.float32) / np.float32(half))).astype(np.float32)
    invf = np.zeros((128, 1), np.float32)
    for p in range(64, 96):
        invf[p, 0] = inv_freq[(p - 64) % 16]
    return c.astype(ml_dtypes.bfloat16), invf


def make_in_maps(inputs, n_cores, nseq):
    f = lambda a: np.ascontiguousarray(np.asarray(a))
    cst, invf = _consts()
    shared = {
        "attn_norm": f(inputs["attn_norm"]).reshape(1, 1024),
        "w_in": f(inputs["w_in"]).reshape(1024, 1952),
        "q_norm": f(inputs["q_norm"]).reshape(256, 1),
        "w_uq": f(inputs["w_uq"]).reshape(256, 768),
        "kv_norm": f(inputs["kv_norm"]).reshape(128, 1),
        "w_ukv": f(inputs["w_ukv"]).reshape(128, 1024),
        "out_norm": np.concatenate([f(inputs["sb_out_norm"]).reshape(-1), f(inputs["mla_out_norm"]).reshape(-1)]).reshape(1024, 1),
        "w_out": f(inputs["w_out"]).reshape(1024, 1024),
        "ffn_norm": f(inputs["ffn_norm"]).reshape(1, 1024),
        "w_group_router": f(inputs["w_group_router"]).reshape(1024, 4),
        "b_group_router": f(inputs["b_group_router"]).reshape(1, 4),
        "w_expert_router": f(inputs["w_expert_router"]).reshape(1024, 32),
        "b_expert_router": f(inputs["b_expert_router"]).reshape(1, 32),
        "w_gate": f(inputs["w_gate"]).reshape(32, 1024, 256),
        "w_up": f(inputs["w_up"]).reshape(32, 1024, 256),
        "w_down": f(inputs["w_down"]).reshape(32, 256, 1024),
        "final_norm": f(inputs["final_norm"]).reshape(1, 1024),
        "consts": cst,
        "invf": invf,
    }
    xs = f(inputs["x"])
    ps = f(inputs["positions"]).astype(np.int32)
    maps = []
    for c in range(n_cores):
        m = dict(shared)
        m["x"] = xs[c * nseq:(c + 1) * nseq]
        m["positions"] = ps[c * nseq:(c + 1) * nseq]
        maps.append(m)
    return maps


def kernel(**inputs):
    n_cores = 8
    nseq = 4
    nc = build(NSEQ=nseq)
    maps = make_in_maps(inputs, n_cores, nseq)
    res = run_bass_kernel_spmd(nc, maps, core_ids=list(range(n_cores)))
    out = np.concatenate([np.asarray(r["y"]) for r in res.results], axis=0)
    return out.astype(np.float32)
```
